# Optimizing a Trainium2 kernel written in Bass

```python
import math
import jax
import jax.numpy as jnp
from jax import lax
import numpy as np


D_MODEL = 1024
BATCH = 32
SEQ = 2048
DEPTH = 2

GRID_W = 64
CTX_LEN = 256

DA_HEADS = 4
DA_HEAD_DIM = 64
DA_WIDTH = DA_HEADS * 2 * DA_HEAD_DIM
Q_BLOCK = 128
ROPE_BASE = 10000.0
CV_WIDTH = 512
CV_KERNEL = 31
GLA_HEADS = 4
GLA_DK = 64
GLA_DV = 128
GLA_KW = GLA_HEADS * GLA_DK
GLA_VW = GLA_HEADS * GLA_DV
GLA_RANK = 16
GLA_NORMALIZER = 16.0
GLA_CHUNK = 64
N_BRANCH = 3
D_FF = 2816
N_EXPERTS = 8
TOP_K = 2
EXPERT_FF = 2816
N_DENSE = (DEPTH + 1) // 2
N_MOE = DEPTH // 2
EPS = 1e-6

IN_SPLITS = (DA_WIDTH, DA_WIDTH, DA_WIDTH, 2 * CV_WIDTH, GLA_KW, GLA_KW, GLA_VW, GLA_VW, 2 * GLA_RANK, N_BRANCH * D_MODEL)
IN_WIDTH = 3 * DA_WIDTH + 2 * CV_WIDTH + 2 * GLA_KW + 2 * GLA_VW + 2 * GLA_RANK + N_BRANCH * D_MODEL

kernel_name = 'hybrid_diffattn_conformer_gla_moe_block'


def rms_norm(x, g):
    xf = x.astype(jnp.float32)
    y = xf * lax.rsqrt(jnp.mean(xf * xf, axis=-1, keepdims=True) + EPS)
    return (y * g.astype(jnp.float32)).astype(x.dtype)


def layer_norm(x, g, b):
    xf = x.astype(jnp.float32)
    mu = jnp.mean(xf, axis=-1, keepdims=True)
    xc = xf - mu
    var = jnp.mean(xc * xc, axis=-1, keepdims=True)
    return (xc * lax.rsqrt(var + EPS) * g.astype(jnp.float32) + b.astype(jnp.float32)).astype(x.dtype)


def modulate(xn, shift, scale):
    return xn * (1.0 + scale) + shift


def split_in(z):
    out = []
    off = 0
    for w in IN_SPLITS:
        out.append(z[..., off:off + w])
        off += w
    return out


def axial_rope(L):
    rows = L // GRID_W
    row = jnp.repeat(jnp.arange(rows), GRID_W)
    col = jnp.tile(jnp.arange(GRID_W), rows)
    n_freq = DA_HEAD_DIM // 4
    inv = ROPE_BASE ** (-jnp.arange(n_freq, dtype=jnp.float32) / n_freq)
    ang = jnp.stack([row, col], axis=-1).astype(jnp.float32)[:, :, None] * inv
    return jnp.cos(ang), jnp.sin(ang)


def apply_rope(x, cos, sin):
    B, L, H, C, d = x.shape
    xr = x.reshape(B, L, H, C, 2, 2, d // 4).astype(jnp.float32)
    x1, x2 = xr[..., 0, :], xr[..., 1, :]
    cs, sn = cos[:, None, None], sin[:, None, None]
    out = jnp.stack([x1 * cs - x2 * sn, x2 * cs + x1 * sn], axis=-2)
    return out.reshape(B, L, H, C, d).astype(x.dtype)


def diff_attention(q, k, v, lam, subln_g, lambda_init):
    B, Lq, H, _, d = q.shape
    nb = Lq // Q_BLOCK
    scale = d ** -0.5
    qb = jnp.moveaxis(q.reshape(B, nb, Q_BLOCK, H, 2, d), 1, 0)

    def one_block(qi):
        s = jnp.einsum('bqhcd,bkhcd->bhcqk', qi, k, preferred_element_type=jnp.float32) * scale
        p = jax.nn.softmax(s, axis=-1)
        pd = p[:, :, 0] - lam * p[:, :, 1]
        return jnp.einsum('bhqk,bkhe->bqhe', pd.astype(v.dtype), v)

    o = lax.map(one_block, qb)
    o = jnp.moveaxis(o, 0, 1).reshape(B, Lq, H, 2 * d)
    o = rms_norm(o, subln_g) * (1.0 - lambda_init)
    return o.reshape(B, Lq, H * 2 * d)


def conformer_conv(u, dw, dw_b, ln_g, ln_b, proj):
    a, g = jnp.split(u, 2, axis=-1)
    h = a * jax.nn.sigmoid(g)
    h = lax.conv_general_dilated(h, dw[:, None, :].astype(h.dtype), window_strides=(1,),
                                 padding=[(CV_KERNEL // 2, CV_KERNEL // 2)],
                                 dimension_numbers=('NWC', 'WIO', 'NWC'),
                                 feature_group_count=CV_WIDTH) + dw_b
    h = jax.nn.silu(layer_norm(h, ln_g, ln_b))
    return h @ proj


def gla_direction(q, k, v, log_a, s0):
    B, L, H, dk = q.shape
    dv = v.shape[-1]
    n = L // GLA_CHUNK
    f32 = jnp.float32
    qc = q.reshape(B, n, GLA_CHUNK, H, dk).astype(f32)
    kc = k.reshape(B, n, GLA_CHUNK, H, dk).astype(f32)
    vc = v.reshape(B, n, GLA_CHUNK, H, dv).astype(f32)
    b = jnp.cumsum(log_a.reshape(B, n, GLA_CHUNK, H, dk).astype(f32), axis=2)
    b_last = b[:, :, -1:]
    q_dec = qc * jnp.exp(b)
    k_inv = kc * jnp.exp(-b)
    scores = jnp.einsum('bnihd,bnjhd->bnhij', q_dec, k_inv)
    lower = jnp.tril(jnp.ones((GLA_CHUNK, GLA_CHUNK), dtype=bool))
    scores = jnp.where(lower, scores, 0.0)
    o_intra = jnp.einsum('bnhij,bnjhe->bnihe', scores, vc)
    k_tail = kc * jnp.exp(b_last - b)
    ds = jnp.einsum('bnjhd,bnjhe->nbhde', k_tail, vc)
    decay = jnp.moveaxis(jnp.exp(b_last[:, :, 0]), 1, 0)

    def step(s, inp):
        dcy, d_s = inp
        return dcy[..., None] * s + d_s, s

    s_final, s_in = lax.scan(step, s0.astype(f32), (decay, ds))
    o_inter = jnp.einsum('bnihd,nbhde->bnihe', q_dec, s_in)
    return (o_intra + o_inter).reshape(B, L, H, dv), s_final


def gla_inputs(gq, gk, gv, ga, a_up, a_b):
    B, L, _ = gq.shape
    q = gq.reshape(B, L, GLA_HEADS, GLA_DK) * (GLA_DK ** -0.5)
    k = gk.reshape(B, L, GLA_HEADS, GLA_DK)
    v = gv.reshape(B, L, GLA_HEADS, GLA_DV)
    la_f = jax.nn.log_sigmoid((ga[..., :GLA_RANK] @ a_up[0] + a_b[0]).astype(jnp.float32)) / GLA_NORMALIZER
    la_b = jax.nn.log_sigmoid((ga[..., GLA_RANK:] @ a_up[1] + a_b[1]).astype(jnp.float32)) / GLA_NORMALIZER
    return q, k, v, la_f.reshape(B, L, GLA_HEADS, GLA_DK), la_b.reshape(B, L, GLA_HEADS, GLA_DK)


def gla_bidirectional(q, k, v, la_f, la_b, s0_f, s0_b):
    o_f, s_f = gla_direction(q, k, v, la_f, s0_f)
    o_b, s_b = gla_direction(jnp.flip(q, 1), jnp.flip(k, 1), jnp.flip(v, 1), jnp.flip(la_b, 1), s0_b)
    return o_f + jnp.flip(o_b, 1), s_f, s_b


def gla_readout(o, gg, norm_g, proj, dtype):
    B, L = o.shape[:2]
    o = rms_norm(o, norm_g) * jax.nn.silu(gg.reshape(B, L, GLA_HEADS, GLA_DV).astype(jnp.float32))
    return o.astype(dtype).reshape(B, L, GLA_VW) @ proj


def gated_merge(gates, ya, yb, yc, w_out):
    g = jax.nn.sigmoid(gates.reshape(*gates.shape[:-1], N_BRANCH, D_MODEL))
    return (g[..., 0, :] * ya + g[..., 1, :] * yb + g[..., 2, :] * yc) @ w_out


def token_mixer(h, hc, lambda_init, need_ctx, rope_cos, rope_sin, w_in, da_lam, da_subln, da_proj,
                cv_dw, cv_dw_b, cv_ln_g, cv_ln_b, cv_proj, gla_a_up, gla_a_b, gla_norm, gla_proj, w_out):
    B, L, _ = h.shape
    Lc = hc.shape[1]
    aq, ak, av, cin, gq, gk, gv, gg, ga, gates = split_in(h @ w_in)
    aqc, akc, avc, cinc, gqc, gkc, gvc, ggc, gac, gatesc = split_in(hc @ w_in)

    lp = da_lam.astype(jnp.float32)
    lam = jnp.exp(jnp.sum(lp[0] * lp[1])) - jnp.exp(jnp.sum(lp[2] * lp[3])) + lambda_init
    q = apply_rope(aq.reshape(B, L, DA_HEADS, 2, DA_HEAD_DIM), rope_cos, rope_sin)
    k = apply_rope(ak.reshape(B, L, DA_HEADS, 2, DA_HEAD_DIM), rope_cos, rope_sin)
    v = av.reshape(B, L, DA_HEADS, 2 * DA_HEAD_DIM)
    kc = akc.reshape(B, Lc, DA_HEADS, 2, DA_HEAD_DIM)
    vc = avc.reshape(B, Lc, DA_HEADS, 2 * DA_HEAD_DIM)
    k_all = jnp.concatenate([k, kc], axis=1)
    v_all = jnp.concatenate([v, vc], axis=1)
    y_a = diff_attention(q, k_all, v_all, lam, da_subln, lambda_init) @ da_proj

    y_b = conformer_conv(cin, cv_dw, cv_dw_b, cv_ln_g, cv_ln_b, cv_proj)

    zero_s = jnp.zeros((B, GLA_HEADS, GLA_DK, GLA_DV), jnp.float32)
    qgc, kgc, vgc, lafc, labc = gla_inputs(gqc, gkc, gvc, gac, gla_a_up, gla_a_b)
    o_gc, s_ctx_f, s_ctx_b = gla_bidirectional(qgc, kgc, vgc, lafc, labc, zero_s, zero_s)
    qg, kg, vg, laf, lab = gla_inputs(gq, gk, gv, ga, gla_a_up, gla_a_b)
    o_g, _, _ = gla_bidirectional(qg, kg, vg, laf, lab, s_ctx_f, s_ctx_b)
    y_c = gla_readout(o_g, gg, gla_norm, gla_proj, h.dtype)

    y = gated_merge(gates, y_a, y_b, y_c, w_out)
    if not need_ctx:
        return y, None
    qc = aqc.reshape(B, Lc, DA_HEADS, 2, DA_HEAD_DIM)
    y_ac = diff_attention(qc, kc, vc, lam, da_subln, lambda_init) @ da_proj
    y_bc = conformer_conv(cinc, cv_dw, cv_dw_b, cv_ln_g, cv_ln_b, cv_proj)
    y_cc = gla_readout(o_gc, ggc, gla_norm, gla_proj, hc.dtype)
    yc = gated_merge(gatesc, y_ac, y_bc, y_cc, w_out)
    return y, yc


def swiglu(x, w1, w3, w2):
    return (jax.nn.silu(x @ w1) * (x @ w3)) @ w2


def moe_swiglu(x, router, w1, w3, w2):
    logits = (x @ router).astype(jnp.float32)
    top_v, top_i = lax.top_k(logits, TOP_K)
    wts = jax.nn.softmax(top_v, axis=-1)
    gate = jnp.sum(jax.nn.one_hot(top_i, N_EXPERTS, dtype=jnp.float32) * wts[..., None], axis=-2)
    out = jnp.zeros(x.shape[:-1] + (w2.shape[-1],), x.dtype)
    for e in range(N_EXPERTS):
        out = out + gate[..., e:e + 1].astype(x.dtype) * swiglu(x, w1[e], w3[e], w2[e])
    return out


def channel_mixer(h, layer, ffn_w1, ffn_w3, ffn_w2, moe_router, moe_w1, moe_w3, moe_w2):
    i = layer // 2
    if layer % 2 == 0:
        return swiglu(h, ffn_w1[i], ffn_w3[i], ffn_w2[i])
    return moe_swiglu(h, moe_router[i], moe_w1[i], moe_w3[i], moe_w2[i])


def setup_inputs(seed: int = 0) -> dict:
    key = jax.random.key(seed)
    ks = iter(jax.random.split(key, 40))
    f32 = jnp.float32

    def nrm(shape, scale):
        return jax.random.normal(next(ks), shape, f32) * scale

    def gain(shape):
        return 1.0 + nrm(shape, 0.05)

    D = D_MODEL
    return {
        'x': nrm((BATCH, SEQ, D), 1.0),
        'c': nrm((BATCH, D), 1.0),
        'ctx': nrm((BATCH, CTX_LEN, D), 1.0),
        'c_ctx': nrm((D,), 1.0),
        'ada_w': nrm((DEPTH, D, 6 * D), 0.5 * D ** -0.5),
        'ada_b': nrm((DEPTH, 6 * D), 0.01),
        'g_mix_pre': gain((DEPTH, D)),
        'g_mix_post': gain((DEPTH, D)),
        'g_ffn_pre': gain((DEPTH, D)),
        'g_ffn_post': gain((DEPTH, D)),
        'w_in': nrm((DEPTH, D, IN_WIDTH), D ** -0.5),
        'da_lambda': nrm((DEPTH, 4, DA_HEAD_DIM), 0.1),
        'da_subln': gain((DEPTH, 2 * DA_HEAD_DIM)),
        'da_proj': nrm((DEPTH, DA_WIDTH, D), DA_WIDTH ** -0.5),
        'cv_dw': nrm((DEPTH, CV_KERNEL, CV_WIDTH), CV_KERNEL ** -0.5),
        'cv_dw_b': nrm((DEPTH, CV_WIDTH), 0.01),
        'cv_ln_g': gain((DEPTH, CV_WIDTH)),
        'cv_ln_b': nrm((DEPTH, CV_WIDTH), 0.01),
        'cv_proj': nrm((DEPTH, CV_WIDTH, D), CV_WIDTH ** -0.5),
        'gla_a_up': nrm((DEPTH, 2, GLA_RANK, GLA_KW), GLA_RANK ** -0.5),
        'gla_a_b': nrm((DEPTH, 2, GLA_KW), 0.1),
        'gla_norm': gain((DEPTH, GLA_DV)),
        'gla_proj': nrm((DEPTH, GLA_VW, D), GLA_VW ** -0.5),
        'w_out': nrm((DEPTH, D, D), D ** -0.5),
        'ffn_w1': nrm((N_DENSE, D, D_FF), D ** -0.5),
        'ffn_w3': nrm((N_DENSE, D, D_FF), D ** -0.5),
        'ffn_w2': nrm((N_DENSE, D_FF, D), D_FF ** -0.5),
        'moe_router': nrm((N_MOE, D, N_EXPERTS), D ** -0.5),
        'moe_w1': nrm((N_MOE, N_EXPERTS, D, EXPERT_FF), D ** -0.5),
        'moe_w3': nrm((N_MOE, N_EXPERTS, D, EXPERT_FF), D ** -0.5),
        'moe_w2': nrm((N_MOE, N_EXPERTS, EXPERT_FF, D), EXPERT_FF ** -0.5),
    }


def reference(x, c, ctx, c_ctx, ada_w, ada_b, g_mix_pre, g_mix_post, g_ffn_pre, g_ffn_post, w_in,
              da_lambda, da_subln, da_proj, cv_dw, cv_dw_b, cv_ln_g, cv_ln_b, cv_proj, gla_a_up, gla_a_b,
              gla_norm, gla_proj, w_out, ffn_w1, ffn_w3, ffn_w2, moe_router, moe_w1, moe_w3, moe_w2):
    L = x.shape[1]
    rope_cos, rope_sin = axial_rope(L)
    xc = ctx
    silu_c = jax.nn.silu(c)[:, None, :]
    silu_cc = jax.nn.silu(c_ctx)[None, None, :]
    for l in range(DEPTH):
        need_ctx = l < DEPTH - 1
        lambda_init = 0.8 - 0.6 * math.exp(-0.3 * l)
        mod = jnp.split(silu_c @ ada_w[l] + ada_b[l], 6, axis=-1)
        modc = jnp.split(silu_cc @ ada_w[l] + ada_b[l], 6, axis=-1)
        h = modulate(rms_norm(x, g_mix_pre[l]), mod[0], mod[1])
        hc = modulate(rms_norm(xc, g_mix_pre[l]), modc[0], modc[1])
        y, yc = token_mixer(h, hc, lambda_init, need_ctx, rope_cos, rope_sin, w_in[l], da_lambda[l],
                            da_subln[l], da_proj[l], cv_dw[l], cv_dw_b[l], cv_ln_g[l], cv_ln_b[l],
                            cv_proj[l], gla_a_up[l], gla_a_b[l], gla_norm[l], gla_proj[l], w_out[l])
        x = x + mod[2] * rms_norm(y, g_mix_post[l])
        h = modulate(rms_norm(x, g_ffn_pre[l]), mod[3], mod[4])
        f = channel_mixer(h, l, ffn_w1, ffn_w3, ffn_w2, moe_router, moe_w1, moe_w3, moe_w2)
        x = x + mod[5] * rms_norm(f, g_ffn_post[l])
        if need_ctx:
            xc = xc + modc[2] * rms_norm(yc, g_mix_post[l])
            hc = modulate(rms_norm(xc, g_ffn_pre[l]), modc[3], modc[4])
            fc = channel_mixer(hc, l, ffn_w1, ffn_w3, ffn_w2, moe_router, moe_w1, moe_w3, moe_w2)
            xc = xc + modc[5] * rms_norm(fc, g_ffn_post[l])
    return x
```

```python
import math
import os
from contextlib import ExitStack, contextmanager

import numpy as np
import concourse.bass as bass
import concourse.mybir as mybir
from concourse.bass_utils import run_bass_kernel_spmd

F32 = mybir.dt.float32
BF16 = mybir.dt.bfloat16
AF = mybir.ActivationFunctionType
ALU = mybir.AluOpType

NCORES = 8
D = 1024
SEQ = 2048
CTX = 256
NTOK = SEQ + CTX
NT = NTOK // 128
DEPTH = 2
DFF = 2816
NFC = DFF // 128
NEXP = 8
INW = 7200
OFF = dict(aq=0, ak=512, av=1024, cin=1536, gq=2560, gk=2816, gv=3072, gg=3584, ga=4096, gates=4128)
EPS = 1e-6


def _ovl(a, b):
    for (al, ah), (bl, bh) in zip(a, b):
        if al >= bh or bl >= ah:
            return False
    return True


def _inside(a, b):
    for (al, ah), (bl, bh) in zip(a, b):
        if al < bl or ah > bh:
            return False
    return True


class V:
    __slots__ = ("ap", "tt", "reg")

    def __init__(self, ap, tt, reg):
        self.ap, self.tt, self.reg = ap, tt, reg


class TT:
    def __init__(self, name, t, shape, local=True, psum=False):
        self.name, self.t, self.shape, self.local, self.psum = name, t, list(shape), local, psum
        self.w = {}
        self.r = {}

    def reg_of(self, idx):
        if not isinstance(idx, tuple):
            idx = (idx,)
        reg = []
        for d, n in enumerate(self.shape):
            if d < len(idx):
                i = idx[d]
                if isinstance(i, slice):
                    lo = 0 if i.start is None else i.start
                    hi = n if i.stop is None else i.stop
                else:
                    lo, hi = i, i + 1
            else:
                lo, hi = 0, n
            assert 0 <= lo < hi <= n, (self.name, idx, self.shape)
            reg.append((lo, hi))
        return tuple(reg)

    def __getitem__(self, idx):
        return V(self.t[idx], self, self.reg_of(idx))

    def all(self):
        return self[tuple(slice(None) for _ in self.shape)]

    def view(self, ap, idx):
        return V(ap, self, self.reg_of(idx))


class Prog:
    def __init__(self, nc, E):
        self.nc, self.E = nc, E
        self.eng = dict(pe=nc.tensor, act=nc.scalar, dve=nc.vector, pool=nc.gpsimd, sp=nc.sync)
        self.semh = {}
        self.cnt = {}
        for e in ("pe", "act", "dve", "pool"):
            self.semh[e] = E(nc.semaphore("s_" + e))
            self.cnt[e] = 0
        self.pe_pending = False
        self.waited = {e: {} for e in self.eng}
        self.dq = {}
        for q, n in (("sp", 20), ("pool", 12), ("act", 6)):
            self.dq[q] = [0, []]
            for i in range(n):
                sk = "d_%s%d" % (q, i)
                self.semh[sk] = E(nc.semaphore(sk))
                self.cnt[sk] = 0
                self.dq[q][1].append(sk)
        self.local_dma = {}
        self.bar_toks = []
        self.uid = 0
        self.nins = 0
        self.psb = [TT("ps%d" % i, E(nc.psum_tensor("ps%d" % i, [128, 512], F32)), [128, 512], local=False,
                       psum=True) for i in range(8)]
        self.psb16 = [t.t.bitcast(BF16) for t in self.psb]
        self.bank_rr = 0

    def sb(self, st, name, shape, dtype, local=True):
        self.uid += 1
        t = st.enter_context(self.nc.sbuf_tensor("%s_%d" % (name, self.uid), list(shape), dtype))
        return TT(name, t, shape, local=local)

    def bank(self):
        b = self.bank_rr
        self.bank_rr = (b + 1) % 8
        return b

    def pf(self, b, c0, c1, p0=0, p1=128):
        return self.psb[b][p0:p1, c0:c1]

    def pb(self, b, c0, c1, p0=0, p1=128):
        return V(self.psb16[b][p0:p1, c0:c1], self.psb[b], ((p0, p1), (c0 // 2, (c1 + 1) // 2)))

    def _deps(self, outs, ins):
        toks = []
        for v in ins:
            if v.tt.psum:
                toks.extend(v.tt.w.values())
                continue
            for reg, tok in v.tt.w.items():
                if _ovl(reg, v.reg):
                    toks.append(tok)
        for v in outs:
            if v.tt.psum:
                toks.extend(v.tt.w.values())
                continue
            for reg, tok in v.tt.w.items():
                if _ovl(reg, v.reg):
                    toks.append(tok)
            for reg, d in v.tt.r.items():
                if _ovl(reg, v.reg):
                    toks.extend(d.items())
        return toks

    def _wait(self, eng, toks):
        need = {}
        w = self.waited[eng]
        for sk, val in toks:
            if sk == "pe" and eng == "pe":
                continue
            if w.get(sk, 0) >= val:
                continue
            if need.get(sk, 0) < val:
                need[sk] = val
        for sk, val in need.items():
            self.eng[eng].wait_ge(self.semh[sk], val)
            w[sk] = val

    def _record(self, outs, ins, tok):
        sk, val = tok
        for v in list(ins) + list(outs):
            if v.tt.psum:
                v.tt.w = {((0, 128), (0, 512)): tok}
        for v in ins:
            if v.tt.psum:
                continue
            v.tt.r.setdefault(v.reg, {})[sk] = val
        for v in outs:
            tt = v.tt
            if tt.psum:
                continue
            for reg in [r for r in tt.w if _inside(r, v.reg)]:
                del tt.w[reg]
            for reg in [r for r in tt.r if _inside(r, v.reg)]:
                del tt.r[reg]
            tt.w[v.reg] = tok

    def op(self, eng, fn, outs, ins, signal=True):
        self._wait(eng, self._deps(outs, ins))
        inst = fn(self.eng[eng])
        self.nins += 1
        if eng == "pe" and not signal:
            tok = ("pe", self.cnt["pe"] + 1)
            self.pe_pending = True
        else:
            self.cnt[eng] += 1
            inst.then_inc(self.semh[eng], 1)
            tok = (eng, self.cnt[eng])
            if eng == "pe":
                self.pe_pending = False
        self._record(outs, ins, tok)

    def dma(self, q, out, in_, **kw):
        st = self.dq[q]
        sk = st[1][st[0]]
        st[0] = (st[0] + 1) % len(st[1])
        toks = self._deps([out], [in_])
        if self.cnt[sk]:
            toks.append((sk, self.cnt[sk]))
        local = out.tt.local or in_.tt.local
        if out.tt.local:
            toks.extend(self.bar_toks)
        self._wait(q, toks)
        inst = self.eng[q].dma_start(out=out.ap, in_=in_.ap, **kw)
        self.nins += 1
        self.cnt[sk] += 16
        inst.then_inc(self.semh[sk], 16)
        tok = (sk, self.cnt[sk])
        if local:
            self.local_dma[sk] = self.cnt[sk]
        self._record([out], [in_], tok)
        return tok

    def barrier(self):
        assert not self.pe_pending
        toks = [(e, self.cnt[e]) for e in ("pe", "act", "dve", "pool") if self.cnt[e]]
        toks += list(self.local_dma.items())
        for e in ("pe", "act", "dve", "pool"):
            self._wait(e, [t for t in toks if t[0] != e])
        self.bar_toks = [(e, self.cnt[e]) for e in ("pe", "act", "dve", "pool") if self.cnt[e]]
        self.local_dma = {}

    @contextmanager
    def phase(self, name=None):
        st = ExitStack()
        st.__enter__()
        if name and os.environ.get("K_SCOPES"):
            st.enter_context(self.nc.named_scope(name))
        try:
            yield st
        finally:
            self.barrier()
            st.__exit__(None, None, None)

    def finish(self, toks):
        self._wait("sp", toks)

    def mm(self, out, lhsT, rhs, start=True, stop=True):
        self.op("pe", lambda e: e.matmul(out.ap, lhsT=lhsT.ap, rhs=rhs.ap, start=start, stop=stop),
                [out], [lhsT, rhs], signal=stop)

    def tr(self, out, in_, ident, signal=True):
        self.op("pe", lambda e: e.transpose(out=out.ap, in_=in_.ap, identity=ident.ap), [out], [in_, ident],
                signal=signal)

    @staticmethod
    def _sc(x):
        return x.ap if isinstance(x, V) else x

    def act(self, out, in_, func, bias=None, scale=None, accum=None):
        kw = {}
        ins = [in_]
        outs = [out]
        if bias is not None:
            kw["bias"] = self._sc(bias)
            if isinstance(bias, V):
                ins.append(bias)
        if scale is not None:
            kw["scale"] = self._sc(scale)
            if isinstance(scale, V):
                ins.append(scale)
        if accum is not None:
            kw["accum_out"] = accum.ap
            outs.append(accum)
        self.op("act", lambda e: e.activation(out=out.ap, in_=in_.ap, func=func, **kw), outs, ins)

    def tt(self, eng, out, a, b, op):
        self.op(eng, lambda e: e.tensor_tensor(out=out.ap, in0=a.ap, in1=b.ap, op=op), [out], [a, b])

    def ts(self, eng, out, a, s1, op0, s2=None, op1=None, accum=None):
        ins = [a] + [s for s in (s1, s2) if isinstance(s, V)]
        outs = [out] + ([accum] if accum is not None else [])
        kw = {}
        if op1 is not None:
            kw["op1"] = op1
        if accum is not None:
            kw["accum_out"] = accum.ap
        self.op(eng, lambda e: e.tensor_scalar(out=out.ap, in0=a.ap, scalar1=self._sc(s1), scalar2=self._sc(s2),
                                               op0=op0, **kw), outs, ins)

    def stt(self, out, a, s, b, op0, op1, accum=None):
        ins = [a, b] + ([s] if isinstance(s, V) else [])
        outs = [out] + ([accum] if accum is not None else [])
        kw = {"accum_out": accum.ap} if accum is not None else {}
        self.op("dve", lambda e: e.scalar_tensor_tensor(out=out.ap, in0=a.ap, scalar=self._sc(s), in1=b.ap,
                                                        op0=op0, op1=op1, **kw), outs, ins)

    def copy(self, eng, out, in_):
        if eng == "act":
            self.op("act", lambda e: e.copy(out=out.ap, in_=in_.ap), [out], [in_])
        else:
            self.op(eng, lambda e: e.tensor_copy(out=out.ap, in_=in_.ap), [out], [in_])

    def recip(self, out, in_):
        self.op("dve", lambda e: e.reciprocal(out=out.ap, in_=in_.ap), [out], [in_])

    def memset(self, eng, out, val):
        self.op(eng, lambda e: e.memset(out.ap, val), [out], [])

    def scan(self, out, d0, d1, init, op0, op1):
        self.op("dve", lambda e: e.tensor_tensor_scan(out=out.ap, data0=d0.ap, data1=d1.ap, initial=init,
                                                      op0=op0, op1=op1), [out], [d0, d1])

    def rmax(self, out, in_):
        self.op("dve", lambda e: e.reduce_max(out=out.ap, in_=in_.ap, axis=mybir.AxisListType.X), [out], [in_])


def _host_consts():
    t = np.arange(SEQ)
    pos = np.stack([t // 64, t % 64], 0).astype(np.float32)
    inv = (10000.0 ** (-np.arange(16, dtype=np.float32) / 16)).astype(np.float32)
    p = np.arange(128)
    j = p % 64
    ang = pos[j // 32][:, :] * inv[j % 16][:, None]
    cosT = np.cos(ang).astype(np.float32)
    sinT = np.sin(ang).astype(np.float32)
    rot = np.zeros((128, 128), np.float32)
    for m in range(128):
        half = (m % 32) // 16
        if half == 0:
            rot[m + 16, m] = -1.0
        else:
            rot[m - 16, m] = 1.0
    ident = np.eye(128, dtype=np.float32)
    jj, ii = np.meshgrid(np.arange(128), np.arange(128), indexing="ij")
    mlow = (jj <= ii).astype(np.float32)
    mup = (jj >= ii).astype(np.float32)
    smask = np.ones((128, NTOK), np.float32)
    smask[:, ::128] = 0.0
    consts = np.concatenate([ident, rot, mlow, mup, np.ones((128, 128), np.float32)], 1)
    return dict(k_consts=consts, k_cos=cosT, k_sin=sinT, k_smask=smask)


WEIGHT_NAMES = ["ada_w", "ada_b", "g_mix_pre", "g_mix_post", "g_ffn_pre", "g_ffn_post", "w_in", "da_lambda",
                "da_subln", "da_proj", "cv_dw", "cv_dw_b", "cv_ln_g", "cv_ln_b", "cv_proj", "gla_a_up", "gla_a_b",
                "gla_norm", "gla_proj", "w_out", "ffn_w1", "ffn_w3", "ffn_w2", "moe_router", "moe_w1", "moe_w3",
                "moe_w2"]
WEIGHT_SHAPES = dict(ada_w=[2, 1024, 6144], ada_b=[2, 6144], g_mix_pre=[2, 1024], g_mix_post=[2, 1024],
                     g_ffn_pre=[2, 1024], g_ffn_post=[2, 1024], w_in=[2, 1024, 7200], da_lambda=[2, 4, 64],
                     da_subln=[2, 128], da_proj=[2, 512, 1024], cv_dw=[2, 31, 512], cv_dw_b=[2, 512],
                     cv_ln_g=[2, 512], cv_ln_b=[2, 512], cv_proj=[2, 512, 1024], gla_a_up=[2, 2, 16, 256],
                     gla_a_b=[2, 2, 256], gla_norm=[2, 128], gla_proj=[2, 512, 1024], w_out=[2, 1024, 1024],
                     ffn_w1=[1, 1024, 2816], ffn_w3=[1, 1024, 2816], ffn_w2=[1, 2816, 1024],
                     moe_router=[1, 1024, 8], moe_w1=[1, 8, 1024, 2816], moe_w3=[1, 8, 1024, 2816],
                     moe_w2=[1, 8, 2816, 1024])


def _finish(nc, P, E_, tap_out):
    assert not P.pe_pending
    toks = [(sk, P.cnt[sk]) for q in P.dq.values() for sk in q[1] if P.cnt[sk]]
    toks += [(e, P.cnt[e]) for e in ("pe", "act", "dve", "pool") if P.cnt[e]]
    P._wait("sp", toks)
    E_.close()
    return nc, list(tap_out.keys())


PV_DWB, PV_LNG, PV_LNB, PV_AB, PV_GN, PV_DW = 0, 4, 8, 12, 16, 17
PV_N = 17 + 124
TG512 = [(0, 512), (512, 512), (1024, 512), (1536, 512), (2048, 256)]


class _Stop(Exception):
    pass


class Rot:
    def __init__(self, items):
        self.items, self.i = list(items), 0

    def __call__(self):
        x = self.items[self.i]
        self.i = (self.i + 1) % len(self.items)
        return x


def _build(ctxd, NB, taps, stop, layers):
    nc = bass.Bass("TRN2", target_bir_lowering=False)
    E_ = ExitStack()
    E = E_.enter_context
    P = Prog(nc, E)
    tap_out = {}
    ctxd.update(nc=nc, P=P, E_=E_, tap_out=tap_out)

    def dram(name, shape, kind, dtype=F32):
        t = nc.dram_tensor(name, list(shape), dtype, kind=kind)
        return TT(name, t.ap(), shape, local=False)

    d_x = dram("x", [NB, SEQ, D], "ExternalInput")
    d_ctx = dram("ctx", [NB, CTX, D], "ExternalInput")
    d_cT = dram("cT", [128, 8, 5], "ExternalInput")
    d_pv = dram("pvec", [2, 128, PV_N], "ExternalInput")
    d_k = {k: dram(k, list(v.shape), "ExternalInput") for k, v in _host_consts().items()}
    W = {k: dram(k, WEIGHT_SHAPES[k], "ExternalInput") for k in WEIGHT_NAMES if k not in
         ("cv_dw", "cv_dw_b", "cv_ln_g", "cv_ln_b", "gla_a_b", "gla_norm")
         and not (k.startswith("moe_") and 1 not in layers)}
    d_out = dram("out", [NB, SEQ, D], "ExternalOutput")
    d_xs = dram("xs", [NTOK, D], "Internal")
    d_modd = dram("modd", [2, 5, 6144], "Internal")
    d_br = [dram("br%d" % i, [512, NTOK], "Internal", BF16) for i in range(3)]

    def tap(name, v, dtype=F32):
        if name not in taps:
            return
        shape = [hi - lo for lo, hi in v.reg]
        t = dram("tap_" + name, shape, "ExternalOutput", dtype)
        tap_out[name] = t
        P.dma("sp", t.all(), v)

    def dump_br(i):
        t = dram("tap_br%d" % i, [512, NTOK], "ExternalOutput", BF16)
        tap_out["br%d" % i] = t
        P.dma("sp", t.all(), d_br[i].all())

    def dump_xs():
        t = dram("tap_xs", [NTOK, D], "ExternalOutput")
        tap_out["xs"] = t
        P.dma("sp", t.all(), d_xs.all())

    kc_f = P.sb(E_, "kconst", [128, 640], F32, local=False)
    P.dma("sp", kc_f.all(), d_k["k_consts"].all())
    kc_b = P.sb(E_, "kconstb", [128, 640], BF16, local=False)
    P.copy("dve", kc_b.all(), kc_f.all())
    identb, rotb, onesb = kc_b[:, 0:128], kc_b[:, 128:256], kc_b[:, 512:640]
    identf = kc_f[:, 0:128]
    maskf = [kc_f[:, 256:384], kc_f[:, 384:512]]
    od = P.sb(E_, "onesdiv", [128, 256], BF16, local=False)
    P.memset("dve", od[:, 0:128], 1.0 / 512)
    P.memset("dve", od[:, 128:256], 1.0 / 128)
    od512, od128 = od[:, 0:128], od[:, 128:256]
    hT = P.sb(E_, "hT", [128, 8, NTOK], BF16, local=False)

    def bcast_load(st, name, dT, idx, n=1024):
        t = P.sb(st, name, [128, n], F32)
        ap = dT.t[idx].broadcast_to([128, n])
        P.dma("sp", t.all(), dT.view(ap, idx))
        return t

    def rsqrt(out, ss, scale, eps=EPS):
        P.act(out, ss, AF.Ln, bias=eps, scale=scale)
        P.act(out, out, AF.Exp, scale=-0.5)

    def wview(dT, pre, r0, nr, c0, ncol):
        idx = tuple(pre) + (slice(r0, r0 + nr), slice(c0, c0 + ncol))
        return dT.view(dT.t[idx].rearrange("(k p) n -> p k n", p=128), idx)

    with P.phase() as st:
        cT = P.sb(st, "cT", [128, 8, 5], F32)
        P.dma("sp", cT.all(), d_cT.all())
        sc = P.sb(st, "sc", [128, 8, 5], F32)
        P.act(sc.all(), cT.all(), AF.Silu)
        ones5 = P.sb(st, "ones5", [1, 8], F32)
        P.memset("dve", ones5.all(), 1.0)
        modsb = P.sb(st, "modsb", [5, 6144], F32)
        wsl = [P.sb(st, "adaw%d" % i, [128, 8, 512], F32) for i in range(2)]
        bsb = [P.sb(st, "adab%d" % i, [1, 6144], F32) for i in range(2)]
        rb = Rot([0, 1, 2, 3])
        for l in range(2):
            P.dma("sp", bsb[l].all(), W["ada_b"][l:l + 1, :])
            for g in range(12):
                slot = wsl[g % 2]
                P.dma("sp", slot.all(), wview(W["ada_w"], (l,), 0, 1024, g * 512, 512))
                b = rb()
                o = P.pf(b, 0, 512, 0, 5)
                for kc in range(8):
                    P.mm(o, sc[:, kc, :], slot[:, kc, :], start=(kc == 0), stop=False)
                P.mm(o, ones5[0:1, 0:5], bsb[l][0:1, g * 512:(g + 1) * 512], start=False, stop=True)
                P.copy("dve", modsb[:, g * 512:(g + 1) * 512], o)
            P.dma("sp", d_modd[l], modsb.all())
    if stop == "mod":
        tapall = dram("tap_modd", [2, 5, 6144], "ExternalOutput")
        tap_out["modd"] = tapall
        with P.phase() as st:
            t_ = P.sb(st, "t_", [10, 6144], F32)
            P.dma("sp", t_.all(), d_modd.view(d_modd.t[:, :, :].rearrange("a b c -> (a b) c"), (slice(None),) * 3))
            P.dma("sp", tapall.view(tapall.t[:, :, :].rearrange("a b c -> (a b) c"), (slice(None),) * 3), t_.all())
        raise _Stop()

    def modvec(st, name, l, row, j):
        return bcast_load(st, name, d_modd, (l, slice(row, row + 1), slice(j * 1024, (j + 1) * 1024)))

    def gvec(st, name, wname, l):
        return bcast_load(st, name, W[wname], (slice(l, l + 1), slice(None)))

    def affine_vecs(st, l, row, gname, jshift, jscale, tag, stmp=None):
        s = modvec(st, "s" + tag, l, row, jscale)
        Bv = modvec(st, "B" + tag, l, row, jshift)
        with P.phase() as stt_:
            g = gvec(stt_, "g" + tag, gname, l)
            P.stt(s.all(), s.all(), 1.0, g.all(), ALU.add, ALU.mult)
        return s, Bv

    def gate_vec(st, l, row, gname, j, tag, stmp=None):
        m = modvec(st, "gm" + tag, l, row, j)
        with P.phase() as stt_:
            g = gvec(stt_, "gg" + tag, gname, l)
            P.tt("dve", m.all(), m.all(), g.all(), ALU.mult)
        return m

    def src_tile(l, b, t):
        if l == 0:
            if t < 16:
                return d_x[b, t * 128:(t + 1) * 128, :]
            return d_ctx[b, (t - 16) * 128:(t - 15) * 128, :]
        return d_xs[t * 128:(t + 1) * 128, :]

    def norm_tiles(st, tiles, srcfn, vecs_for, dstT, col_of):
        xin = [P.sb(st, "xin%d" % i, [128, 1024], F32) for i in range(3)]
        junk = [P.sb(st, "junk%d" % i, [128, 1024], F32) for i in range(3)]
        hb = [P.sb(st, "hb%d" % i, [128, 1024], BF16) for i in range(2)]
        stats = P.sb(st, "nstats", [128, 2 * len(tiles)], F32)
        rb = Rot([0, 1, 2, 3])
        def stage_a(i, t):
            xt, jk = xin[i % 3], junk[i % 3]
            P.dma("sp", xt.all(), srcfn(t))
            ss, rs = stats[:, 2 * i:2 * i + 1], stats[:, 2 * i + 1:2 * i + 2]
            P.stt(jk.all(), xt.all(), 1.0, xt.all(), ALU.mult, ALU.mult, accum=ss)
            rsqrt(rs, ss, 1.0 / D)

        def stage_b(i, t):
            xt, jk, hbt = xin[i % 3], junk[i % 3], hb[i % 2]
            A, Bv = vecs_for(t)
            rs = stats[:, 2 * i + 1:2 * i + 2]
            P.stt(jk.all(), xt.all(), rs, A.all(), ALU.mult, ALU.mult)
            P.tt("pool", hbt.all(), jk.all(), Bv.all(), ALU.add)

        def stage_c(i, t):
            hbt = hb[i % 2]
            b = rb()
            for kc in range(8):
                P.tr(P.pb(b, kc * 128, (kc + 1) * 128), hbt[:, kc * 128:(kc + 1) * 128], identb, signal=(kc == 7))
            c0 = col_of(t)
            src = V(P.psb16[b][:, 0:1024].rearrange("p (k n) -> p k n", k=8), P.psb[b], ((0, 128), (0, 512)))
            P.copy("act", dstT[:, :, c0:c0 + 128], src)

        n_ = len(tiles)
        for i in range(n_ + 2):
            if i < n_:
                stage_a(i, tiles[i])
            if 0 <= i - 1 < n_:
                stage_b(i - 1, tiles[i - 1])
            if 0 <= i - 2 < n_:
                stage_c(i - 2, tiles[i - 2])

    def proj_fm(out, wslot, wc0, M, src, tok0, n, KC=8):
        for kc in range(KC):
            P.mm(out, wslot[:, kc, wc0:wc0 + M], src[:, kc, tok0:tok0 + n], start=(kc == 0), stop=(kc == KC - 1))

    out_tokens = []

    for b in range(NB):
        for l in layers:
            need_ctx = (l == 0)
            lam_init = 0.8 - 0.6 * math.exp(-0.3 * l)
            tiles_in = list(range(18))
            tiles_out = list(range(18)) if need_ctx else list(range(16))
            TGo = TG512 if need_ctx else TG512[:4]
            ntok_o = NTOK if need_ctx else SEQ

            with P.phase("p1_%d" % l) as st:
                A1, B1 = affine_vecs(st, l, b, "g_mix_pre", 0, 1, "l")
                A1c, B1c = affine_vecs(st, l, 4, "g_mix_pre", 0, 1, "c")
                norm_tiles(st, tiles_in, lambda t: src_tile(l, b, t),
                           lambda t: (A1, B1) if t < 16 else (A1c, B1c), hT, lambda t: t * 128)
            if b == 0 and l == 0:
                tap("hT", hT.all(), BF16)
            if stop == "p1":
                raise _Stop()

            pv = None
            with P.phase("brA_%d" % l) as st:
                pv = P.sb(st, "pv", [128, PV_N], F32)
                P.dma("sp", pv.all(), d_pv[l])
                cosb = P.sb(st, "cos", [128, SEQ], F32)
                sinb = P.sb(st, "sin", [128, SEQ], F32)
                P.dma("sp", cosb.all(), d_k["k_cos"].all())
                P.dma("sp", sinb.all(), d_k["k_sin"].all())
                sm = P.sb(st, "asm", [128, 16], F32)
                lamt = P.sb(st, "lamt", [128, 256], F32)
                lidx = (slice(l, l + 1), slice(None), slice(None))
                P.dma("sp", lamt.all(), W["da_lambda"].view(
                    W["da_lambda"].t[lidx].rearrange("o a b -> o (a b)").broadcast_to([128, 256]), lidx))
                jk256 = P.sb(st, "jk256", [128, 64], F32)
                P.stt(jk256.all(), lamt[:, 0:64], 1.0, lamt[:, 64:128], ALU.mult, ALU.mult, accum=sm[:, 0:1])
                P.stt(jk256.all(), lamt[:, 128:192], 1.0, lamt[:, 192:256], ALU.mult, ALU.mult, accum=sm[:, 1:2])
                P.act(sm[:, 2:4], sm[:, 0:2], AF.Exp)
                P.tt("dve", sm[:, 4:5], sm[:, 2:3], sm[:, 3:4], ALU.subtract)
                P.ts("dve", sm[:, 5:6], sm[:, 4:5], lam_init, ALU.add, -1.0, ALU.mult)
                neglam = sm[:, 5:6]
                gsub = bcast_load(st, "gsub", W["da_subln"], (slice(l, l + 1), slice(None)), 128)
                P.ts("dve", gsub.all(), gsub.all(), 1.0 - lam_init, ALU.mult)
                gcol = P.sb(st, "gsubc", [128, 1], F32)
                sidx_ = (l, slice(None))
                P.dma("sp", gcol.all(), W["da_subln"].view(W["da_subln"].t[l].rearrange("(p o) -> p o", o=1), sidx_))
                P.ts("dve", gcol.all(), gcol.all(), 1.0 - lam_init, ALU.mult)
                vaug = P.sb(st, "vaug", [128, NT, 512], BF16)
                wv = P.sb(st, "wAv", [128, 8, 512], BF16)
                P.dma("pool", wv.all(), wview(W["w_in"], (l,), 0, 1024, OFF["av"], 512))
                rb = Rot([0, 1, 2, 3])
                for t in range(NT):
                    bk = rb()
                    o = P.pf(bk, 0, 512)
                    for kc in range(8):
                        P.mm(o, hT[:, kc, t * 128:(t + 1) * 128], wv[:, kc, :], start=(kc == 0), stop=(kc == 7))
                    P.copy("act" if t % 2 else "dve", vaug[:, t, :], o)
                if stop == "brA_v":
                    raise _Stop()
                qT = P.sb(st, "qT", [128, NTOK], BF16)
                qz = [P.sb(st, "qz%d" % c, [128, NTOK], BF16) for c in range(2)]
                P.memset("pool", qz[0][64:128, :], 0.0)
                P.memset("pool", qz[1][0:64, :], 0.0)
                kT = P.sb(st, "kT", [128, NTOK], BF16)
                wqk = [P.sb(st, "wqk%d" % i, [128, 8, 256], BF16) for i in range(2)]
                xsb = [P.sb(st, "xsb%d" % i, [128, 512], BF16) for i in range(2)]
                t1 = [P.sb(st, "rt1_%d" % i, [128, 512], F32) for i in range(2)]
                t2 = [P.sb(st, "rt2_%d" % i, [128, 512], F32) for i in range(2)]
                Et = [P.sb(st, "Et%d" % i, [128, 512], BF16) for i in range(3)]
                rd = [P.sb(st, "ard%d" % i, [128, 512], F32) for i in range(2)]
                o1b = [P.sb(st, "ao1_%d" % i, [128, 512], F32) for i in range(2)]
                o2b = [P.sb(st, "ao2_%d" % i, [128, 512], F32) for i in range(2)]
                sqa = [P.sb(st, "asq%d" % i, [128, 512], BF16) for i in range(2)]
                rsa = [P.sb(st, "ars%d" % i, [128, 512], F32) for i in range(2)]
                ostage = [P.sb(st, "oast%d" % i, [128, NTOK], BF16) for i in range(2)]
                rproj = Rot([4, 5, 6, 7])
                rsc = Rot([4, 5, 6])
                cnt = [0]
                for h in range(4):
                    wq = wqk[h % 2]
                    P.dma("pool", wq[:, :, 0:128], wview(W["w_in"], (l,), 0, 1024, OFF["aq"] + h * 128, 128))
                    P.dma("pool", wq[:, :, 128:256], wview(W["w_in"], (l,), 0, 1024, OFF["ak"] + h * 128, 128))
                    for which, dst in ((0, qT), (1, kT)):
                        for (tok0, n) in TG512:
                            if which == 0 and tok0 >= SEQ and not need_ctx:
                                continue
                            bk = rproj()
                            o = P.pf(bk, 0, n)
                            proj_fm(o, wq, which * 128, 128, hT, tok0, n)
                            if tok0 >= SEQ or os.environ.get("NOROPE"):
                                P.copy("act", dst[:, tok0:tok0 + n], o)
                                continue
                            i2 = cnt[0] % 2
                            cnt[0] += 1
                            P.copy("act", xsb[i2][:, 0:n], o)
                            b2 = rproj()
                            o2 = P.pf(b2, 0, n)
                            P.mm(o2, rotb, xsb[i2][:, 0:n])
                            P.tt("dve", t1[i2][:, 0:n], o, cosb[:, tok0:tok0 + n], ALU.mult)
                            P.tt("dve", t2[i2][:, 0:n], o2, sinb[:, tok0:tok0 + n], ALU.mult)
                            P.tt("pool", dst[:, tok0:tok0 + n], t1[i2][:, 0:n], t2[i2][:, 0:n], ALU.add)
                    if b == 0 and l == 0 and h == 0:
                        tap("qT0", qT.all(), BF16)
                    if stop == "brA_qk":
                        tap("kT0", kT.all(), BF16)
                        raise _Stop()
                    nq_ = NTOK if need_ctx else SEQ
                    P.copy("pool", qz[0][0:64, 0:nq_], qT[0:64, 0:nq_])
                    P.copy("pool", qz[1][64:128, 0:nq_], qT[64:128, 0:nq_])
                    osg = ostage[h % 2]
                    qgroups = [(tok0, n, list(range(NT))) for (tok0, n) in TG512[:4]]
                    if need_ctx:
                        qgroups.append((SEQ, CTX, [16, 17]))
                    pend_norm = []
                    sbanks = [4, 5, 6]
                    gi_ = [0]
                    for (q0, nq, ktiles) in qgroups:
                        nk = len(ktiles)
                        seq = [(c, ki) for c in (0, 1) for ki in range(nk)]

                        def S(i, q0=q0, nq=nq, ktiles=ktiles, seq=seq):
                            c, ki = seq[i]
                            kt = ktiles[ki]
                            r0 = c * 64
                            s_ps = P.pf(sbanks[i % 3], 0, nq)
                            P.mm(s_ps, kT[:, kt * 128:(kt + 1) * 128], qz[c][:, q0:q0 + nq])
                            P.act(Et[i % 3][:, 0:nq], s_ps, AF.Exp, scale=0.125)

                        S(0)
                        S(1)
                        while pend_norm:
                            pend_norm.pop(0)()
                        for i, (c, ki) in enumerate(seq):
                            et = Et[i % 3]
                            kt = ktiles[ki]
                            P.mm(P.pf(c, 0, nq), vaug[:, kt, h * 128:(h + 1) * 128], et[:, 0:nq],
                                 start=(ki == 0), stop=(ki == nk - 1))
                            P.mm(P.pf(2 + c, 0, nq), onesb, et[:, 0:nq], start=(ki == 0), stop=(ki == nk - 1))
                            if i + 2 < len(seq):
                                S(i + 2)

                        def norm(q0=q0, nq=nq):
                            g2 = gi_[0] % 2
                            gi_[0] += 1
                            P.recip(rd[0][:, 0:nq], P.pf(2, 0, nq))
                            P.tt("dve", o1b[g2][:, 0:nq], P.pf(0, 0, nq), rd[0][:, 0:nq], ALU.mult)
                            P.recip(rd[1][:, 0:nq], P.pf(3, 0, nq))
                            P.tt("dve", o2b[g2][:, 0:nq], P.pf(1, 0, nq), rd[1][:, 0:nq], ALU.mult)
                            P.stt(o1b[g2][:, 0:nq], o2b[g2][:, 0:nq], neglam, o1b[g2][:, 0:nq], ALU.mult, ALU.add)
                            P.tt("pool", sqa[g2][:, 0:nq], o1b[g2][:, 0:nq], o1b[g2][:, 0:nq], ALU.mult)
                            P.mm(P.pf(7, 0, nq), od128, sqa[g2][:, 0:nq])
                            rsqrt(rsa[g2][:, 0:nq], P.pf(7, 0, nq), 1.0)
                            P.stt(osg[:, q0:q0 + nq], o1b[g2][:, 0:nq], gcol[:, 0:1], rsa[g2][:, 0:nq], ALU.mult, ALU.mult)
                        pend_norm.append(norm)
                    while pend_norm:
                        pend_norm.pop(0)()
                        if stop == "brA_g1":
                            tap("osg", osg[:, 0:512], BF16)
                            raise _Stop()
                    P.dma("sp", d_br[0][h * 128:(h + 1) * 128, 0:ntok_o], osg[:, 0:ntok_o])
            if stop == "brA":
                dump_br(0)
                raise _Stop()

            with P.phase("brB_%d" % l) as st:
                pv = P.sb(st, "pv", [128, PV_N], F32)
                P.dma("sp", pv.all(), d_pv[l])
                PADW = 2364
                cb = P.sb(st, "cb", [128, 4, PADW], BF16)
                P.memset("pool", cb.all(), 0.0)
                wB = P.sb(st, "wB", [128, 8, 1024], BF16)
                P.dma("pool", wB.all(), wview(W["w_in"], (l,), 0, 1024, OFF["cin"], 1024))
                Dm = P.sb(st, "Dm", [128, 4, 31, 128], BF16)
                for j4 in range(4):
                    for k in range(31):
                        c_ = PV_DW + j4 * 31 + k
                        P.ts("pool" if k % 2 else "dve", Dm[:, j4, k, :], identf, pv[:, c_:c_ + 1], ALU.mult)
                sig = [P.sb(st, "sig%d" % i, [128, 512], F32) for i in range(2)]
                pairs = Rot([(0, 1), (2, 3), (4, 5), (6, 7)])
                k_ = 0
                for j4 in range(4):
                    for (tok0, n) in TGo:
                        ba, bg = pairs()
                        proj_fm(P.pf(ba, 0, n), wB, j4 * 128, 128, hT, tok0, n)
                        proj_fm(P.pf(bg, 0, n), wB, 512 + j4 * 128, 128, hT, tok0, n)
                        sg_ = sig[k_ % 2]
                        k_ += 1
                        P.act(sg_[:, 0:n], P.pf(bg, 0, n), AF.Sigmoid)
                        base = 15 + tok0 if tok0 < SEQ else 2078 + 15
                        P.tt("dve", cb[:, j4, base:base + n], P.pf(ba, 0, n), sg_[:, 0:n], ALU.mult)
                cvf = P.sb(st, "cvf", [128, 4, 512], F32)
                cvb = P.sb(st, "cvb", [128, 4, 512], BF16)
                sq = P.sb(st, "cvsq", [128, 4, 512], BF16)
                mean_s = P.sb(st, "cvmean", [128, 512], F32)
                m2 = P.sb(st, "cvm2", [128, 512], F32)
                var = P.sb(st, "cvvar", [128, 512], F32)
                rstd = P.sb(st, "cvrstd", [128, 512], F32)
                xc_ = [P.sb(st, "cvxc%d" % i, [128, 512], F32) for i in range(2)]
                obst = [P.sb(st, "obst%d" % i, [128, 4, 512], BF16) for i in range(2)]
                rconv = Rot([0, 1, 2, 3])
                rstat = Rot([(4, 5), (6, 7)])
                for gi, (tok0, n) in enumerate(TGo):
                    base = tok0 if tok0 < SEQ else 2078
                    for j4 in range(4):
                        bk = rconv()
                        o = P.pf(bk, 0, n)
                        for k in range(31):
                            P.mm(o, Dm[:, j4, k, :], cb[:, j4, base + k:base + k + n], start=(k == 0), stop=(k == 30))
                        P.act(cvf[:, j4, 0:n], o, AF.Identity, bias=pv[:, PV_DWB + j4:PV_DWB + j4 + 1])
                        P.copy("pool", cvb[:, j4, 0:n], cvf[:, j4, 0:n])
                        P.tt("pool", sq[:, j4, 0:n], cvf[:, j4, 0:n], cvf[:, j4, 0:n], ALU.mult)
                    bm, bq = rstat()
                    for j4 in range(4):
                        P.mm(P.pf(bm, 0, n), od512, cvb[:, j4, 0:n], start=(j4 == 0), stop=(j4 == 3))
                    for j4 in range(4):
                        P.mm(P.pf(bq, 0, n), od512, sq[:, j4, 0:n], start=(j4 == 0), stop=(j4 == 3))
                    P.copy("act", mean_s[:, 0:n], P.pf(bm, 0, n))
                    P.tt("pool", m2[:, 0:n], mean_s[:, 0:n], mean_s[:, 0:n], ALU.mult)
                    P.tt("dve", var[:, 0:n], P.pf(bq, 0, n), m2[:, 0:n], ALU.subtract)
                    rsqrt(rstd[:, 0:n], var[:, 0:n], 1.0)
                    ost = obst[gi % 2]
                    for j4 in range(4):
                        x_ = xc_[j4 % 2]
                        P.tt("dve", x_[:, 0:n], cvf[:, j4, 0:n], mean_s[:, 0:n], ALU.subtract)
                        P.tt("pool", x_[:, 0:n], x_[:, 0:n], rstd[:, 0:n], ALU.mult)
                        P.act(ost[:, j4, 0:n], x_[:, 0:n], AF.Silu, scale=pv[:, PV_LNG + j4:PV_LNG + j4 + 1],
                              bias=pv[:, PV_LNB + j4:PV_LNB + j4 + 1])
                    idx = (slice(None), slice(tok0, tok0 + n))
                    P.dma("sp", d_br[1].view(d_br[1].t[:, tok0:tok0 + n].rearrange("(j p) n -> p j n", p=128), idx),
                          ost[:, :, 0:n])
            if stop == "brB":
                dump_br(1)
                raise _Stop()

            with P.phase("brC_%d" % l) as st:
                pv = P.sb(st, "pv", [128, PV_N], F32)
                P.dma("sp", pv.all(), d_pv[l])
                nab = P.sb(st, "nab", [128, 4], F32)
                P.ts("dve", nab.all(), pv[:, PV_AB:PV_AB + 4], -1.0, ALU.mult)
                wv = P.sb(st, "wgv", [128, 8, 512], BF16)
                P.dma("pool", wv.all(), wview(W["w_in"], (l,), 0, 1024, OFF["gv"], 512))
                wgg = P.sb(st, "wgg", [128, 8, 512], BF16)
                P.dma("pool", wgg.all(), wview(W["w_in"], (l,), 0, 1024, OFF["gg"], 512))
                qd = [P.sb(st, "qd%d" % d, [128, 2, NTOK], BF16) for d in range(2)]
                kiT = [P.sb(st, "kiT%d" % d, [128, 2, NTOK], BF16) for d in range(2)]
                dec = P.sb(st, "dec", [128, 2, 2, NT], F32)
                full2 = (slice(None), slice(None))
                with P.phase() as st2:
                    smask = P.sb(st2, "smask", [128, NTOK], F32)
                    P.dma("sp", smask.all(), d_k["k_smask"].all())
                    aup = P.sb(st2, "aup", [16, 2, 256], BF16)
                    aidx = (l, slice(None), slice(None), slice(None))
                    P.dma("pool", aup.all(), W["gla_a_up"].view(W["gla_a_up"].t[l].rearrange("d r n -> r d n"), aidx))
                    wga = P.sb(st2, "wga", [128, 8, 32], BF16)
                    P.dma("pool", wga.all(), wview(W["w_in"], (l,), 0, 1024, OFF["ga"], 32))
                    wqk = P.sb(st2, "wgqk", [128, 8, 512], BF16)
                    P.dma("pool", wqk.all(), wview(W["w_in"], (l,), 0, 1024, OFF["gq"], 512))
                    gaT = [P.sb(st2, "gaT%d" % d, [16, NTOK], BF16) for d in range(2)]
                    rb = Rot(range(8))
                    for d in range(2):
                        for (tok0, n) in TG512:
                            bk = rb()
                            o = P.pf(bk, 0, n, 0, 16)
                            proj_fm(o, wga, d * 16, 16, hT, tok0, n)
                            P.copy("act", gaT[d][:, tok0:tok0 + n], o)
                    spt = P.sb(st2, "spt", [128, NTOK], F32)
                    cs = P.sb(st2, "cs", [128, NTOK], F32)
                    Eq = P.sb(st2, "Eq", [128, NTOK], F32)
                    Ek = P.sb(st2, "Ek", [128, NTOK], F32)
                    tmpe = [P.sb(st2, "tmpe%d" % i, [128, 512], F32) for i in range(2)]

                    def v3(tt_):
                        return V(tt_.t[:, :].rearrange("p (c i) -> p c i", i=128), tt_, tt_.reg_of(full2))
                    k_ = 0
                    for d in range(2):
                        for fc in range(2):
                            for (tok0, n) in TG512:
                                bk = rb()
                                o = P.pf(bk, 0, n)
                                P.mm(o, aup[0:16, d, fc * 128:(fc + 1) * 128], gaT[d][0:16, tok0:tok0 + n])
                                te = tmpe[k_ % 2]
                                k_ += 1
                                ci = d * 2 + fc
                                P.act(te[:, 0:n], o, AF.Exp, scale=-1.0, bias=nab[:, ci:ci + 1])
                                P.act(spt[:, tok0:tok0 + n], te[:, 0:n], AF.Ln, bias=1.0)
                            P.scan(cs.all(), smask.all(), spt.all(), 0.0, ALU.mult, ALU.add)
                            cs3 = v3(cs)
                            tot = V(cs3.ap[:, :, 127:128].broadcast_to([128, NT, 128]), cs, cs.reg_of(full2))
                            if d == 1:
                                P.tt("dve", v3(Eq), tot, cs3, ALU.subtract)
                                P.tt("pool", v3(Eq), v3(Eq), v3(spt), ALU.add)
                                Ssrc = Eq
                            else:
                                Ssrc = cs
                            P.act(dec[:, d, fc, :], V(cs3.ap[:, :, 127], cs, cs.reg_of(full2)), AF.Exp, scale=-1.0 / 16)
                            P.act(Ek.all(), Ssrc.all(), AF.Exp, scale=1.0 / 16)
                            P.act(Eq.all(), Ssrc.all(), AF.Exp, scale=-1.0 / 16, bias=math.log(0.125))
                            for (tok0, n) in TG512:
                                bq_ = rb()
                                proj_fm(P.pf(bq_, 0, n), wqk, fc * 128, 128, hT, tok0, n)
                                P.tt("dve", qd[d][:, fc, tok0:tok0 + n], P.pf(bq_, 0, n), Eq[:, tok0:tok0 + n], ALU.mult)
                                bk_ = rb()
                                proj_fm(P.pf(bk_, 0, n), wqk, 256 + fc * 128, 128, hT, tok0, n)
                                P.tt("dve", kiT[d][:, fc, tok0:tok0 + n], P.pf(bk_, 0, n), Ek[:, tok0:tok0 + n], ALU.mult)
                ki_tm = [P.sb(st, "kitm%d" % d, [128, NT, 256], BF16) for d in range(2)]
                v_tm = P.sb(st, "vtm", [128, NT, 512], BF16)
                for d in range(2):
                    for t in range(NT):
                        bk = rb()
                        for fc in range(2):
                            P.tr(P.pb(bk, fc * 128, (fc + 1) * 128), kiT[d][:, fc, t * 128:(t + 1) * 128], identb,
                                 signal=(fc == 1))
                        P.copy("act" if t % 2 else "dve", ki_tm[d][:, t, :], P.pb(bk, 0, 256))
                for t in range(NT):
                    bk = rb()
                    o = P.pf(bk, 0, 512)
                    for kc in range(8):
                        P.mm(o, hT[:, kc, t * 128:(t + 1) * 128], wv[:, kc, :], start=(kc == 0), stop=(kc == 7))
                    P.copy("act" if t % 2 else "dve", v_tm[:, t, :], o)
                Sst = [[P.sb(st, "Sst%d%d" % (d, fc), [128, NT, 256], BF16) for fc in range(2)] for d in range(2)]
                sfl = [[P.sb(st, "sfl%d%d" % (d, fc), [128, 256], F32) for fc in range(2)] for d in range(2)]
                tmpS = [[P.sb(st, "tmpS%d%d" % (d, fc), [128, 256], F32) for fc in range(2)] for d in range(2)]
                order = {0: [16, 17] + list(range(16)), 1: [17, 16] + list(range(15, -1, -1))}
                for d in range(2):
                    for fc in range(2):
                        P.memset("dve", sfl[d][fc].all(), 0.0)
                for i in range(NT):
                    for d in range(2):
                        for fc in range(2):
                            n_ = order[d][i]
                            s_ = sfl[d][fc]
                            P.copy("pool", Sst[d][fc][:, n_, :], s_.all())
                            if i == NT - 1:
                                continue
                            bk = rb()
                            o = P.pf(bk, 0, 256)
                            P.mm(o, ki_tm[d][:, n_, fc * 128:(fc + 1) * 128], v_tm[:, n_, fc * 256:(fc + 1) * 256])
                            P.tt("dve", tmpS[d][fc].all(), o, s_.all(), ALU.add)
                            P.act(s_.all(), tmpS[d][fc].all(), AF.Copy, scale=dec[:, d, fc, n_:n_ + 1])
                gnv = pv[:, PV_GN:PV_GN + 1]
                PT = [[P.sb(st, "PT%d%d" % (i, d), [128, 128], BF16) for d in range(2)] for i in range(2)]
                og = [P.sb(st, "og%d" % i, [128, 512], F32) for i in range(2)]
                sqb = [P.sb(st, "sqb%d" % i, [128, 512], BF16) for i in range(2)]
                rstg = [P.sb(st, "rstg%d" % i, [128, 512], F32) for i in range(2)]
                sgg = [P.sb(st, "sgg%d" % i, [128, 512], F32) for i in range(2)]
                t1g = [P.sb(st, "t1g%d" % i, [128, 512], F32) for i in range(2)]
                ocst = [P.sb(st, "ocst%d" % i, [128, 4, 512], BF16) for i in range(2)]
                rot_o = Rot([0, 1, 2, 3])
                rot_s = Rot([4, 5, 6])
                k_ = 0
                hcount = 0
                for gi, (tok0, n) in enumerate(TGo):
                    tl = list(range(tok0 // 128, (tok0 + n) // 128))
                    ost = ocst[gi % 2]
                    for h in range(4):
                        fc, hh = h // 2, h % 2
                        r0 = hh * 64
                        bo = rot_o()
                        for ti, t in enumerate(tl):
                            cols = slice(t * 128, (t + 1) * 128)
                            pts = []
                            for d in range(2):
                                bs = rot_s()
                                P.mm(P.pf(bs, 0, 128), kiT[d][r0:r0 + 64, fc, cols], qd[d][r0:r0 + 64, fc, cols])
                                pt = PT[k_ % 2][d]
                                P.tt("dve", pt.all(), P.pf(bs, 0, 128), maskf[d], ALU.mult)
                                pts.append(pt)
                            k_ += 1
                            o = P.pf(bo, ti * 128, (ti + 1) * 128)
                            P.mm(o, v_tm[:, t, h * 128:(h + 1) * 128], pts[0].all(), start=True, stop=False)
                            P.mm(o, Sst[0][fc][r0:r0 + 64, t, hh * 128:(hh + 1) * 128], qd[0][r0:r0 + 64, fc, cols],
                                 start=False, stop=False)
                            P.mm(o, v_tm[:, t, h * 128:(h + 1) * 128], pts[1].all(), start=False, stop=False)
                            P.mm(o, Sst[1][fc][r0:r0 + 64, t, hh * 128:(hh + 1) * 128], qd[1][r0:r0 + 64, fc, cols],
                                 start=False, stop=True)
                        i2 = hcount % 2
                        hcount += 1
                        P.copy("act", og[i2][:, 0:n], P.pf(bo, 0, n))
                        P.tt("pool", sqb[i2][:, 0:n], og[i2][:, 0:n], og[i2][:, 0:n], ALU.mult)
                        P.mm(P.pf(7, 0, n), od128, sqb[i2][:, 0:n])
                        rsqrt(rstg[i2][:, 0:n], P.pf(7, 0, n), 1.0)
                        bg = rot_o()
                        proj_fm(P.pf(bg, 0, n), wgg, h * 128, 128, hT, tok0, n)
                        P.act(sgg[i2][:, 0:n], P.pf(bg, 0, n), AF.Silu)
                        P.tt("dve", t1g[i2][:, 0:n], og[i2][:, 0:n], rstg[i2][:, 0:n], ALU.mult)
                        P.stt(ost[:, h, 0:n], t1g[i2][:, 0:n], gnv, sgg[i2][:, 0:n], ALU.mult, ALU.mult)
                    idx = (slice(None), slice(tok0, tok0 + n))
                    P.dma("sp", d_br[2].view(d_br[2].t[:, tok0:tok0 + n].rearrange("(j p) n -> p j n", p=128), idx),
                          ost[:, :, 0:n])
            if stop == "brC":
                dump_br(2)
                raise _Stop()

            with P.phase("merge_%d" % l) as st:
                mT = P.sb(st, "mT", [128, 8, NTOK], BF16)
                with P.phase() as st2:
                    oX = [P.sb(st2, "oX%d" % i, [128, 4, NTOK], BF16) for i in range(3)]
                    for i in range(3):
                        idx = (slice(None), slice(0, ntok_o))
                        P.dma("sp", oX[i][:, :, 0:ntok_o],
                              d_br[i].view(d_br[i].t[:, 0:ntok_o].rearrange("(j p) n -> p j n", p=128), idx))
                    wg = [P.sb(st2, "wg%d" % i, [128, 8, 384], BF16) for i in range(2)]
                    wp = [P.sb(st2, "wp%d" % i, [128, 4, 384], BF16) for i in range(2)]
                    sgm = [P.sb(st2, "sgm%d" % i, [128, 512], F32) for i in range(2)]
                    m_ = [P.sb(st2, "mm%d" % i, [128, 512], F32) for i in range(2)]
                    t_ = [P.sb(st2, "mt%d" % i, [128, 512], F32) for i in range(2)]
                    pairs = Rot([(0, 1), (2, 3), (4, 5), (6, 7)])
                    k_ = 0
                    g_i = 0
                    pnames = ("da_proj", "cv_proj", "gla_proj")
                    for fo in range(8):
                        g_, p_ = wg[fo % 2], wp[fo % 2]
                        for br in range(3):
                            P.dma("pool", g_[:, :, br * 128:(br + 1) * 128],
                                  wview(W["w_in"], (l,), 0, 1024, OFF["gates"] + br * 1024 + fo * 128, 128))
                            P.dma("pool", p_[:, :, br * 128:(br + 1) * 128],
                                  wview(W[pnames[br]], (l,), 0, 512, fo * 128, 128))
                        for (tok0, n) in TGo:
                            i2 = g_i % 2
                            g_i += 1
                            for br in range(3):
                                by, bg = pairs()
                                proj_fm(P.pf(by, 0, n), p_, br * 128, 128, oX[br], tok0, n, KC=4)
                                proj_fm(P.pf(bg, 0, n), g_, br * 128, 128, hT, tok0, n)
                                sg_ = sgm[k_ % 2]
                                tt_ = t_[k_ % 2]
                                k_ += 1
                                P.act(sg_[:, 0:n], P.pf(bg, 0, n), AF.Sigmoid)
                                if br == 0:
                                    P.tt("dve", m_[i2][:, 0:n], P.pf(by, 0, n), sg_[:, 0:n], ALU.mult)
                                elif br == 1:
                                    P.tt("dve", tt_[:, 0:n], P.pf(by, 0, n), sg_[:, 0:n], ALU.mult)
                                    P.tt("pool", m_[i2][:, 0:n], m_[i2][:, 0:n], tt_[:, 0:n], ALU.add)
                                else:
                                    P.tt("dve", tt_[:, 0:n], P.pf(by, 0, n), sg_[:, 0:n], ALU.mult)
                                    P.tt("pool", mT[:, fo, tok0:tok0 + n], m_[i2][:, 0:n], tt_[:, 0:n], ALU.add)
                if b == 0 and l == 0:
                    tap("mT", mT.all(), BF16)
                wout = P.sb(st, "wout", [128, 8, 1024], BF16)
                P.dma("pool", wout.all(), wview(W["w_out"], (l,), 0, 1024, 0, 1024))
                G1 = gate_vec(st, l, b, "g_mix_post", 2, "l")
                G1c = gate_vec(st, l, 4, "g_mix_post", 2, "c") if need_ctx else None
                xin = [P.sb(st, "rxin%d" % i, [128, 1024], F32) for i in range(3)]
                tm = [P.sb(st, "rtm%d" % i, [128, 1024], F32) for i in range(2)]
                xo = [P.sb(st, "rxo%d" % i, [128, 1024], F32) for i in range(2)]
                rst = P.sb(st, "rstat", [128, 4 * NT], F32)
                pairs = Rot([(0, 1), (2, 3), (4, 5), (6, 7)])
                for i, t in enumerate(tiles_out):
                    bks = pairs()
                    for hf in range(2):
                        o = P.pf(bks[hf], 0, 512)
                        for kc in range(8):
                            P.mm(o, mT[:, kc, t * 128:(t + 1) * 128], wout[:, kc, hf * 512:(hf + 1) * 512],
                                 start=(kc == 0), stop=(kc == 7))
                    xt, tmi, xoi = xin[i % 3], tm[i % 2], xo[i % 2]
                    P.dma("sp", xt.all(), src_tile(l, b, t))
                    ss0, ss1, ss, rs = (rst[:, 4 * i + q:4 * i + q + 1] for q in range(4))
                    P.act(tmi[:, 0:512], P.pf(bks[0], 0, 512), AF.Square, accum=ss0)
                    P.act(tmi[:, 512:1024], P.pf(bks[1], 0, 512), AF.Square, accum=ss1)
                    P.tt("dve", ss, ss0, ss1, ALU.add)
                    rsqrt(rs, ss, 1.0 / D)
                    G = G1 if t < 16 else G1c
                    for hf in range(2):
                        P.stt(tmi[:, hf * 512:(hf + 1) * 512], P.pf(bks[hf], 0, 512), rs, G[:, hf * 512:(hf + 1) * 512],
                              ALU.mult, ALU.mult)
                    P.tt("pool", xoi.all(), tmi.all(), xt.all(), ALU.add)
                    P.dma("sp", d_xs[t * 128:(t + 1) * 128, :], xoi.all())
            if stop == "mixer" and l == layers[-1]:
                dump_xs()
                raise _Stop()

            moe = (l == 1)
            nexp = NEXP if moe else 1
            if moe:
                stl = [list(range(0, 8)), list(range(8, 16))]
            else:
                stl = [list(range(0, 6)), list(range(6, 12)), list(range(12, 18))]
            for tl in stl:
                with P.phase("ffn_%d" % l) as st:
                    T = len(tl) * 128
                    A2, B2 = affine_vecs(st, l, b, "g_ffn_pre", 3, 4, "l")
                    G2 = gate_vec(st, l, b, "g_ffn_post", 5, "l")
                    if need_ctx:
                        A2c, B2c = affine_vecs(st, l, 4, "g_ffn_pre", 3, 4, "c")
                        G2c = gate_vec(st, l, 4, "g_ffn_post", 5, "c")
                    h2T = hT
                    with P.phase() as st2:
                        norm_tiles(st2, tl, lambda t: d_xs[t * 128:(t + 1) * 128, :],
                                   lambda t: (A2, B2) if t < 16 else (A2c, B2c), h2T, lambda t: (t - tl[0]) * 128)
                    acc = P.sb(st, "acc", [128, len(tl), 1024], F32)
                    if moe:
                        gates = P.sb(st, "gates", [128, len(tl), 8], F32)
                    st3 = ExitStack()
                    st3.__enter__()
                    aT = P.sb(st3, "aT", [128, NFC, T], BF16)
                    w13 = [P.sb(st3, "w13_%d" % i, [128, 8, 2, 256], BF16) for i in range(3)]
                    w2h = [P.sb(st3, "w2h%d" % i, [128, NFC, 512], BF16) for i in range(2)]
                    sl = [P.sb(st3, "sl%d" % i, [128, 512], F32) for i in range(2)]
                    rup = Rot([(0, 1), (2, 3)])
                    rdn = Rot([4, 5, 6, 7])
                    if moe:
                        rw = P.sb(st3, "rw", [128, 8, 8], BF16)
                        ridx = (0, slice(None), slice(None))
                        P.dma("pool", rw.all(), W["moe_router"].view(
                            W["moe_router"].t[0].rearrange("(k p) e -> p k e", p=128), ridx))
                        lg = P.sb(st3, "lg", [128, len(tl), 8], F32)
                        l2 = P.sb(st3, "l2", [128, len(tl), 8], F32)
                        eq1 = P.sb(st3, "eq1", [128, len(tl), 8], F32)
                        eq2 = P.sb(st3, "eq2", [128, len(tl), 8], F32)
                        gs = P.sb(st3, "gs", [128, len(tl), 8], F32)
                        for j in range(len(tl)):
                            bk = rdn()
                            o = P.pf(bk, 0, 8)
                            for kc in range(8):
                                P.mm(o, h2T[:, kc, j * 128:(j + 1) * 128], rw[:, kc, :], start=(kc == 0), stop=(kc == 7))
                            P.copy("dve", lg[:, j, :], o)
                            m1, m2_, dd, ee, den, w1, w2 = (gs[:, j, q:q + 1] for q in range(7))
                            P.rmax(m1, lg[:, j, :])
                            P.ts("dve", eq1[:, j, :], lg[:, j, :], m1, ALU.is_equal)
                            P.stt(l2[:, j, :], eq1[:, j, :], -1e30, lg[:, j, :], ALU.mult, ALU.add)
                            P.rmax(m2_, l2[:, j, :])
                            P.ts("dve", eq2[:, j, :], l2[:, j, :], m2_, ALU.is_equal)
                            P.tt("dve", dd, m2_, m1, ALU.subtract)
                            P.act(ee, dd, AF.Exp)
                            P.ts("dve", den, ee, 1.0, ALU.add)
                            P.recip(w1, den)
                            P.tt("dve", w2, ee, w1, ALU.mult)
                            P.ts("dve", gates[:, j, :], eq1[:, j, :], w1, ALU.mult)
                            P.stt(gates[:, j, :], eq2[:, j, :], w2, gates[:, j, :], ALU.mult, ALU.add)
                    subs = [(s0, min(512, T - s0)) for s0 in range(0, T, 512)]
                    k_ = 0
                    for e in range(nexp):
                        if moe:
                            w1d, w3d, w2d, pre = W["moe_w1"], W["moe_w3"], W["moe_w2"], (0, e)
                        else:
                            w1d, w3d, w2d, pre = W["ffn_w1"], W["ffn_w3"], W["ffn_w2"], (0,)

                        def load_w2(hf):
                            wh = w2h[hf]
                            idx = pre + (slice(None), slice(hf * 512, (hf + 1) * 512))
                            P.dma("pool", wh.all(), w2d.view(w2d.t[idx].rearrange("(f p) n -> p f n", p=128), idx))
                        for fp in range(NFC // 2):
                            ws = w13[(e * (NFC // 2) + fp) % 3]
                            P.dma("pool", ws[:, :, 0, :], wview(w1d, pre, 0, 1024, fp * 256, 256))
                            P.dma("pool", ws[:, :, 1, :], wview(w3d, pre, 0, 1024, fp * 256, 256))
                            if fp == 3:
                                load_w2(0)
                            if fp == 7:
                                load_w2(1)
                            for fi in range(2):
                                fc = fp * 2 + fi
                                for (s0, n) in subs:
                                    b1_, b3_ = rup()
                                    for kc in range(8):
                                        P.mm(P.pf(b1_, 0, n), ws[:, kc, 0, fi * 128:(fi + 1) * 128], h2T[:, kc, s0:s0 + n],
                                             start=(kc == 0), stop=(kc == 7))
                                    for kc in range(8):
                                        P.mm(P.pf(b3_, 0, n), ws[:, kc, 1, fi * 128:(fi + 1) * 128], h2T[:, kc, s0:s0 + n],
                                             start=(kc == 0), stop=(kc == 7))
                                    sl_ = sl[k_ % 2]
                                    k_ += 1
                                    P.act(sl_[:, 0:n], P.pf(b1_, 0, n), AF.Silu)
                                    P.tt("dve", aT[:, fc, s0:s0 + n], P.pf(b3_, 0, n), sl_[:, 0:n], ALU.mult)
                        for hf in range(2):
                            wh = w2h[hf]
                            for j in range(len(tl)):
                                bk = rdn()
                                o = P.pf(bk, 0, 512)
                                for fc in range(NFC):
                                    P.mm(o, aT[:, fc, j * 128:(j + 1) * 128], wh[:, fc, :], start=(fc == 0),
                                         stop=(fc == NFC - 1))
                                a_ = acc[:, j, hf * 512:(hf + 1) * 512]
                                if not moe:
                                    P.copy("act", a_, o)
                                elif e == 0:
                                    P.ts("dve", a_, o, gates[:, j, 0:1], ALU.mult)
                                else:
                                    P.stt(a_, o, gates[:, j, e:e + 1], a_, ALU.mult, ALU.add)
                    P.barrier()
                    st3.__exit__(None, None, None)
                    xin = [P.sb(st, "fxin%d" % i, [128, 1024], F32) for i in range(2)]
                    jk = [P.sb(st, "fjk%d" % i, [128, 1024], F32) for i in range(2)]
                    xo = [P.sb(st, "fxo%d" % i, [128, 1024], F32) for i in range(2)]
                    fst = P.sb(st, "fstat", [128, 2 * len(tl)], F32)
                    for j, t in enumerate(tl):
                        xt, jki, xoi = xin[j % 2], jk[j % 2], xo[j % 2]
                        P.dma("sp", xt.all(), d_xs[t * 128:(t + 1) * 128, :])
                        ss, rs = fst[:, 2 * j:2 * j + 1], fst[:, 2 * j + 1:2 * j + 2]
                        P.stt(jki.all(), acc[:, j, :], 1.0, acc[:, j, :], ALU.mult, ALU.mult, accum=ss)
                        rsqrt(rs, ss, 1.0 / D)
                        G = G2 if t < 16 else G2c
                        P.stt(jki.all(), acc[:, j, :], rs, G.all(), ALU.mult, ALU.mult)
                        P.tt("pool", xoi.all(), jki.all(), xt.all(), ALU.add)
                        if l == DEPTH - 1:
                            P.dma("sp", d_out[b, t * 128:(t + 1) * 128, :], xoi.all())
                        else:
                            P.dma("sp", d_xs[t * 128:(t + 1) * 128, :], xoi.all())
            if stop == "ffn" and l == layers[-1]:
                dump_xs()
                raise _Stop()


def make_in_maps(inp, NB, ncores, layers=(0, 1)):
    consts = _host_consts()
    pv = np.zeros((2, 128, PV_N), np.float32)
    for l in range(2):
        pv[l, :, PV_DWB:PV_DWB + 4] = inp["cv_dw_b"][l].reshape(4, 128).T
        pv[l, :, PV_LNG:PV_LNG + 4] = inp["cv_ln_g"][l].reshape(4, 128).T
        pv[l, :, PV_LNB:PV_LNB + 4] = inp["cv_ln_b"][l].reshape(4, 128).T
        pv[l, :, PV_AB:PV_AB + 4] = inp["gla_a_b"][l].reshape(4, 128).T
        pv[l, :, PV_GN] = inp["gla_norm"][l]
        pv[l, :, PV_DW:PV_DW + 124] = inp["cv_dw"][l].T.reshape(4, 128, 31).transpose(1, 0, 2).reshape(128, 124)
    maps = []
    for c in range(ncores):
        b0 = c * NB
        m = {"x": np.ascontiguousarray(inp["x"][b0:b0 + NB]), "ctx": np.ascontiguousarray(inp["ctx"][b0:b0 + NB])}
        cc = np.concatenate([inp["c"][b0:b0 + NB], np.zeros((4 - NB, D), np.float32), inp["c_ctx"][None]], 0)
        m["cT"] = np.ascontiguousarray(cc.reshape(5, 8, 128).transpose(2, 1, 0))
        m["pvec"] = pv
        m.update(consts)
        for k in WEIGHT_NAMES:
            if k not in ("cv_dw", "cv_dw_b", "cv_ln_g", "cv_ln_b", "gla_a_b", "gla_norm") \
                    and not (k.startswith("moe_") and 1 not in layers):
                m[k] = inp[k]
        maps.append(m)
    return maps


def build(NB=4, taps=(), stop=None, layers=(0, 1)):
    ctxd = {}
    try:
        _build(ctxd, NB, taps, stop, layers)
    except _Stop:
        pass
    return _finish(ctxd["nc"], ctxd["P"], ctxd["E_"], ctxd["tap_out"])


_CACHE = {}


def kernel(**inputs):
    inp = {k: np.ascontiguousarray(np.asarray(v)) for k, v in inputs.items()}
    NB = inp["x"].shape[0] // NCORES
    if "nc" not in _CACHE:
        _CACHE["nc"] = build(NB=NB)[0]
    maps = make_in_maps(inp, NB, NCORES)
    res = run_bass_kernel_spmd(_CACHE["nc"], maps, core_ids=list(range(NCORES)))
    out = np.concatenate([np.asarray(r["out"]) for r in res.results], axis=0)
    return out.astype(np.float32, copy=False)
```

```python
import math
import os
from contextlib import ExitStack, contextmanager

import numpy as np
import concourse.bass as bass
import concourse.mybir as mybir
from concourse.bass_utils import run_bass_kernel_spmd

F32 = mybir.dt.float32
BF16 = mybir.dt.bfloat16
AF = mybir.ActivationFunctionType
ALU = mybir.AluOpType

NCORES = 8
D = 1024
SEQ = 2048
CTX = 256
NTOK = SEQ + CTX
NT = NTOK // 128
DEPTH = 2
DFF = 2816
NFC = DFF // 128
NEXP = 8
INW = 7200
OFF = dict(aq=0, ak=512, av=1024, cin=1536, gq=2560, gk=2816, gv=3072, gg=3584, ga=4096, gates=4128)
EPS = 1e-6


def _ovl(a, b):
    for (al, ah), (bl, bh) in zip(a, b):
        if al >= bh or bl >= ah:
            return False
    return True


def _inside(a, b):
    for (al, ah), (bl, bh) in zip(a, b):
        if al < bl or ah > bh:
            return False
    return True


class V:
    __slots__ = ("ap", "tt", "reg")

    def __init__(self, ap, tt, reg):
        self.ap, self.tt, self.reg = ap, tt, reg


class TT:
    def __init__(self, name, t, shape, local=True, psum=False):
        self.name, self.t, self.shape, self.local, self.psum = name, t, list(shape), local, psum
        self.w = {}
        self.r = {}

    def reg_of(self, idx):
        if not isinstance(idx, tuple):
            idx = (idx,)
        reg = []
        for d, n in enumerate(self.shape):
            if d < len(idx):
                i = idx[d]
                if isinstance(i, slice):
                    lo = 0 if i.start is None else i.start
                    hi = n if i.stop is None else i.stop
                else:
                    lo, hi = i, i + 1
            else:
                lo, hi = 0, n
            assert 0 <= lo < hi <= n, (self.name, idx, self.shape)
            reg.append((lo, hi))
        return tuple(reg)

    def __getitem__(self, idx):
        return V(self.t[idx], self, self.reg_of(idx))

    def all(self):
        return self[tuple(slice(None) for _ in self.shape)]

    def view(self, ap, idx):
        return V(ap, self, self.reg_of(idx))


class Prog:
    def __init__(self, nc, E):
        self.nc, self.E = nc, E
        self.eng = dict(pe=nc.tensor, act=nc.scalar, dve=nc.vector, pool=nc.gpsimd, sp=nc.sync)
        self.semh = {}
        self.cnt = {}
        for e in ("pe", "act", "dve", "pool"):
            self.semh[e] = E(nc.semaphore("s_" + e))
            self.cnt[e] = 0
        self.pe_pending = False
        self.waited = {e: {} for e in self.eng}
        self.dq = {}
        for q, n in (("sp", 20), ("pool", 12), ("act", 6)):
            self.dq[q] = [0, []]
            for i in range(n):
                sk = "d_%s%d" % (q, i)
                self.semh[sk] = E(nc.semaphore(sk))
                self.cnt[sk] = 0
                self.dq[q][1].append(sk)
        self.local_dma = {}
        self.bar_toks = []
        self.uid = 0
        self.nins = 0
        self.psb = [TT("ps%d" % i, E(nc.psum_tensor("ps%d" % i, [128, 512], F32)), [128, 512], local=False,
                       psum=True) for i in range(8)]
        self.psb16 = [t.t.bitcast(BF16) for t in self.psb]
        self.bank_rr = 0

    def sb(self, st, name, shape, dtype, local=True):
        self.uid += 1
        t = st.enter_context(self.nc.sbuf_tensor("%s_%d" % (name, self.uid), list(shape), dtype))
        return TT(name, t, shape, local=local)

    def bank(self):
        b = self.bank_rr
        self.bank_rr = (b + 1) % 8
        return b

    def pf(self, b, c0, c1, p0=0, p1=128):
        return self.psb[b][p0:p1, c0:c1]

    def pb(self, b, c0, c1, p0=0, p1=128):
        return V(self.psb16[b][p0:p1, c0:c1], self.psb[b], ((p0, p1), (c0 // 2, (c1 + 1) // 2)))

    def _deps(self, outs, ins):
        toks = []
        for v in ins:
            if v.tt.psum:
                toks.extend(v.tt.w.values())
                continue
            for reg, tok in v.tt.w.items():
                if _ovl(reg, v.reg):
                    toks.append(tok)
        for v in outs:
            if v.tt.psum:
                toks.extend(v.tt.w.values())
                continue
            for reg, tok in v.tt.w.items():
                if _ovl(reg, v.reg):
                    toks.append(tok)
            for reg, d in v.tt.r.items():
                if _ovl(reg, v.reg):
                    toks.extend(d.items())
        return toks

    def _wait(self, eng, toks):
        need = {}
        w = self.waited[eng]
        for sk, val in toks:
            if sk == "pe" and eng == "pe":
                continue
            if w.get(sk, 0) >= val:
                continue
            if need.get(sk, 0) < val:
                need[sk] = val
        for sk, val in need.items():
            self.eng[eng].wait_ge(self.semh[sk], val)
            w[sk] = val

    def _record(self, outs, ins, tok):
        sk, val = tok
        for v in list(ins) + list(outs):
            if v.tt.psum:
                v.tt.w = {((0, 128), (0, 512)): tok}
        for v in ins:
            if v.tt.psum:
                continue
            v.tt.r.setdefault(v.reg, {})[sk] = val
        for v in outs:
            tt = v.tt
            if tt.psum:
                continue
            for reg in [r for r in tt.w if _inside(r, v.reg)]:
                del tt.w[reg]
            for reg in [r for r in tt.r if _inside(r, v.reg)]:
                del tt.r[reg]
            tt.w[v.reg] = tok

    def op(self, eng, fn, outs, ins, signal=True):
        self._wait(eng, self._deps(outs, ins))
        inst = fn(self.eng[eng])
        self.nins += 1
        if eng == "pe" and not signal:
            tok = ("pe", self.cnt["pe"] + 1)
            self.pe_pending = True
        else:
            self.cnt[eng] += 1
            inst.then_inc(self.semh[eng], 1)
            tok = (eng, self.cnt[eng])
            if eng == "pe":
                self.pe_pending = False
        self._record(outs, ins, tok)

    def dma(self, q, out, in_, **kw):
        st = self.dq[q]
        sk = st[1][st[0]]
        st[0] = (st[0] + 1) % len(st[1])
        toks = self._deps([out], [in_])
        if self.cnt[sk]:
            toks.append((sk, self.cnt[sk]))
        local = out.tt.local or in_.tt.local
        if out.tt.local:
            toks.extend(self.bar_toks)
        self._wait(q, toks)
        inst = self.eng[q].dma_start(out=out.ap, in_=in_.ap, **kw)
        self.nins += 1
        self.cnt[sk] += 16
        inst.then_inc(self.semh[sk], 16)
        tok = (sk, self.cnt[sk])
        if local:
            self.local_dma[sk] = self.cnt[sk]
        self._record([out], [in_], tok)
        return tok

    def barrier(self):
        assert not self.pe_pending
        toks = [(e, self.cnt[e]) for e in ("pe", "act", "dve", "pool") if self.cnt[e]]
        toks += list(self.local_dma.items())
        for e in ("pe", "act", "dve", "pool"):
            self._wait(e, [t for t in toks if t[0] != e])
        self.bar_toks = [(e, self.cnt[e]) for e in ("pe", "act", "dve", "pool") if self.cnt[e]]
        self.local_dma = {}

    @contextmanager
    def phase(self, name=None):
        st = ExitStack()
        st.__enter__()
        if name and os.environ.get("K_SCOPES"):
            st.enter_context(self.nc.named_scope(name))
        try:
            yield st
        finally:
            self.barrier()
            st.__exit__(None, None, None)

    def finish(self, toks):
        self._wait("sp", toks)

    def mm(self, out, lhsT, rhs, start=True, stop=True):
        self.op("pe", lambda e: e.matmul(out.ap, lhsT=lhsT.ap, rhs=rhs.ap, start=start, stop=stop),
                [out], [lhsT, rhs], signal=stop)

    def tr(self, out, in_, ident, signal=True):
        self.op("pe", lambda e: e.transpose(out=out.ap, in_=in_.ap, identity=ident.ap), [out], [in_, ident],
                signal=signal)

    @staticmethod
    def _sc(x):
        return x.ap if isinstance(x, V) else x

    def act(self, out, in_, func, bias=None, scale=None, accum=None):
        kw = {}
        ins = [in_]
        outs = [out]
        if bias is not None:
            kw["bias"] = self._sc(bias)
            if isinstance(bias, V):
                ins.append(bias)
        if scale is not None:
            kw["scale"] = self._sc(scale)
            if isinstance(scale, V):
                ins.append(scale)
        if accum is not None:
            kw["accum_out"] = accum.ap
            outs.append(accum)
        self.op("act", lambda e: e.activation(out=out.ap, in_=in_.ap, func=func, **kw), outs, ins)

    def tt(self, eng, out, a, b, op):
        self.op(eng, lambda e: e.tensor_tensor(out=out.ap, in0=a.ap, in1=b.ap, op=op), [out], [a, b])

    def ts(self, eng, out, a, s1, op0, s2=None, op1=None, accum=None):
        ins = [a] + [s for s in (s1, s2) if isinstance(s, V)]
        outs = [out] + ([accum] if accum is not None else [])
        kw = {}
        if op1 is not None:
            kw["op1"] = op1
        if accum is not None:
            kw["accum_out"] = accum.ap
        self.op(eng, lambda e: e.tensor_scalar(out=out.ap, in0=a.ap, scalar1=self._sc(s1), scalar2=self._sc(s2),
                                               op0=op0, **kw), outs, ins)

    def stt(self, out, a, s, b, op0, op1, accum=None):
        ins = [a, b] + ([s] if isinstance(s, V) else [])
        outs = [out] + ([accum] if accum is not None else [])
        kw = {"accum_out": accum.ap} if accum is not None else {}
        self.op("dve", lambda e: e.scalar_tensor_tensor(out=out.ap, in0=a.ap, scalar=self._sc(s), in1=b.ap,
                                                        op0=op0, op1=op1, **kw), outs, ins)

    def copy(self, eng, out, in_):
        if eng == "act":
            self.op("act", lambda e: e.copy(out=out.ap, in_=in_.ap), [out], [in_])
        else:
            self.op(eng, lambda e: e.tensor_copy(out=out.ap, in_=in_.ap), [out], [in_])

    def recip(self, out, in_):
        self.op("dve", lambda e: e.reciprocal(out=out.ap, in_=in_.ap), [out], [in_])

    def memset(self, eng, out, val):
        self.op(eng, lambda e: e.memset(out.ap, val), [out], [])

    def scan(self, out, d0, d1, init, op0, op1):
        self.op("dve", lambda e: e.tensor_tensor_scan(out=out.ap, data0=d0.ap, data1=d1.ap, initial=init,
                                                      op0=op0, op1=op1), [out], [d0, d1])

    def rmax(self, out, in_):
        self.op("dve", lambda e: e.reduce_max(out=out.ap, in_=in_.ap, axis=mybir.AxisListType.X), [out], [in_])


def _host_consts():
    t = np.arange(SEQ)
    pos = np.stack([t // 64, t % 64], 0).astype(np.float32)
    inv = (10000.0 ** (-np.arange(16, dtype=np.float32) / 16)).astype(np.float32)
    p = np.arange(128)
    j = p % 64
    ang = pos[j // 32][:, :] * inv[j % 16][:, None]
    cosT = np.cos(ang).astype(np.float32)
    sinT = np.sin(ang).astype(np.float32)
    rot = np.zeros((128, 128), np.float32)
    for m in range(128):
        half = (m % 32) // 16
        if half == 0:
            rot[m + 16, m] = -1.0
        else:
            rot[m - 16, m] = 1.0
    ident = np.eye(128, dtype=np.float32)
    jj, ii = np.meshgrid(np.arange(128), np.arange(128), indexing="ij")
    mlow = (jj <= ii).astype(np.float32)
    mup = (jj >= ii).astype(np.float32)
    smask = np.ones((128, NTOK), np.float32)
    smask[:, ::128] = 0.0
    consts = np.concatenate([ident, rot, mlow, mup, np.ones((128, 128), np.float32)], 1)
    return dict(k_consts=consts, k_cos=cosT, k_sin=sinT, k_smask=smask)


WEIGHT_NAMES = ["ada_w", "ada_b", "g_mix_pre", "g_mix_post", "g_ffn_pre", "g_ffn_post", "w_in", "da_lambda",
                "da_subln", "da_proj", "cv_dw", "cv_dw_b", "cv_ln_g", "cv_ln_b", "cv_proj", "gla_a_up", "gla_a_b",
                "gla_norm", "gla_proj", "w_out", "ffn_w1", "ffn_w3", "ffn_w2", "moe_router", "moe_w1", "moe_w3",
                "moe_w2"]
WEIGHT_SHAPES = dict(ada_w=[2, 1024, 6144], ada_b=[2, 6144], g_mix_pre=[2, 1024], g_mix_post=[2, 1024],
                     g_ffn_pre=[2, 1024], g_ffn_post=[2, 1024], w_in=[2, 1024, 7200], da_lambda=[2, 4, 64],
                     da_subln=[2, 128], da_proj=[2, 512, 1024], cv_dw=[2, 31, 512], cv_dw_b=[2, 512],
                     cv_ln_g=[2, 512], cv_ln_b=[2, 512], cv_proj=[2, 512, 1024], gla_a_up=[2, 2, 16, 256],
                     gla_a_b=[2, 2, 256], gla_norm=[2, 128], gla_proj=[2, 512, 1024], w_out=[2, 1024, 1024],
                     ffn_w1=[1, 1024, 2816], ffn_w3=[1, 1024, 2816], ffn_w2=[1, 2816, 1024],
                     moe_router=[1, 1024, 8], moe_w1=[1, 8, 1024, 2816], moe_w3=[1, 8, 1024, 2816],
                     moe_w2=[1, 8, 2816, 1024])


def _finish(nc, P, E_, tap_out):
    assert not P.pe_pending
    toks = [(sk, P.cnt[sk]) for q in P.dq.values() for sk in q[1] if P.cnt[sk]]
    toks += [(e, P.cnt[e]) for e in ("pe", "act", "dve", "pool") if P.cnt[e]]
    P._wait("sp", toks)
    E_.close()
    return nc, list(tap_out.keys())


PV_DWB, PV_LNG, PV_LNB, PV_AB, PV_GN, PV_DW = 0, 4, 8, 12, 16, 17
PV_N = 17 + 124
TG512 = [(0, 512), (512, 512), (1024, 512), (1536, 512), (2048, 256)]


class _Stop(Exception):
    pass


class Rot:
    def __init__(self, items):
        self.items, self.i = list(items), 0

    def __call__(self):
        x = self.items[self.i]
        self.i = (self.i + 1) % len(self.items)
        return x


def _build(ctxd, NB, taps, stop, layers):
    nc = bass.Bass("TRN2", target_bir_lowering=False)
    E_ = ExitStack()
    E = E_.enter_context
    P = Prog(nc, E)
    tap_out = {}
    ctxd.update(nc=nc, P=P, E_=E_, tap_out=tap_out)

    def dram(name, shape, kind, dtype=F32):
        t = nc.dram_tensor(name, list(shape), dtype, kind=kind)
        return TT(name, t.ap(), shape, local=False)

    d_x = dram("x", [NB, SEQ, D], "ExternalInput")
    d_ctx = dram("ctx", [NB, CTX, D], "ExternalInput")
    d_cT = dram("cT", [128, 8, 5], "ExternalInput")
    d_pv = dram("pvec", [2, 128, PV_N], "ExternalInput")
    d_k = {k: dram(k, list(v.shape), "ExternalInput") for k, v in _host_consts().items()}
    W = {k: dram(k, WEIGHT_SHAPES[k], "ExternalInput") for k in WEIGHT_NAMES if k not in
         ("cv_dw", "cv_dw_b", "cv_ln_g", "cv_ln_b", "gla_a_b", "gla_norm")
         and not (k.startswith("moe_") and 1 not in layers)}
    d_out = dram("out", [NB, SEQ, D], "ExternalOutput")
    d_xs = dram("xs", [NTOK, D], "Internal")
    d_modd = dram("modd", [2, 5, 6144], "Internal")
    d_br = [dram("br%d" % i, [512, NTOK], "Internal", BF16) for i in range(3)]

    def tap(name, v, dtype=F32):
        if name not in taps:
            return
        shape = [hi - lo for lo, hi in v.reg]
        t = dram("tap_" + name, shape, "ExternalOutput", dtype)
        tap_out[name] = t
        P.dma("sp", t.all(), v)

    def dump_br(i):
        t = dram("tap_br%d" % i, [512, NTOK], "ExternalOutput", BF16)
        tap_out["br%d" % i] = t
        P.dma("sp", t.all(), d_br[i].all())

    def dump_xs():
        t = dram("tap_xs", [NTOK, D], "ExternalOutput")
        tap_out["xs"] = t
        P.dma("sp", t.all(), d_xs.all())

    kc_f = P.sb(E_, "kconst", [128, 640], F32, local=False)
    P.dma("sp", kc_f.all(), d_k["k_consts"].all())
    kc_b = P.sb(E_, "kconstb", [128, 640], BF16, local=False)
    P.copy("dve", kc_b.all(), kc_f.all())
    identb, rotb, onesb = kc_b[:, 0:128], kc_b[:, 128:256], kc_b[:, 512:640]
    identf = kc_f[:, 0:128]
    maskf = [kc_f[:, 256:384], kc_f[:, 384:512]]
    od = P.sb(E_, "onesdiv", [128, 256], BF16, local=False)
    P.memset("dve", od[:, 0:128], 1.0 / 512)
    P.memset("dve", od[:, 128:256], 1.0 / 128)
    od512, od128 = od[:, 0:128], od[:, 128:256]
    hT = P.sb(E_, "hT", [128, 8, NTOK], BF16, local=False)

    def bcast_load(st, name, dT, idx, n=1024):
        t = P.sb(st, name, [128, n], F32)
        ap = dT.t[idx].broadcast_to([128, n])
        P.dma("sp", t.all(), dT.view(ap, idx))
        return t

    def rsqrt(out, ss, scale, eps=EPS):
        P.act(out, ss, AF.Ln, bias=eps, scale=scale)
        P.act(out, out, AF.Exp, scale=-0.5)

    def wview(dT, pre, r0, nr, c0, ncol):
        idx = tuple(pre) + (slice(r0, r0 + nr), slice(c0, c0 + ncol))
        return dT.view(dT.t[idx].rearrange("(k p) n -> p k n", p=128), idx)

    with P.phase() as st:
        cT = P.sb(st, "cT", [128, 8, 5], F32)
        P.dma("sp", cT.all(), d_cT.all())
        sc = P.sb(st, "sc", [128, 8, 5], F32)
        P.act(sc.all(), cT.all(), AF.Silu)
        ones5 = P.sb(st, "ones5", [1, 8], F32)
        P.memset("dve", ones5.all(), 1.0)
        modsb = P.sb(st, "modsb", [5, 6144], F32)
        wsl = [P.sb(st, "adaw%d" % i, [128, 8, 512], F32) for i in range(2)]
        bsb = [P.sb(st, "adab%d" % i, [1, 6144], F32) for i in range(2)]
        rb = Rot([0, 1, 2, 3])
        for l in range(2):
            P.dma("sp", bsb[l].all(), W["ada_b"][l:l + 1, :])
            for g in range(12):
                slot = wsl[g % 2]
                P.dma("sp", slot.all(), wview(W["ada_w"], (l,), 0, 1024, g * 512, 512))
                b = rb()
                o = P.pf(b, 0, 512, 0, 5)
                for kc in range(8):
                    P.mm(o, sc[:, kc, :], slot[:, kc, :], start=(kc == 0), stop=False)
                P.mm(o, ones5[0:1, 0:5], bsb[l][0:1, g * 512:(g + 1) * 512], start=False, stop=True)
                P.copy("dve", modsb[:, g * 512:(g + 1) * 512], o)
            P.dma("sp", d_modd[l], modsb.all())
    if stop == "mod":
        tapall = dram("tap_modd", [2, 5, 6144], "ExternalOutput")
        tap_out["modd"] = tapall
        with P.phase() as st:
            t_ = P.sb(st, "t_", [10, 6144], F32)
            P.dma("sp", t_.all(), d_modd.view(d_modd.t[:, :, :].rearrange("a b c -> (a b) c"), (slice(None),) * 3))
            P.dma("sp", tapall.view(tapall.t[:, :, :].rearrange("a b c -> (a b) c"), (slice(None),) * 3), t_.all())
        raise _Stop()

    def modvec(st, name, l, row, j):
        return bcast_load(st, name, d_modd, (l, slice(row, row + 1), slice(j * 1024, (j + 1) * 1024)))

    def gvec(st, name, wname, l):
        return bcast_load(st, name, W[wname], (slice(l, l + 1), slice(None)))

    def affine_vecs(st, l, row, gname, jshift, jscale, tag, stmp=None):
        s = modvec(st, "s" + tag, l, row, jscale)
        Bv = modvec(st, "B" + tag, l, row, jshift)
        with P.phase() as stt_:
            g = gvec(stt_, "g" + tag, gname, l)
            P.stt(s.all(), s.all(), 1.0, g.all(), ALU.add, ALU.mult)
        return s, Bv

    def gate_vec(st, l, row, gname, j, tag, stmp=None):
        m = modvec(st, "gm" + tag, l, row, j)
        with P.phase() as stt_:
            g = gvec(stt_, "gg" + tag, gname, l)
            P.tt("dve", m.all(), m.all(), g.all(), ALU.mult)
        return m

    def src_tile(l, b, t):
        if l == 0:
            if t < 16:
                return d_x[b, t * 128:(t + 1) * 128, :]
            return d_ctx[b, (t - 16) * 128:(t - 15) * 128, :]
        return d_xs[t * 128:(t + 1) * 128, :]

    def norm_tiles(st, tiles, srcfn, vecs_for, dstT, col_of):
        xin = [P.sb(st, "xin%d" % i, [128, 1024], F32) for i in range(3)]
        junk = [P.sb(st, "junk%d" % i, [128, 1024], F32) for i in range(3)]
        hb = [P.sb(st, "hb%d" % i, [128, 1024], BF16) for i in range(2)]
        stats = P.sb(st, "nstats", [128, 2 * len(tiles)], F32)
        rb = Rot([0, 1, 2, 3])
        def stage_a(i, t):
            xt, jk = xin[i % 3], junk[i % 3]
            P.dma("sp", xt.all(), srcfn(t))
            ss, rs = stats[:, 2 * i:2 * i + 1], stats[:, 2 * i + 1:2 * i + 2]
            P.stt(jk.all(), xt.all(), 1.0, xt.all(), ALU.mult, ALU.mult, accum=ss)
            rsqrt(rs, ss, 1.0 / D)

        def stage_b(i, t):
            xt, jk, hbt = xin[i % 3], junk[i % 3], hb[i % 2]
            A, Bv = vecs_for(t)
            rs = stats[:, 2 * i + 1:2 * i + 2]
            P.stt(jk.all(), xt.all(), rs, A.all(), ALU.mult, ALU.mult)
            P.tt("pool", hbt.all(), jk.all(), Bv.all(), ALU.add)

        def stage_c(i, t):
            hbt = hb[i % 2]
            b = rb()
            for kc in range(8):
                P.tr(P.pb(b, kc * 128, (kc + 1) * 128), hbt[:, kc * 128:(kc + 1) * 128], identb, signal=(kc == 7))
            c0 = col_of(t)
            src = V(P.psb16[b][:, 0:1024].rearrange("p (k n) -> p k n", k=8), P.psb[b], ((0, 128), (0, 512)))
            P.copy("act", dstT[:, :, c0:c0 + 128], src)

        n_ = len(tiles)
        for i in range(n_ + 2):
            if i < n_:
                stage_a(i, tiles[i])
            if 0 <= i - 1 < n_:
                stage_b(i - 1, tiles[i - 1])
            if 0 <= i - 2 < n_:
                stage_c(i - 2, tiles[i - 2])

    def proj_fm(out, wslot, wc0, M, src, tok0, n, KC=8):
        for kc in range(KC):
            P.mm(out, wslot[:, kc, wc0:wc0 + M], src[:, kc, tok0:tok0 + n], start=(kc == 0), stop=(kc == KC - 1))

    out_tokens = []

    for b in range(NB):
        for l in layers:
            need_ctx = (l == 0)
            lam_init = 0.8 - 0.6 * math.exp(-0.3 * l)
            tiles_in = list(range(18))
            tiles_out = list(range(18)) if need_ctx else list(range(16))
            TGo = TG512 if need_ctx else TG512[:4]
            ntok_o = NTOK if need_ctx else SEQ

            with P.phase("p1_%d" % l) as st:
                A1, B1 = affine_vecs(st, l, b, "g_mix_pre", 0, 1, "l")
                A1c, B1c = affine_vecs(st, l, 4, "g_mix_pre", 0, 1, "c")
                norm_tiles(st, tiles_in, lambda t: src_tile(l, b, t),
                           lambda t: (A1, B1) if t < 16 else (A1c, B1c), hT, lambda t: t * 128)
            if b == 0 and l == 0:
                tap("hT", hT.all(), BF16)
            if stop == "p1":
                raise _Stop()

            pv = None
            with P.phase("brA_%d" % l) as st:
                pv = P.sb(st, "pv", [128, PV_N], F32)
                P.dma("sp", pv.all(), d_pv[l])
                cosb = P.sb(st, "cos", [128, SEQ], F32)
                sinb = P.sb(st, "sin", [128, SEQ], F32)
                P.dma("sp", cosb.all(), d_k["k_cos"].all())
                P.dma("sp", sinb.all(), d_k["k_sin"].all())
                sm = P.sb(st, "asm", [128, 16], F32)
                lamt = P.sb(st, "lamt", [128, 256], F32)
                lidx = (slice(l, l + 1), slice(None), slice(None))
                P.dma("sp", lamt.all(), W["da_lambda"].view(
                    W["da_lambda"].t[lidx].rearrange("o a b -> o (a b)").broadcast_to([128, 256]), lidx))
                jk256 = P.sb(st, "jk256", [128, 64], F32)
                P.stt(jk256.all(), lamt[:, 0:64], 1.0, lamt[:, 64:128], ALU.mult, ALU.mult, accum=sm[:, 0:1])
                P.stt(jk256.all(), lamt[:, 128:192], 1.0, lamt[:, 192:256], ALU.mult, ALU.mult, accum=sm[:, 1:2])
                P.act(sm[:, 2:4], sm[:, 0:2], AF.Exp)
                P.tt("dve", sm[:, 4:5], sm[:, 2:3], sm[:, 3:4], ALU.subtract)
                P.ts("dve", sm[:, 5:6], sm[:, 4:5], lam_init, ALU.add, -1.0, ALU.mult)
                neglam = sm[:, 5:6]
                gsub = bcast_load(st, "gsub", W["da_subln"], (slice(l, l + 1), slice(None)), 128)
                P.ts("dve", gsub.all(), gsub.all(), 1.0 - lam_init, ALU.mult)
                gcol = P.sb(st, "gsubc", [128, 1], F32)
                sidx_ = (l, slice(None))
                P.dma("sp", gcol.all(), W["da_subln"].view(W["da_subln"].t[l].rearrange("(p o) -> p o", o=1), sidx_))
                P.ts("dve", gcol.all(), gcol.all(), 1.0 - lam_init, ALU.mult)
                vaug = P.sb(st, "vaug", [128, NT, 512], BF16)
                wv = P.sb(st, "wAv", [128, 8, 512], BF16)
                P.dma("pool", wv.all(), wview(W["w_in"], (l,), 0, 1024, OFF["av"], 512))
                rb = Rot([0, 1, 2, 3])
                for t in range(NT):
                    bk = rb()
                    o = P.pf(bk, 0, 512)
                    for kc in range(8):
                        P.mm(o, hT[:, kc, t * 128:(t + 1) * 128], wv[:, kc, :], start=(kc == 0), stop=(kc == 7))
                    P.copy("act" if t % 2 else "dve", vaug[:, t, :], o)
                if stop == "brA_v":
                    raise _Stop()
                qT = P.sb(st, "qT", [128, NTOK], BF16)
                qz = [P.sb(st, "qz%d" % c, [128, NTOK], BF16) for c in range(2)]
                P.memset("pool", qz[0][64:128, :], 0.0)
                P.memset("pool", qz[1][0:64, :], 0.0)
                kT = P.sb(st, "kT", [128, NTOK], BF16)
                wqk = [P.sb(st, "wqk%d" % i, [128, 8, 256], BF16) for i in range(2)]
                xsb = [P.sb(st, "xsb%d" % i, [128, 512], BF16) for i in range(2)]
                t1 = [P.sb(st, "rt1_%d" % i, [128, 512], F32) for i in range(2)]
                t2 = [P.sb(st, "rt2_%d" % i, [128, 512], F32) for i in range(2)]
                Et = [P.sb(st, "Et%d" % i, [128, 512], BF16) for i in range(3)]
                rd = [P.sb(st, "ard%d" % i, [128, 512], F32) for i in range(2)]
                o1b = [P.sb(st, "ao1_%d" % i, [128, 512], F32) for i in range(2)]
                o2b = [P.sb(st, "ao2_%d" % i, [128, 512], F32) for i in range(2)]
                sqa = [P.sb(st, "asq%d" % i, [128, 512], BF16) for i in range(2)]
                rsa = [P.sb(st, "ars%d" % i, [128, 512], F32) for i in range(2)]
                ostage = [P.sb(st, "oast%d" % i, [128, NTOK], BF16) for i in range(2)]
                rproj = Rot([4, 5, 6, 7])
                rsc = Rot([4, 5, 6])
                cnt = [0]
                for h in range(4):
                    wq = wqk[h % 2]
                    P.dma("pool", wq[:, :, 0:128], wview(W["w_in"], (l,), 0, 1024, OFF["aq"] + h * 128, 128))
                    P.dma("pool", wq[:, :, 128:256], wview(W["w_in"], (l,), 0, 1024, OFF["ak"] + h * 128, 128))
                    for which, dst in ((0, qT), (1, kT)):
                        for (tok0, n) in TG512:
                            if which == 0 and tok0 >= SEQ and not need_ctx:
                                continue
                            bk = rproj()
                            o = P.pf(bk, 0, n)
                            proj_fm(o, wq, which * 128, 128, hT, tok0, n)
                            if tok0 >= SEQ or os.environ.get("NOROPE"):
                                P.copy("act", dst[:, tok0:tok0 + n], o)
                                continue
                            i2 = cnt[0] % 2
                            cnt[0] += 1
                            P.copy("act", xsb[i2][:, 0:n], o)
                            b2 = rproj()
                            o2 = P.pf(b2, 0, n)
                            P.mm(o2, rotb, xsb[i2][:, 0:n])
                            P.tt("dve", t1[i2][:, 0:n], o, cosb[:, tok0:tok0 + n], ALU.mult)
                            P.tt("dve", t2[i2][:, 0:n], o2, sinb[:, tok0:tok0 + n], ALU.mult)
                            P.tt("pool", dst[:, tok0:tok0 + n], t1[i2][:, 0:n], t2[i2][:, 0:n], ALU.add)
                    if b == 0 and l == 0 and h == 0:
                        tap("qT0", qT.all(), BF16)
                    if stop == "brA_qk":
                        tap("kT0", kT.all(), BF16)
                        raise _Stop()
                    nq_ = NTOK if need_ctx else SEQ
                    P.copy("pool", qz[0][0:64, 0:nq_], qT[0:64, 0:nq_])
                    P.copy("pool", qz[1][64:128, 0:nq_], qT[64:128, 0:nq_])
                    osg = ostage[h % 2]
                    qgroups = [(tok0, n, list(range(NT))) for (tok0, n) in TG512[:4]]
                    if need_ctx:
                        qgroups.append((SEQ, CTX, [16, 17]))
                    pend_norm = []
                    sbanks = [4, 5, 6]
                    gi_ = [0]
                    for (q0, nq, ktiles) in qgroups:
                        nk = len(ktiles)
                        seq = [(c, ki) for c in (0, 1) for ki in range(nk)]

                        def S(i, q0=q0, nq=nq, ktiles=ktiles, seq=seq):
                            c, ki = seq[i]
                            kt = ktiles[ki]
                            r0 = c * 64
                            s_ps = P.pf(sbanks[i % 3], 0, nq)
                            P.mm(s_ps, kT[:, kt * 128:(kt + 1) * 128], qz[c][:, q0:q0 + nq])
                            P.act(Et[i % 3][:, 0:nq], s_ps, AF.Exp, scale=0.125)

                        S(0)
                        S(1)
                        while pend_norm:
                            pend_norm.pop(0)()
                        for i, (c, ki) in enumerate(seq):
                            et = Et[i % 3]
                            kt = ktiles[ki]
                            P.mm(P.pf(c, 0, nq), vaug[:, kt, h * 128:(h + 1) * 128], et[:, 0:nq],
                                 start=(ki == 0), stop=(ki == nk - 1))
                            P.mm(P.pf(2 + c, 0, nq), onesb, et[:, 0:nq], start=(ki == 0), stop=(ki == nk - 1))
                            if i + 2 < len(seq):
                                S(i + 2)

                        def norm(q0=q0, nq=nq):
                            g2 = gi_[0] % 2
                            gi_[0] += 1
                            P.recip(rd[0][:, 0:nq], P.pf(2, 0, nq))
                            P.tt("dve", o1b[g2][:, 0:nq], P.pf(0, 0, nq), rd[0][:, 0:nq], ALU.mult)
                            P.recip(rd[1][:, 0:nq], P.pf(3, 0, nq))
                            P.tt("dve", o2b[g2][:, 0:nq], P.pf(1, 0, nq), rd[1][:, 0:nq], ALU.mult)
                            P.stt(o1b[g2][:, 0:nq], o2b[g2][:, 0:nq], neglam, o1b[g2][:, 0:nq], ALU.mult, ALU.add)
                            P.tt("pool", sqa[g2][:, 0:nq], o1b[g2][:, 0:nq], o1b[g2][:, 0:nq], ALU.mult)
                            P.mm(P.pf(7, 0, nq), od128, sqa[g2][:, 0:nq])
                            rsqrt(rsa[g2][:, 0:nq], P.pf(7, 0, nq), 1.0)
                            P.stt(osg[:, q0:q0 + nq], o1b[g2][:, 0:nq], gcol[:, 0:1], rsa[g2][:, 0:nq], ALU.mult, ALU.mult)
                        pend_norm.append(norm)
                    while pend_norm:
                        pend_norm.pop(0)()
                        if stop == "brA_g1":
                            tap("osg", osg[:, 0:512], BF16)
                            raise _Stop()
                    P.dma("sp", d_br[0][h * 128:(h + 1) * 128, 0:ntok_o], osg[:, 0:ntok_o])
            if stop == "brA":
                dump_br(0)
                raise _Stop()

            with P.phase("brB_%d" % l) as st:
                pv = P.sb(st, "pv", [128, PV_N], F32)
                P.dma("sp", pv.all(), d_pv[l])
                PADW = 2364
                cb = P.sb(st, "cb", [128, 4, PADW], BF16)
                P.memset("dve", cb.all(), 0.0)
                wB = P.sb(st, "wB", [128, 8, 1024], BF16)
                P.dma("pool", wB.all(), wview(W["w_in"], (l,), 0, 1024, OFF["cin"], 1024))
                Dm = P.sb(st, "Dm", [128, 4, 31, 128], BF16)
                for j4 in range(4):
                    for k in range(31):
                        c_ = PV_DW + j4 * 31 + k
                        if k % 2:
                            P.act(Dm[:, j4, k, :], identf, AF.Copy, scale=pv[:, c_:c_ + 1])
                        else:
                            P.ts("dve", Dm[:, j4, k, :], identf, pv[:, c_:c_ + 1], ALU.mult)
                sig = [P.sb(st, "sig%d" % i, [128, 512], F32) for i in range(2)]
                pairs = Rot([(0, 1), (2, 3), (4, 5), (6, 7)])
                k_ = 0
                for j4 in range(4):
                    for (tok0, n) in TGo:
                        ba, bg = pairs()
                        proj_fm(P.pf(ba, 0, n), wB, j4 * 128, 128, hT, tok0, n)
                        proj_fm(P.pf(bg, 0, n), wB, 512 + j4 * 128, 128, hT, tok0, n)
                        sg_ = sig[k_ % 2]
                        k_ += 1
                        P.act(sg_[:, 0:n], P.pf(bg, 0, n), AF.Sigmoid)
                        base = 15 + tok0 if tok0 < SEQ else 2078 + 15
                        P.tt("dve", cb[:, j4, base:base + n], P.pf(ba, 0, n), sg_[:, 0:n], ALU.mult)
                cvf = P.sb(st, "cvf", [128, 4, 512], F32)
                cvb = P.sb(st, "cvb", [128, 4, 512], BF16)
                sq = P.sb(st, "cvsq", [128, 4, 512], BF16)
                mean_s = P.sb(st, "cvmean", [128, 512], F32)
                m2 = P.sb(st, "cvm2", [128, 512], F32)
                var = P.sb(st, "cvvar", [128, 512], F32)
                rstd = P.sb(st, "cvrstd", [128, 512], F32)
                xc_ = [P.sb(st, "cvxc%d" % i, [128, 512], F32) for i in range(2)]
                obst = [P.sb(st, "obst%d" % i, [128, 4, 512], BF16) for i in range(2)]
                rconv = Rot([0, 1, 2, 3])
                rstat = Rot([(4, 5), (6, 7)])
                for gi, (tok0, n) in enumerate(TGo):
                    base = tok0 if tok0 < SEQ else 2078
                    for j4 in range(4):
                        bk = rconv()
                        o = P.pf(bk, 0, n)
                        for k in range(31):
                            P.mm(o, Dm[:, j4, k, :], cb[:, j4, base + k:base + k + n], start=(k == 0), stop=(k == 30))
                        P.act(cvf[:, j4, 0:n], o, AF.Identity, bias=pv[:, PV_DWB + j4:PV_DWB + j4 + 1])
                        P.copy("dve", cvb[:, j4, 0:n], cvf[:, j4, 0:n])
                        P.tt("pool", sq[:, j4, 0:n], cvf[:, j4, 0:n], cvf[:, j4, 0:n], ALU.mult)
                    bm, bq = rstat()
                    for j4 in range(4):
                        P.mm(P.pf(bm, 0, n), od512, cvb[:, j4, 0:n], start=(j4 == 0), stop=(j4 == 3))
                    for j4 in range(4):
                        P.mm(P.pf(bq, 0, n), od512, sq[:, j4, 0:n], start=(j4 == 0), stop=(j4 == 3))
                    P.copy("act", mean_s[:, 0:n], P.pf(bm, 0, n))
                    P.tt("pool", m2[:, 0:n], mean_s[:, 0:n], mean_s[:, 0:n], ALU.mult)
                    P.tt("dve", var[:, 0:n], P.pf(bq, 0, n), m2[:, 0:n], ALU.subtract)
                    rsqrt(rstd[:, 0:n], var[:, 0:n], 1.0)
                    ost = obst[gi % 2]
                    for j4 in range(4):
                        x_ = xc_[j4 % 2]
                        P.tt("dve", x_[:, 0:n], cvf[:, j4, 0:n], mean_s[:, 0:n], ALU.subtract)
                        P.tt("pool", x_[:, 0:n], x_[:, 0:n], rstd[:, 0:n], ALU.mult)
                        P.act(ost[:, j4, 0:n], x_[:, 0:n], AF.Silu, scale=pv[:, PV_LNG + j4:PV_LNG + j4 + 1],
                              bias=pv[:, PV_LNB + j4:PV_LNB + j4 + 1])
                    idx = (slice(None), slice(tok0, tok0 + n))
                    P.dma("sp", d_br[1].view(d_br[1].t[:, tok0:tok0 + n].rearrange("(j p) n -> p j n", p=128), idx),
                          ost[:, :, 0:n])
            if stop == "brB":
                dump_br(1)
                raise _Stop()

            with P.phase("brC_%d" % l) as st:
                pv = P.sb(st, "pv", [128, PV_N], F32)
                P.dma("sp", pv.all(), d_pv[l])
                nab = P.sb(st, "nab", [128, 4], F32)
                P.ts("dve", nab.all(), pv[:, PV_AB:PV_AB + 4], -1.0, ALU.mult)
                wgg = P.sb(st, "wgg", [128, 8, 512], BF16)
                P.dma("pool", wgg.all(), wview(W["w_in"], (l,), 0, 1024, OFF["gg"], 512))
                qdz = [P.sb(st, "qdz%d" % d, [128, 2, NT, 256], BF16) for d in range(2)]
                for d in range(2):
                    P.memset("dve" if d else "pool", qdz[d].all(), 0.0)
                kiT = [P.sb(st, "kiT%d" % d, [128, 2, NTOK], BF16) for d in range(2)]
                dec = P.sb(st, "dec", [128, 2, 2, NT], F32)
                full2 = (slice(None), slice(None))
                ki_tm = [P.sb(st, "kitm%d" % d, [128, NT, 256], BF16) for d in range(2)]
                v_tm = P.sb(st, "vtm", [128, NT, 512], BF16)
                with P.phase() as stv:
                    wv = P.sb(stv, "wgv", [128, 8, 512], BF16)
                    P.dma("pool", wv.all(), wview(W["w_in"], (l,), 0, 1024, OFF["gv"], 512))
                    rbv = Rot(range(8))
                    for t in range(NT):
                        bk = rbv()
                        o = P.pf(bk, 0, 512)
                        for kc in range(8):
                            P.mm(o, hT[:, kc, t * 128:(t + 1) * 128], wv[:, kc, :], start=(kc == 0), stop=(kc == 7))
                        P.copy("act" if t % 2 else "dve", v_tm[:, t, :], o)
                with P.phase() as st2:
                    smask = P.sb(st2, "smask", [128, NTOK], F32)
                    P.dma("sp", smask.all(), d_k["k_smask"].all())
                    aup = P.sb(st2, "aup", [16, 2, 256], BF16)
                    aidx = (l, slice(None), slice(None), slice(None))
                    P.dma("pool", aup.all(), W["gla_a_up"].view(W["gla_a_up"].t[l].rearrange("d r n -> r d n"), aidx))
                    wga = P.sb(st2, "wga", [128, 8, 32], BF16)
                    P.dma("pool", wga.all(), wview(W["w_in"], (l,), 0, 1024, OFF["ga"], 32))
                    wqk = P.sb(st2, "wgqk", [128, 8, 512], BF16)
                    P.dma("pool", wqk.all(), wview(W["w_in"], (l,), 0, 1024, OFF["gq"], 512))
                    gaT = [P.sb(st2, "gaT%d" % d, [16, NTOK], BF16) for d in range(2)]
                    rb = Rot(range(8))
                    for d in range(2):
                        for (tok0, n) in TG512:
                            bk = rb()
                            o = P.pf(bk, 0, n, 0, 16)
                            proj_fm(o, wga, d * 16, 16, hT, tok0, n)
                            P.copy("act", gaT[d][:, tok0:tok0 + n], o)
                    spt = P.sb(st2, "spt", [128, NTOK], F32)
                    cs = P.sb(st2, "cs", [128, NTOK], F32)
                    Eq = P.sb(st2, "Eq", [128, NTOK], F32)
                    Ek = P.sb(st2, "Ek", [128, NTOK], F32)
                    tmpe = [P.sb(st2, "tmpe%d" % i, [128, 512], F32) for i in range(2)]

                    def v3(tt_):
                        return V(tt_.t[:, :].rearrange("p (c i) -> p c i", i=128), tt_, tt_.reg_of(full2))
                    k_ = 0
                    for d in range(2):
                        for fc in range(2):
                            for (tok0, n) in TG512:
                                bk = rb()
                                o = P.pf(bk, 0, n)
                                P.mm(o, aup[0:16, d, fc * 128:(fc + 1) * 128], gaT[d][0:16, tok0:tok0 + n])
                                te = tmpe[k_ % 2]
                                k_ += 1
                                ci = d * 2 + fc
                                P.act(te[:, 0:n], o, AF.Exp, scale=-1.0, bias=nab[:, ci:ci + 1])
                                P.act(spt[:, tok0:tok0 + n], te[:, 0:n], AF.Ln, bias=1.0)
                            P.scan(cs.all(), smask.all(), spt.all(), 0.0, ALU.mult, ALU.add)
                            cs3 = v3(cs)
                            tot = V(cs3.ap[:, :, 127:128].broadcast_to([128, NT, 128]), cs, cs.reg_of(full2))
                            if d == 1:
                                P.tt("dve", v3(Eq), tot, cs3, ALU.subtract)
                                P.tt("pool", v3(Eq), v3(Eq), v3(spt), ALU.add)
                                Ssrc = Eq
                            else:
                                Ssrc = cs
                            P.act(dec[:, d, fc, :], V(cs3.ap[:, :, 127], cs, cs.reg_of(full2)), AF.Exp, scale=-1.0 / 16)
                            P.act(Ek.all(), Ssrc.all(), AF.Exp, scale=1.0 / 16)
                            P.act(Eq.all(), Ssrc.all(), AF.Exp, scale=-1.0 / 16, bias=math.log(0.125))
                            for (tok0, n) in TG512:
                                bq_ = rb()
                                proj_fm(P.pf(bq_, 0, n), wqk, fc * 128, 128, hT, tok0, n)
                                t0_, nt_ = tok0 // 128, n // 128
                                for hh in range(2):
                                    r0, r1 = hh * 64, hh * 64 + 64
                                    ps3 = V(P.psb[bq_].t[r0:r1, 0:n].rearrange("p (t i) -> p t i", i=128), P.psb[bq_],
                                            ((r0, r1), (0, n)))
                                    eq3 = V(Eq.t[r0:r1, tok0:tok0 + n].rearrange("p (t i) -> p t i", i=128), Eq,
                                            ((r0, r1), (tok0, tok0 + n)))
                                    P.tt("dve", qdz[d][r0:r1, fc, t0_:t0_ + nt_, hh * 128:(hh + 1) * 128], ps3, eq3,
                                         ALU.mult)
                                bk_ = rb()
                                proj_fm(P.pf(bk_, 0, n), wqk, 256 + fc * 128, 128, hT, tok0, n)
                                P.tt("dve", kiT[d][:, fc, tok0:tok0 + n], P.pf(bk_, 0, n), Ek[:, tok0:tok0 + n], ALU.mult)
                for d in range(2):
                    for t in range(NT):
                        bk = rb()
                        for fc in range(2):
                            P.tr(P.pb(bk, fc * 128, (fc + 1) * 128), kiT[d][:, fc, t * 128:(t + 1) * 128], identb,
                                 signal=(fc == 1))
                        P.copy("act" if t % 2 else "dve", ki_tm[d][:, t, :], P.pb(bk, 0, 256))
                Sst = [[P.sb(st, "Sst%d%d" % (d, fc), [128, NT, 256], BF16) for fc in range(2)] for d in range(2)]
                sfl = [[P.sb(st, "sfl%d%d" % (d, fc), [128, 256], F32) for fc in range(2)] for d in range(2)]
                tmpS = [[P.sb(st, "tmpS%d%d" % (d, fc), [128, 256], F32) for fc in range(2)] for d in range(2)]
                order = {0: [16, 17] + list(range(16)), 1: [17, 16] + list(range(15, -1, -1))}
                for d in range(2):
                    for fc in range(2):
                        P.memset("dve", sfl[d][fc].all(), 0.0)
                for i in range(NT):
                    for d in range(2):
                        for fc in range(2):
                            n_ = order[d][i]
                            s_ = sfl[d][fc]
                            P.copy("pool", Sst[d][fc][:, n_, :], s_.all())
                            if i == NT - 1:
                                continue
                            bk = rb()
                            o = P.pf(bk, 0, 256)
                            P.mm(o, ki_tm[d][:, n_, fc * 128:(fc + 1) * 128], v_tm[:, n_, fc * 256:(fc + 1) * 256])
                            P.tt("dve", tmpS[d][fc].all(), o, s_.all(), ALU.add)
                            P.act(s_.all(), tmpS[d][fc].all(), AF.Copy, scale=dec[:, d, fc, n_:n_ + 1])
                gnv = pv[:, PV_GN:PV_GN + 1]
                PT = [[P.sb(st, "PT%d%d" % (i, d), [128, 256], BF16) for d in range(2)] for i in range(2)]
                og = [P.sb(st, "og%d" % i, [128, 512], F32) for i in range(2)]
                sqb = [P.sb(st, "sqb%d" % i, [128, 512], BF16) for i in range(2)]
                rstg = [P.sb(st, "rstg%d" % i, [128, 512], F32) for i in range(2)]
                sgg = [P.sb(st, "sgg%d" % i, [128, 512], F32) for i in range(2)]
                ocst = [P.sb(st, "ocst%d" % i, [128, 4, 512], BF16) for i in range(2)]
                mask3 = [V(kc_f.t[:, 256 + 128 * d:384 + 128 * d].rearrange("p (o i) -> p o i", o=1).broadcast_to(
                    [128, 2, 128]), kc_f, ((0, 128), (256 + 128 * d, 384 + 128 * d))) for d in range(2)]
                rot_o = Rot([(0, 1), (2, 3)])
                rot_s = Rot([4, 5, 6])
                k_ = 0
                hcount = 0
                for gi, (tok0, n) in enumerate(TGo):
                    tl = list(range(tok0 // 128, (tok0 + n) // 128))
                    ost = ocst[gi % 2]
                    for fc in range(2):
                        bo = rot_o()
                        for ti, t in enumerate(tl):
                            cols = slice(t * 128, (t + 1) * 128)
                            pts = []
                            for d in range(2):
                                bs = rot_s()
                                P.mm(P.pf(bs, 0, 256), kiT[d][:, fc, cols], qdz[d][:, fc, t, :])
                                pt = PT[k_ % 2][d]
                                ps3 = V(P.psb[bs].t[:, 0:256].rearrange("p (o i) -> p o i", o=2), P.psb[bs],
                                        ((0, 128), (0, 256)))
                                pt3 = V(pt.t[:, :].rearrange("p (o i) -> p o i", o=2), pt, ((0, 128), (0, 256)))
                                P.tt("dve", pt3, ps3, mask3[d], ALU.mult)
                                pts.append(pt)
                            k_ += 1
                            for hh in range(2):
                                h = 2 * fc + hh
                                hs = slice(hh * 128, (hh + 1) * 128)
                                o = P.pf(bo[hh], ti * 128, (ti + 1) * 128)
                                P.mm(o, v_tm[:, t, h * 128:(h + 1) * 128], pts[0][:, hs], start=True, stop=False)
                                P.mm(o, Sst[0][fc][:, t, hs], qdz[0][:, fc, t, hs], start=False, stop=False)
                                P.mm(o, v_tm[:, t, h * 128:(h + 1) * 128], pts[1][:, hs], start=False, stop=False)
                                P.mm(o, Sst[1][fc][:, t, hs], qdz[1][:, fc, t, hs], start=False, stop=True)
                        for hh in range(2):
                            h = 2 * fc + hh
                            i2 = hcount % 2
                            hcount += 1
                            P.copy("act", og[i2][:, 0:n], P.pf(bo[hh], 0, n))
                            P.tt("pool", sqb[i2][:, 0:n], og[i2][:, 0:n], og[i2][:, 0:n], ALU.mult)
                            P.mm(P.pf(7, 0, n), od128, sqb[i2][:, 0:n])
                            rsqrt(rstg[i2][:, 0:n], P.pf(7, 0, n), 1.0)
                            bg = rot_s()
                            proj_fm(P.pf(bg, 0, n), wgg, h * 128, 128, hT, tok0, n)
                            P.act(sgg[i2][:, 0:n], P.pf(bg, 0, n), AF.Silu)
                            P.tt("dve", og[i2][:, 0:n], og[i2][:, 0:n], rstg[i2][:, 0:n], ALU.mult)
                            P.stt(ost[:, h, 0:n], og[i2][:, 0:n], gnv, sgg[i2][:, 0:n], ALU.mult, ALU.mult)
                    idx = (slice(None), slice(tok0, tok0 + n))
                    P.dma("sp", d_br[2].view(d_br[2].t[:, tok0:tok0 + n].rearrange("(j p) n -> p j n", p=128), idx),
                          ost[:, :, 0:n])
            if stop == "brC":
                dump_br(2)
                raise _Stop()

            with P.phase("merge_%d" % l) as st:
                mT = P.sb(st, "mT", [128, 8, NTOK], BF16)
                with P.phase() as st2:
                    oX = [P.sb(st2, "oX%d" % i, [128, 4, NTOK], BF16) for i in range(3)]
                    for i in range(3):
                        idx = (slice(None), slice(0, ntok_o))
                        P.dma("sp", oX[i][:, :, 0:ntok_o],
                              d_br[i].view(d_br[i].t[:, 0:ntok_o].rearrange("(j p) n -> p j n", p=128), idx))
                    wg = [P.sb(st2, "wg%d" % i, [128, 8, 384], BF16) for i in range(2)]
                    wp = [P.sb(st2, "wp%d" % i, [128, 4, 384], BF16) for i in range(2)]
                    sgm = [P.sb(st2, "sgm%d" % i, [128, 512], F32) for i in range(2)]
                    m_ = [P.sb(st2, "mm%d" % i, [128, 512], F32) for i in range(2)]
                    t_ = [P.sb(st2, "mt%d" % i, [128, 512], F32) for i in range(2)]
                    pairs = Rot([(0, 1), (2, 3), (4, 5), (6, 7)])
                    k_ = 0
                    g_i = 0
                    pnames = ("da_proj", "cv_proj", "gla_proj")
                    for fo in range(8):
                        g_, p_ = wg[fo % 2], wp[fo % 2]
                        for br in range(3):
                            P.dma("pool", g_[:, :, br * 128:(br + 1) * 128],
                                  wview(W["w_in"], (l,), 0, 1024, OFF["gates"] + br * 1024 + fo * 128, 128))
                            P.dma("pool", p_[:, :, br * 128:(br + 1) * 128],
                                  wview(W[pnames[br]], (l,), 0, 512, fo * 128, 128))
                        for (tok0, n) in TGo:
                            i2 = g_i % 2
                            g_i += 1
                            for br in range(3):
                                by, bg = pairs()
                                proj_fm(P.pf(by, 0, n), p_, br * 128, 128, oX[br], tok0, n, KC=4)
                                proj_fm(P.pf(bg, 0, n), g_, br * 128, 128, hT, tok0, n)
                                sg_ = sgm[k_ % 2]
                                tt_ = t_[k_ % 2]
                                k_ += 1
                                P.act(sg_[:, 0:n], P.pf(bg, 0, n), AF.Sigmoid)
                                if br == 0:
                                    P.tt("dve", m_[i2][:, 0:n], P.pf(by, 0, n), sg_[:, 0:n], ALU.mult)
                                elif br == 1:
                                    P.tt("dve", tt_[:, 0:n], P.pf(by, 0, n), sg_[:, 0:n], ALU.mult)
                                    P.tt("dve", m_[i2][:, 0:n], m_[i2][:, 0:n], tt_[:, 0:n], ALU.add)
                                else:
                                    P.tt("dve", tt_[:, 0:n], P.pf(by, 0, n), sg_[:, 0:n], ALU.mult)
                                    P.tt("pool", mT[:, fo, tok0:tok0 + n], m_[i2][:, 0:n], tt_[:, 0:n], ALU.add)
                if b == 0 and l == 0:
                    tap("mT", mT.all(), BF16)
                wout = P.sb(st, "wout", [128, 8, 1024], BF16)
                P.dma("pool", wout.all(), wview(W["w_out"], (l,), 0, 1024, 0, 1024))
                G1 = gate_vec(st, l, b, "g_mix_post", 2, "l")
                G1c = gate_vec(st, l, 4, "g_mix_post", 2, "c") if need_ctx else None
                xin = [P.sb(st, "rxin%d" % i, [128, 1024], F32) for i in range(3)]
                tm = [P.sb(st, "rtm%d" % i, [128, 1024], F32) for i in range(2)]
                xo = [P.sb(st, "rxo%d" % i, [128, 1024], F32) for i in range(2)]
                rst = P.sb(st, "rstat", [128, 4 * NT], F32)
                pairs = Rot([(0, 1), (2, 3), (4, 5), (6, 7)])
                for i, t in enumerate(tiles_out):
                    bks = pairs()
                    for hf in range(2):
                        o = P.pf(bks[hf], 0, 512)
                        for kc in range(8):
                            P.mm(o, mT[:, kc, t * 128:(t + 1) * 128], wout[:, kc, hf * 512:(hf + 1) * 512],
                                 start=(kc == 0), stop=(kc == 7))
                    xt, tmi, xoi = xin[i % 3], tm[i % 2], xo[i % 2]
                    P.dma("sp", xt.all(), src_tile(l, b, t))
                    ss0, ss1, ss, rs = (rst[:, 4 * i + q:4 * i + q + 1] for q in range(4))
                    P.act(tmi[:, 0:512], P.pf(bks[0], 0, 512), AF.Square, accum=ss0)
                    P.act(tmi[:, 512:1024], P.pf(bks[1], 0, 512), AF.Square, accum=ss1)
                    P.tt("dve", ss, ss0, ss1, ALU.add)
                    rsqrt(rs, ss, 1.0 / D)
                    G = G1 if t < 16 else G1c
                    for hf in range(2):
                        P.stt(tmi[:, hf * 512:(hf + 1) * 512], P.pf(bks[hf], 0, 512), rs, G[:, hf * 512:(hf + 1) * 512],
                              ALU.mult, ALU.mult)
                    P.tt("pool", xoi.all(), tmi.all(), xt.all(), ALU.add)
                    P.dma("sp", d_xs[t * 128:(t + 1) * 128, :], xoi.all())
            if stop == "mixer" and l == layers[-1]:
                dump_xs()
                raise _Stop()

            moe = (l == 1)
            nexp = NEXP if moe else 1
            if moe:
                stl = [list(range(0, 8)), list(range(8, 16))]
            else:
                stl = [list(range(0, 9)), list(range(9, 18))]
            for tl in stl:
                with P.phase("ffn_%d" % l) as st:
                    T = len(tl) * 128
                    h2T = hT
                    with P.phase() as st2:
                        A2, B2 = affine_vecs(st2, l, b, "g_ffn_pre", 3, 4, "l")
                        if need_ctx:
                            A2c, B2c = affine_vecs(st2, l, 4, "g_ffn_pre", 3, 4, "c")
                        norm_tiles(st2, tl, lambda t: d_xs[t * 128:(t + 1) * 128, :],
                                   lambda t: (A2, B2) if t < 16 else (A2c, B2c), h2T, lambda t: (t - tl[0]) * 128)
                    acc = P.sb(st, "acc", [128, len(tl), 1024], F32)
                    if moe:
                        gates = P.sb(st, "gates", [128, len(tl), 8], F32)
                    st3 = ExitStack()
                    st3.__enter__()
                    aT = P.sb(st3, "aT", [128, NFC, T], BF16)
                    w13 = [P.sb(st3, "w13_%d" % i, [128, 8, 2, 256], BF16) for i in range(3)]
                    w2h = [P.sb(st3, "w2h%d" % i, [128, NFC, 512], BF16) for i in range(2)]
                    sl = [P.sb(st3, "sl%d" % i, [128, 512], F32) for i in range(2)]
                    rup = Rot([(0, 1), (2, 3)])
                    rdn = Rot([4, 5, 6, 7])
                    if moe:
                        rw = P.sb(st3, "rw", [128, 8, 8], BF16)
                        ridx = (0, slice(None), slice(None))
                        P.dma("pool", rw.all(), W["moe_router"].view(
                            W["moe_router"].t[0].rearrange("(k p) e -> p k e", p=128), ridx))
                        lg = P.sb(st3, "lg", [128, len(tl), 8], F32)
                        l2 = P.sb(st3, "l2", [128, len(tl), 8], F32)
                        eq1 = P.sb(st3, "eq1", [128, len(tl), 8], F32)
                        eq2 = P.sb(st3, "eq2", [128, len(tl), 8], F32)
                        gs = P.sb(st3, "gs", [128, len(tl), 8], F32)
                        for j in range(len(tl)):
                            bk = rdn()
                            o = P.pf(bk, 0, 8)
                            for kc in range(8):
                                P.mm(o, h2T[:, kc, j * 128:(j + 1) * 128], rw[:, kc, :], start=(kc == 0), stop=(kc == 7))
                            P.copy("dve", lg[:, j, :], o)
                            m1, m2_, dd, ee, den, w1, w2 = (gs[:, j, q:q + 1] for q in range(7))
                            P.rmax(m1, lg[:, j, :])
                            P.ts("dve", eq1[:, j, :], lg[:, j, :], m1, ALU.is_equal)
                            P.stt(l2[:, j, :], eq1[:, j, :], -1e30, lg[:, j, :], ALU.mult, ALU.add)
                            P.rmax(m2_, l2[:, j, :])
                            P.ts("dve", eq2[:, j, :], l2[:, j, :], m2_, ALU.is_equal)
                            P.tt("dve", dd, m2_, m1, ALU.subtract)
                            P.act(ee, dd, AF.Exp)
                            P.ts("dve", den, ee, 1.0, ALU.add)
                            P.recip(w1, den)
                            P.tt("dve", w2, ee, w1, ALU.mult)
                            P.ts("dve", gates[:, j, :], eq1[:, j, :], w1, ALU.mult)
                            P.stt(gates[:, j, :], eq2[:, j, :], w2, gates[:, j, :], ALU.mult, ALU.add)
                    subs = [(s0, 384) for s0 in range(0, T, 384)] if T % 512 else [(s0, 512) for s0 in range(0, T, 512)]
                    k_ = 0
                    for e in range(nexp):
                        if moe:
                            w1d, w3d, w2d, pre = W["moe_w1"], W["moe_w3"], W["moe_w2"], (0, e)
                        else:
                            w1d, w3d, w2d, pre = W["ffn_w1"], W["ffn_w3"], W["ffn_w2"], (0,)

                        def load_w2(hf):
                            wh = w2h[hf]
                            idx = pre + (slice(None), slice(hf * 512, (hf + 1) * 512))
                            P.dma("pool", wh.all(), w2d.view(w2d.t[idx].rearrange("(f p) n -> p f n", p=128), idx))
                        for fp in range(NFC // 2):
                            ws = w13[(e * (NFC // 2) + fp) % 3]
                            P.dma("pool", ws[:, :, 0, :], wview(w1d, pre, 0, 1024, fp * 256, 256))
                            P.dma("pool", ws[:, :, 1, :], wview(w3d, pre, 0, 1024, fp * 256, 256))
                            if fp == 3:
                                load_w2(0)
                            if fp == 7:
                                load_w2(1)
                            for fi in range(2):
                                fc = fp * 2 + fi
                                for (s0, n) in subs:
                                    b1_, b3_ = rup()
                                    for kc in range(8):
                                        P.mm(P.pf(b1_, 0, n), ws[:, kc, 0, fi * 128:(fi + 1) * 128], h2T[:, kc, s0:s0 + n],
                                             start=(kc == 0), stop=(kc == 7))
                                    for kc in range(8):
                                        P.mm(P.pf(b3_, 0, n), ws[:, kc, 1, fi * 128:(fi + 1) * 128], h2T[:, kc, s0:s0 + n],
                                             start=(kc == 0), stop=(kc == 7))
                                    sl_ = sl[k_ % 2]
                                    k_ += 1
                                    P.act(sl_[:, 0:n], P.pf(b1_, 0, n), AF.Silu)
                                    P.tt("dve", aT[:, fc, s0:s0 + n], P.pf(b3_, 0, n), sl_[:, 0:n], ALU.mult)
                        for hf in range(2):
                            wh = w2h[hf]
                            for j in range(len(tl)):
                                bk = rdn()
                                o = P.pf(bk, 0, 512)
                                for fc in range(NFC):
                                    P.mm(o, aT[:, fc, j * 128:(j + 1) * 128], wh[:, fc, :], start=(fc == 0),
                                         stop=(fc == NFC - 1))
                                a_ = acc[:, j, hf * 512:(hf + 1) * 512]
                                if not moe:
                                    P.copy("act", a_, o)
                                elif e == 0:
                                    P.ts("dve", a_, o, gates[:, j, 0:1], ALU.mult)
                                else:
                                    P.stt(a_, o, gates[:, j, e:e + 1], a_, ALU.mult, ALU.add)
                    P.barrier()
                    st3.__exit__(None, None, None)
                    G2 = gate_vec(st, l, b, "g_ffn_post", 5, "l")
                    if need_ctx:
                        G2c = gate_vec(st, l, 4, "g_ffn_post", 5, "c")
                    xin = [P.sb(st, "fxin%d" % i, [128, 1024], F32) for i in range(2)]
                    jk = [P.sb(st, "fjk%d" % i, [128, 1024], F32) for i in range(2)]
                    xo = [P.sb(st, "fxo%d" % i, [128, 1024], F32) for i in range(2)]
                    fst = P.sb(st, "fstat", [128, 2 * len(tl)], F32)
                    for j, t in enumerate(tl):
                        xt, jki, xoi = xin[j % 2], jk[j % 2], xo[j % 2]
                        P.dma("sp", xt.all(), d_xs[t * 128:(t + 1) * 128, :])
                        ss, rs = fst[:, 2 * j:2 * j + 1], fst[:, 2 * j + 1:2 * j + 2]
                        P.stt(jki.all(), acc[:, j, :], 1.0, acc[:, j, :], ALU.mult, ALU.mult, accum=ss)
                        rsqrt(rs, ss, 1.0 / D)
                        G = G2 if t < 16 else G2c
                        P.stt(jki.all(), acc[:, j, :], rs, G.all(), ALU.mult, ALU.mult)
                        P.tt("pool", xoi.all(), jki.all(), xt.all(), ALU.add)
                        if l == DEPTH - 1:
                            P.dma("sp", d_out[b, t * 128:(t + 1) * 128, :], xoi.all())
                        else:
                            P.dma("sp", d_xs[t * 128:(t + 1) * 128, :], xoi.all())
            if stop == "ffn" and l == layers[-1]:
                dump_xs()
                raise _Stop()


def make_in_maps(inp, NB, ncores, layers=(0, 1)):
    consts = _host_consts()
    pv = np.zeros((2, 128, PV_N), np.float32)
    for l in range(2):
        pv[l, :, PV_DWB:PV_DWB + 4] = inp["cv_dw_b"][l].reshape(4, 128).T
        pv[l, :, PV_LNG:PV_LNG + 4] = inp["cv_ln_g"][l].reshape(4, 128).T
        pv[l, :, PV_LNB:PV_LNB + 4] = inp["cv_ln_b"][l].reshape(4, 128).T
        pv[l, :, PV_AB:PV_AB + 4] = inp["gla_a_b"][l].reshape(4, 128).T
        pv[l, :, PV_GN] = inp["gla_norm"][l]
        pv[l, :, PV_DW:PV_DW + 124] = inp["cv_dw"][l].T.reshape(4, 128, 31).transpose(1, 0, 2).reshape(128, 124)
    maps = []
    for c in range(ncores):
        b0 = c * NB
        m = {"x": np.ascontiguousarray(inp["x"][b0:b0 + NB]), "ctx": np.ascontiguousarray(inp["ctx"][b0:b0 + NB])}
        cc = np.concatenate([inp["c"][b0:b0 + NB], np.zeros((4 - NB, D), np.float32), inp["c_ctx"][None]], 0)
        m["cT"] = np.ascontiguousarray(cc.reshape(5, 8, 128).transpose(2, 1, 0))
        m["pvec"] = pv
        m.update(consts)
        for k in WEIGHT_NAMES:
            if k not in ("cv_dw", "cv_dw_b", "cv_ln_g", "cv_ln_b", "gla_a_b", "gla_norm") \
                    and not (k.startswith("moe_") and 1 not in layers):
                m[k] = inp[k]
        maps.append(m)
    return maps


def build(NB=4, taps=(), stop=None, layers=(0, 1)):
    ctxd = {}
    try:
        _build(ctxd, NB, taps, stop, layers)
    except _Stop:
        pass
    return _finish(ctxd["nc"], ctxd["P"], ctxd["E_"], ctxd["tap_out"])


_CACHE = {}


def kernel(**inputs):
    inp = {k: np.ascontiguousarray(np.asarray(v)) for k, v in inputs.items()}
    NB = inp["x"].shape[0] // NCORES
    if "nc" not in _CACHE:
        _CACHE["nc"] = build(NB=NB)[0]
    maps = make_in_maps(inp, NB, NCORES)
    res = run_bass_kernel_spmd(_CACHE["nc"], maps, core_ids=list(range(NCORES)))
    out = np.concatenate([np.asarray(r["out"]) for r in res.results], axis=0)
    return out.astype(np.float32, copy=False)
```

```python
import math
import os
from contextlib import ExitStack, contextmanager

import numpy as np
import concourse.bass as bass
import concourse.mybir as mybir
from concourse.bass_utils import run_bass_kernel_spmd

F32 = mybir.dt.float32
BF16 = mybir.dt.bfloat16
AF = mybir.ActivationFunctionType
ALU = mybir.AluOpType

NCORES = 8
D = 1024
SEQ = 2048
CTX = 256
NTOK = SEQ + CTX
NT = NTOK // 128
DEPTH = 2
DFF = 2816
NFC = DFF // 128
NEXP = 8
INW = 7200
OFF = dict(aq=0, ak=512, av=1024, cin=1536, gq=2560, gk=2816, gv=3072, gg=3584, ga=4096, gates=4128)
EPS = 1e-6


def _ovl(a, b):
    for (al, ah), (bl, bh) in zip(a, b):
        if al >= bh or bl >= ah:
            return False
    return True


def _inside(a, b):
    for (al, ah), (bl, bh) in zip(a, b):
        if al < bl or ah > bh:
            return False
    return True


class V:
    __slots__ = ("ap", "tt", "reg")

    def __init__(self, ap, tt, reg):
        self.ap, self.tt, self.reg = ap, tt, reg


class TT:
    def __init__(self, name, t, shape, local=True, psum=False):
        self.name, self.t, self.shape, self.local, self.psum = name, t, list(shape), local, psum
        self.w = {}
        self.r = {}

    def reg_of(self, idx):
        if not isinstance(idx, tuple):
            idx = (idx,)
        reg = []
        for d, n in enumerate(self.shape):
            if d < len(idx):
                i = idx[d]
                if isinstance(i, slice):
                    lo = 0 if i.start is None else i.start
                    hi = n if i.stop is None else i.stop
                else:
                    lo, hi = i, i + 1
            else:
                lo, hi = 0, n
            assert 0 <= lo < hi <= n, (self.name, idx, self.shape)
            reg.append((lo, hi))
        return tuple(reg)

    def __getitem__(self, idx):
        return V(self.t[idx], self, self.reg_of(idx))

    def all(self):
        return self[tuple(slice(None) for _ in self.shape)]

    def view(self, ap, idx):
        return V(ap, self, self.reg_of(idx))


class Prog:
    def __init__(self, nc, E):
        self.nc, self.E = nc, E
        self.eng = dict(pe=nc.tensor, act=nc.scalar, dve=nc.vector, pool=nc.gpsimd, sp=nc.sync)
        self.semh = {}
        self.cnt = {}
        for e in ("pe", "act", "dve", "pool"):
            self.semh[e] = E(nc.semaphore("s_" + e))
            self.cnt[e] = 0
        self.pe_pending = False
        self.waited = {e: {} for e in self.eng}
        self.dq = {}
        for q, n in (("sp", 20), ("pool", 12), ("act", 6)):
            self.dq[q] = [0, []]
            for i in range(n):
                sk = "d_%s%d" % (q, i)
                self.semh[sk] = E(nc.semaphore(sk))
                self.cnt[sk] = 0
                self.dq[q][1].append(sk)
        self.local_dma = {}
        self.bar_toks = []
        self.uid = 0
        self.nins = 0
        self.psall = E(nc.psum_tensor("psall", [128, 4096], F32))
        self.psall16 = self.psall.bitcast(BF16)
        self.psb = [TT("ps%d" % i, self.psall[:, i * 512:(i + 1) * 512], [128, 512], local=False, psum=True)
                    for i in range(8)]
        self.psb16 = [self.psall16[:, i * 1024:(i + 1) * 1024] for i in range(8)]
        self.bank_rr = 0

    def sb(self, st, name, shape, dtype, local=True):
        self.uid += 1
        t = st.enter_context(self.nc.sbuf_tensor("%s_%d" % (name, self.uid), list(shape), dtype))
        return TT(name, t, shape, local=local)

    def bank(self):
        b = self.bank_rr
        self.bank_rr = (b + 1) % 8
        return b

    def pf(self, b, c0, c1, p0=0, p1=128):
        return self.psb[b][p0:p1, c0:c1]

    def pb(self, b, c0, c1, p0=0, p1=128):
        return V(self.psb16[b][p0:p1, c0:c1], self.psb[b], ((p0, p1), (c0 // 2, (c1 + 1) // 2)))

    def _deps(self, outs, ins):
        toks = []
        for v in ins:
            if v.tt.psum:
                toks.extend(v.tt.w.values())
                continue
            for reg, tok in v.tt.w.items():
                if _ovl(reg, v.reg):
                    toks.append(tok)
        for v in outs:
            if v.tt.psum:
                toks.extend(v.tt.w.values())
                continue
            for reg, tok in v.tt.w.items():
                if _ovl(reg, v.reg):
                    toks.append(tok)
            for reg, d in v.tt.r.items():
                if _ovl(reg, v.reg):
                    toks.extend(d.items())
        return toks

    def _wait(self, eng, toks):
        need = {}
        w = self.waited[eng]
        for sk, val in toks:
            if sk == "pe" and eng == "pe":
                continue
            if w.get(sk, 0) >= val:
                continue
            if need.get(sk, 0) < val:
                need[sk] = val
        for sk, val in need.items():
            self.eng[eng].wait_ge(self.semh[sk], val)
            w[sk] = val

    def _record(self, outs, ins, tok):
        sk, val = tok
        for v in list(ins) + list(outs):
            if v.tt.psum:
                v.tt.w = {((0, 128), (0, 512)): tok}
        for v in ins:
            if v.tt.psum:
                continue
            v.tt.r.setdefault(v.reg, {})[sk] = val
        for v in outs:
            tt = v.tt
            if tt.psum:
                continue
            for reg in [r for r in tt.w if _inside(r, v.reg)]:
                del tt.w[reg]
            for reg in [r for r in tt.r if _inside(r, v.reg)]:
                del tt.r[reg]
            tt.w[v.reg] = tok

    def op(self, eng, fn, outs, ins, signal=True):
        self._wait(eng, self._deps(outs, ins))
        inst = fn(self.eng[eng])
        self.nins += 1
        if eng == "pe" and not signal:
            tok = ("pe", self.cnt["pe"] + 1)
            self.pe_pending = True
        else:
            self.cnt[eng] += 1
            inst.then_inc(self.semh[eng], 1)
            tok = (eng, self.cnt[eng])
            if eng == "pe":
                self.pe_pending = False
        self._record(outs, ins, tok)

    def dma(self, q, out, in_, **kw):
        st = self.dq[q]
        sk = st[1][st[0]]
        st[0] = (st[0] + 1) % len(st[1])
        toks = self._deps([out], [in_])
        if self.cnt[sk]:
            toks.append((sk, self.cnt[sk]))
        local = out.tt.local or in_.tt.local
        if out.tt.local:
            toks.extend(self.bar_toks)
        self._wait(q, toks)
        inst = self.eng[q].dma_start(out=out.ap, in_=in_.ap, **kw)
        self.nins += 1
        self.cnt[sk] += 16
        inst.then_inc(self.semh[sk], 16)
        tok = (sk, self.cnt[sk])
        if local:
            self.local_dma[sk] = self.cnt[sk]
        self._record([out], [in_], tok)
        return tok

    def barrier(self):
        assert not self.pe_pending
        toks = [(e, self.cnt[e]) for e in ("pe", "act", "dve", "pool") if self.cnt[e]]
        toks += list(self.local_dma.items())
        for e in ("pe", "act", "dve", "pool"):
            self._wait(e, [t for t in toks if t[0] != e])
        self.bar_toks = [(e, self.cnt[e]) for e in ("pe", "act", "dve", "pool") if self.cnt[e]]
        self.local_dma = {}

    @contextmanager
    def phase(self, name=None):
        st = ExitStack()
        st.__enter__()
        if name and os.environ.get("K_SCOPES"):
            st.enter_context(self.nc.named_scope(name))
        try:
            yield st
        finally:
            self.barrier()
            st.__exit__(None, None, None)

    def finish(self, toks):
        self._wait("sp", toks)

    def mm(self, out, lhsT, rhs, start=True, stop=True):
        self.op("pe", lambda e: e.matmul(out.ap, lhsT=lhsT.ap, rhs=rhs.ap, start=start, stop=stop),
                [out], [lhsT, rhs], signal=stop)

    def tr(self, out, in_, ident, signal=True):
        self.op("pe", lambda e: e.transpose(out=out.ap, in_=in_.ap, identity=ident.ap), [out], [in_, ident],
                signal=signal)

    @staticmethod
    def _sc(x):
        return x.ap if isinstance(x, V) else x

    def act(self, out, in_, func, bias=None, scale=None, accum=None, xins=()):
        kw = {}
        ins = [in_] + list(xins)
        outs = [out]
        if bias is not None:
            kw["bias"] = self._sc(bias)
            if isinstance(bias, V):
                ins.append(bias)
        if scale is not None:
            kw["scale"] = self._sc(scale)
            if isinstance(scale, V):
                ins.append(scale)
        if accum is not None:
            kw["accum_out"] = accum.ap
            outs.append(accum)
        self.op("act", lambda e: e.activation(out=out.ap, in_=in_.ap, func=func, **kw), outs, ins)

    def tt(self, eng, out, a, b, op):
        self.op(eng, lambda e: e.tensor_tensor(out=out.ap, in0=a.ap, in1=b.ap, op=op), [out], [a, b])

    def ts(self, eng, out, a, s1, op0, s2=None, op1=None, accum=None):
        ins = [a] + [s for s in (s1, s2) if isinstance(s, V)]
        outs = [out] + ([accum] if accum is not None else [])
        kw = {}
        if op1 is not None:
            kw["op1"] = op1
        if accum is not None:
            kw["accum_out"] = accum.ap
        self.op(eng, lambda e: e.tensor_scalar(out=out.ap, in0=a.ap, scalar1=self._sc(s1), scalar2=self._sc(s2),
                                               op0=op0, **kw), outs, ins)

    def stt(self, out, a, s, b, op0, op1, accum=None):
        ins = [a, b] + ([s] if isinstance(s, V) else [])
        outs = [out] + ([accum] if accum is not None else [])
        kw = {"accum_out": accum.ap} if accum is not None else {}
        self.op("dve", lambda e: e.scalar_tensor_tensor(out=out.ap, in0=a.ap, scalar=self._sc(s), in1=b.ap,
                                                        op0=op0, op1=op1, **kw), outs, ins)

    def copy(self, eng, out, in_):
        if eng == "act":
            self.op("act", lambda e: e.copy(out=out.ap, in_=in_.ap), [out], [in_])
        else:
            self.op(eng, lambda e: e.tensor_copy(out=out.ap, in_=in_.ap), [out], [in_])

    def recip(self, out, in_):
        self.op("dve", lambda e: e.reciprocal(out=out.ap, in_=in_.ap), [out], [in_])

    def memset(self, eng, out, val):
        self.op(eng, lambda e: e.memset(out.ap, val), [out], [])

    def scan(self, out, d0, d1, init, op0, op1):
        self.op("dve", lambda e: e.tensor_tensor_scan(out=out.ap, data0=d0.ap, data1=d1.ap, initial=init,
                                                      op0=op0, op1=op1), [out], [d0, d1])

    def rmax(self, out, in_):
        self.op("dve", lambda e: e.reduce_max(out=out.ap, in_=in_.ap, axis=mybir.AxisListType.X), [out], [in_])


def _host_consts():
    t = np.arange(SEQ)
    pos = np.stack([t // 64, t % 64], 0).astype(np.float32)
    inv = (10000.0 ** (-np.arange(16, dtype=np.float32) / 16)).astype(np.float32)
    p = np.arange(128)
    j = p % 64
    ang = pos[j // 32][:, :] * inv[j % 16][:, None]
    cosT = np.cos(ang).astype(np.float32)
    sinT = np.sin(ang).astype(np.float32)
    rot = np.zeros((128, 128), np.float32)
    for m in range(128):
        half = (m % 32) // 16
        if half == 0:
            rot[m + 16, m] = -1.0
        else:
            rot[m - 16, m] = 1.0
    ident = np.eye(128, dtype=np.float32)
    jj, ii = np.meshgrid(np.arange(128), np.arange(128), indexing="ij")
    mlow = (jj <= ii).astype(np.float32)
    mup = (jj >= ii).astype(np.float32)
    smask = np.ones((128, NTOK), np.float32)
    smask[:, ::128] = 0.0
    consts = np.concatenate([ident, rot, mlow, mup, np.ones((128, 128), np.float32)], 1)
    return dict(k_consts=consts, k_cos=cosT, k_sin=sinT, k_smask=smask)


WEIGHT_NAMES = ["ada_w", "ada_b", "g_mix_pre", "g_mix_post", "g_ffn_pre", "g_ffn_post", "w_in", "da_lambda",
                "da_subln", "da_proj", "cv_dw", "cv_dw_b", "cv_ln_g", "cv_ln_b", "cv_proj", "gla_a_up", "gla_a_b",
                "gla_norm", "gla_proj", "w_out", "ffn_w1", "ffn_w3", "ffn_w2", "moe_router", "moe_w1", "moe_w3",
                "moe_w2"]
WEIGHT_SHAPES = dict(ada_w=[2, 1024, 6144], ada_b=[2, 6144], g_mix_pre=[2, 1024], g_mix_post=[2, 1024],
                     g_ffn_pre=[2, 1024], g_ffn_post=[2, 1024], w_in=[2, 1024, 7200], da_lambda=[2, 4, 64],
                     da_subln=[2, 128], da_proj=[2, 512, 1024], cv_dw=[2, 31, 512], cv_dw_b=[2, 512],
                     cv_ln_g=[2, 512], cv_ln_b=[2, 512], cv_proj=[2, 512, 1024], gla_a_up=[2, 2, 16, 256],
                     gla_a_b=[2, 2, 256], gla_norm=[2, 128], gla_proj=[2, 512, 1024], w_out=[2, 1024, 1024],
                     ffn_w1=[1, 1024, 2816], ffn_w3=[1, 1024, 2816], ffn_w2=[1, 2816, 1024],
                     moe_router=[1, 1024, 8], moe_w1=[1, 8, 1024, 2816], moe_w3=[1, 8, 1024, 2816],
                     moe_w2=[1, 8, 2816, 1024])


def _finish(nc, P, E_, tap_out):
    assert not P.pe_pending
    toks = [(sk, P.cnt[sk]) for q in P.dq.values() for sk in q[1] if P.cnt[sk]]
    toks += [(e, P.cnt[e]) for e in ("pe", "act", "dve", "pool") if P.cnt[e]]
    P._wait("sp", toks)
    E_.close()
    return nc, list(tap_out.keys())


PV_DWB, PV_LNG, PV_LNB, PV_AB, PV_GN, PV_DW = 0, 4, 8, 12, 16, 17
PV_N = 17 + 124
TG512 = [(0, 512), (512, 512), (1024, 512), (1536, 512), (2048, 256)]


class _Stop(Exception):
    pass


class Rot:
    def __init__(self, items):
        self.items, self.i = list(items), 0

    def __call__(self):
        x = self.items[self.i]
        self.i = (self.i + 1) % len(self.items)
        return x


def _build(ctxd, NB, taps, stop, layers):
    nc = bass.Bass("TRN2", target_bir_lowering=False)
    E_ = ExitStack()
    E = E_.enter_context
    P = Prog(nc, E)
    tap_out = {}
    ctxd.update(nc=nc, P=P, E_=E_, tap_out=tap_out)

    def dram(name, shape, kind, dtype=F32):
        t = nc.dram_tensor(name, list(shape), dtype, kind=kind)
        return TT(name, t.ap(), shape, local=False)

    d_x = dram("x", [NB, SEQ, D], "ExternalInput")
    d_ctx = dram("ctx", [NB, CTX, D], "ExternalInput")
    d_cT = dram("cT", [128, 8, 5], "ExternalInput")
    d_pv = dram("pvec", [2, 128, PV_N], "ExternalInput")
    d_k = {k: dram(k, list(v.shape), "ExternalInput") for k, v in _host_consts().items()}
    W = {k: dram(k, WEIGHT_SHAPES[k], "ExternalInput") for k in WEIGHT_NAMES if k not in
         ("cv_dw", "cv_dw_b", "cv_ln_g", "cv_ln_b", "gla_a_b", "gla_norm")
         and not (k.startswith("moe_") and 1 not in layers)}
    d_out = dram("out", [NB, SEQ, D], "ExternalOutput")
    d_xs = dram("xs", [NTOK, D], "Internal")
    d_modd = dram("modd", [2, 5, 6144], "Internal")
    d_br = [dram("br%d" % i, [512, NTOK], "Internal", BF16) for i in range(3)]

    def tap(name, v, dtype=F32):
        if name not in taps:
            return
        shape = [hi - lo for lo, hi in v.reg]
        t = dram("tap_" + name, shape, "ExternalOutput", dtype)
        tap_out[name] = t
        P.dma("sp", t.all(), v)

    def dump_br(i):
        t = dram("tap_br%d" % i, [512, NTOK], "ExternalOutput", BF16)
        tap_out["br%d" % i] = t
        P.dma("sp", t.all(), d_br[i].all())

    def dump_xs():
        t = dram("tap_xs", [NTOK, D], "ExternalOutput")
        tap_out["xs"] = t
        P.dma("sp", t.all(), d_xs.all())

    kc_f = P.sb(E_, "kconst", [128, 640], F32, local=False)
    P.dma("sp", kc_f.all(), d_k["k_consts"].all())
    kc_b = P.sb(E_, "kconstb", [128, 640], BF16, local=False)
    P.copy("dve", kc_b.all(), kc_f.all())
    identb, rotb, onesb = kc_b[:, 0:128], kc_b[:, 128:256], kc_b[:, 512:640]
    identf = kc_f[:, 0:128]
    maskf = [kc_f[:, 256:384], kc_f[:, 384:512]]
    od = P.sb(E_, "onesdiv", [128, 256], BF16, local=False)
    P.memset("dve", od[:, 0:128], 1.0 / 512)
    P.memset("dve", od[:, 128:256], 1.0 / 128)
    od512, od128 = od[:, 0:128], od[:, 128:256]
    hT = P.sb(E_, "hT", [128, 8, NTOK], BF16, local=False)

    def bcast_load(st, name, dT, idx, n=1024):
        t = P.sb(st, name, [128, n], F32)
        ap = dT.t[idx].broadcast_to([128, n])
        P.dma("sp", t.all(), dT.view(ap, idx))
        return t

    def rsqrt(out, ss, scale, eps=EPS):
        P.act(out, ss, AF.Ln, bias=eps, scale=scale)
        P.act(out, out, AF.Exp, scale=-0.5)

    def wview(dT, pre, r0, nr, c0, ncol):
        idx = tuple(pre) + (slice(r0, r0 + nr), slice(c0, c0 + ncol))
        return dT.view(dT.t[idx].rearrange("(k p) n -> p k n", p=128), idx)

    with P.phase() as st:
        cT = P.sb(st, "cT", [128, 8, 5], F32)
        P.dma("sp", cT.all(), d_cT.all())
        sc = P.sb(st, "sc", [128, 8, 5], F32)
        P.act(sc.all(), cT.all(), AF.Silu)
        ones5 = P.sb(st, "ones5", [1, 8], F32)
        P.memset("dve", ones5.all(), 1.0)
        modsb = P.sb(st, "modsb", [5, 6144], F32)
        wsl = [P.sb(st, "adaw%d" % i, [128, 8, 512], F32) for i in range(2)]
        bsb = [P.sb(st, "adab%d" % i, [1, 6144], F32) for i in range(2)]
        rb = Rot([0, 1, 2, 3])
        for l in range(2):
            P.dma("sp", bsb[l].all(), W["ada_b"][l:l + 1, :])
            for g in range(12):
                slot = wsl[g % 2]
                P.dma("sp", slot.all(), wview(W["ada_w"], (l,), 0, 1024, g * 512, 512))
                b = rb()
                o = P.pf(b, 0, 512, 0, 5)
                for kc in range(8):
                    P.mm(o, sc[:, kc, :], slot[:, kc, :], start=(kc == 0), stop=False)
                P.mm(o, ones5[0:1, 0:5], bsb[l][0:1, g * 512:(g + 1) * 512], start=False, stop=True)
                P.copy("dve", modsb[:, g * 512:(g + 1) * 512], o)
            P.dma("sp", d_modd[l], modsb.all())
    if stop == "mod":
        tapall = dram("tap_modd", [2, 5, 6144], "ExternalOutput")
        tap_out["modd"] = tapall
        with P.phase() as st:
            t_ = P.sb(st, "t_", [10, 6144], F32)
            P.dma("sp", t_.all(), d_modd.view(d_modd.t[:, :, :].rearrange("a b c -> (a b) c"), (slice(None),) * 3))
            P.dma("sp", tapall.view(tapall.t[:, :, :].rearrange("a b c -> (a b) c"), (slice(None),) * 3), t_.all())
        raise _Stop()

    def modvec(st, name, l, row, j):
        return bcast_load(st, name, d_modd, (l, slice(row, row + 1), slice(j * 1024, (j + 1) * 1024)))

    def gvec(st, name, wname, l):
        return bcast_load(st, name, W[wname], (slice(l, l + 1), slice(None)))

    def affine_vecs(st, l, row, gname, jshift, jscale, tag, stmp=None):
        s = modvec(st, "s" + tag, l, row, jscale)
        Bv = modvec(st, "B" + tag, l, row, jshift)
        with P.phase() as stt_:
            g = gvec(stt_, "g" + tag, gname, l)
            P.stt(s.all(), s.all(), 1.0, g.all(), ALU.add, ALU.mult)
        return s, Bv

    def gate_vec(st, l, row, gname, j, tag, stmp=None):
        m = modvec(st, "gm" + tag, l, row, j)
        with P.phase() as stt_:
            g = gvec(stt_, "gg" + tag, gname, l)
            P.tt("dve", m.all(), m.all(), g.all(), ALU.mult)
        return m

    def src_tile(l, b, t):
        if l == 0:
            if t < 16:
                return d_x[b, t * 128:(t + 1) * 128, :]
            return d_ctx[b, (t - 16) * 128:(t - 15) * 128, :]
        return d_xs[t * 128:(t + 1) * 128, :]

    def norm_tiles(st, tiles, srcfn, vecs_for, dstT, col_of):
        xin = [P.sb(st, "xin%d" % i, [128, 1024], F32) for i in range(3)]
        junk = [P.sb(st, "junk%d" % i, [128, 1024], F32) for i in range(3)]
        hb = [P.sb(st, "hb%d" % i, [128, 1024], BF16) for i in range(2)]
        stats = P.sb(st, "nstats", [128, 2 * len(tiles)], F32)
        rb = Rot([0, 1, 2, 3])
        def stage_a(i, t):
            xt, jk = xin[i % 3], junk[i % 3]
            P.dma("sp", xt.all(), srcfn(t))
            ss, rs = stats[:, 2 * i:2 * i + 1], stats[:, 2 * i + 1:2 * i + 2]
            P.stt(jk.all(), xt.all(), 1.0, xt.all(), ALU.mult, ALU.mult, accum=ss)
            rsqrt(rs, ss, 1.0 / D)

        def stage_b(i, t):
            xt, jk, hbt = xin[i % 3], junk[i % 3], hb[i % 2]
            A, Bv = vecs_for(t)
            rs = stats[:, 2 * i + 1:2 * i + 2]
            P.stt(jk.all(), xt.all(), rs, A.all(), ALU.mult, ALU.mult)
            P.tt("pool", hbt[:, 0:384], jk[:, 0:384], Bv[:, 0:384], ALU.add)
            P.tt("dve", hbt[:, 384:1024], jk[:, 384:1024], Bv[:, 384:1024], ALU.add)

        def stage_c(i, t):
            hbt = hb[i % 2]
            b = rb()
            for kc in range(8):
                P.tr(P.pb(b, kc * 128, (kc + 1) * 128), hbt[:, kc * 128:(kc + 1) * 128], identb, signal=(kc == 7))
            c0 = col_of(t)
            src = V(P.psb16[b][:, 0:1024].rearrange("p (k n) -> p k n", k=8), P.psb[b], ((0, 128), (0, 512)))
            P.copy("act", dstT[:, :, c0:c0 + 128], src)

        n_ = len(tiles)
        for i in range(n_ + 2):
            if i < n_:
                stage_a(i, tiles[i])
            if 0 <= i - 1 < n_:
                stage_b(i - 1, tiles[i - 1])
            if 0 <= i - 2 < n_:
                stage_c(i - 2, tiles[i - 2])

    def proj_fm(out, wslot, wc0, M, src, tok0, n, KC=8):
        for kc in range(KC):
            P.mm(out, wslot[:, kc, wc0:wc0 + M], src[:, kc, tok0:tok0 + n], start=(kc == 0), stop=(kc == KC - 1))

    out_tokens = []

    for b in range(NB):
        for l in layers:
            need_ctx = (l == 0)
            lam_init = 0.8 - 0.6 * math.exp(-0.3 * l)
            tiles_in = list(range(18))
            tiles_out = list(range(18)) if need_ctx else list(range(16))
            TGo = TG512 if need_ctx else TG512[:4]
            ntok_o = NTOK if need_ctx else SEQ

            with P.phase("p1_%d" % l) as st:
                A1, B1 = affine_vecs(st, l, b, "g_mix_pre", 0, 1, "l")
                A1c, B1c = affine_vecs(st, l, 4, "g_mix_pre", 0, 1, "c")
                norm_tiles(st, tiles_in, lambda t: src_tile(l, b, t),
                           lambda t: (A1, B1) if t < 16 else (A1c, B1c), hT, lambda t: t * 128)
            if b == 0 and l == 0:
                tap("hT", hT.all(), BF16)
            if stop == "p1":
                raise _Stop()

            pv = None
            with P.phase("brA_%d" % l) as st:
                pv = P.sb(st, "pv", [128, PV_N], F32)
                P.dma("sp", pv.all(), d_pv[l])
                cosb = P.sb(st, "cos", [128, SEQ], F32)
                sinb = P.sb(st, "sin", [128, SEQ], F32)
                P.dma("sp", cosb.all(), d_k["k_cos"].all())
                P.dma("sp", sinb.all(), d_k["k_sin"].all())
                sm = P.sb(st, "asm", [128, 16], F32)
                lamt = P.sb(st, "lamt", [128, 256], F32)
                lidx = (slice(l, l + 1), slice(None), slice(None))
                P.dma("sp", lamt.all(), W["da_lambda"].view(
                    W["da_lambda"].t[lidx].rearrange("o a b -> o (a b)").broadcast_to([128, 256]), lidx))
                jk256 = P.sb(st, "jk256", [128, 64], F32)
                P.stt(jk256.all(), lamt[:, 0:64], 1.0, lamt[:, 64:128], ALU.mult, ALU.mult, accum=sm[:, 0:1])
                P.stt(jk256.all(), lamt[:, 128:192], 1.0, lamt[:, 192:256], ALU.mult, ALU.mult, accum=sm[:, 1:2])
                P.act(sm[:, 2:4], sm[:, 0:2], AF.Exp)
                P.tt("dve", sm[:, 4:5], sm[:, 2:3], sm[:, 3:4], ALU.subtract)
                P.ts("dve", sm[:, 5:6], sm[:, 4:5], lam_init, ALU.add, -1.0, ALU.mult)
                neglam = sm[:, 5:6]
                gsub = bcast_load(st, "gsub", W["da_subln"], (slice(l, l + 1), slice(None)), 128)
                P.ts("dve", gsub.all(), gsub.all(), 1.0 - lam_init, ALU.mult)
                gcol = P.sb(st, "gsubc", [128, 1], F32)
                sidx_ = (l, slice(None))
                P.dma("sp", gcol.all(), W["da_subln"].view(W["da_subln"].t[l].rearrange("(p o) -> p o", o=1), sidx_))
                P.ts("dve", gcol.all(), gcol.all(), 1.0 - lam_init, ALU.mult)
                vaug = P.sb(st, "vaug", [128, NT, 512], BF16)
                wv = P.sb(st, "wAv", [128, 8, 512], BF16)
                P.dma("pool", wv.all(), wview(W["w_in"], (l,), 0, 1024, OFF["av"], 512))
                rb = Rot([0, 1, 2, 3])
                for t in range(NT):
                    bk = rb()
                    o = P.pf(bk, 0, 512)
                    for kc in range(8):
                        P.mm(o, hT[:, kc, t * 128:(t + 1) * 128], wv[:, kc, :], start=(kc == 0), stop=(kc == 7))
                    P.copy("act" if t % 2 else "dve", vaug[:, t, :], o)
                if stop == "brA_v":
                    raise _Stop()
                qT = P.sb(st, "qT", [128, NTOK], BF16)
                qz = [P.sb(st, "qz%d" % c, [128, NTOK], BF16) for c in range(2)]
                P.memset("pool", qz[0][64:128, :], 0.0)
                P.memset("pool", qz[1][0:64, :], 0.0)
                kT = P.sb(st, "kT", [128, NTOK], BF16)
                wqk = [P.sb(st, "wqk%d" % i, [128, 8, 256], BF16) for i in range(2)]
                xsb = [P.sb(st, "xsb%d" % i, [128, 512], BF16) for i in range(2)]
                t1 = [P.sb(st, "rt1_%d" % i, [128, 512], F32) for i in range(2)]
                t2 = [P.sb(st, "rt2_%d" % i, [128, 512], F32) for i in range(2)]
                Et = [P.sb(st, "Et%d" % i, [128, 1024], BF16) for i in range(3)]
                rd = [P.sb(st, "ard%d" % i, [128, 512], F32) for i in range(2)]
                o1b = [P.sb(st, "ao1_%d" % i, [128, 512], F32) for i in range(2)]
                o2b = [P.sb(st, "ao2_%d" % i, [128, 512], F32) for i in range(2)]
                sqa = [P.sb(st, "asq%d" % i, [128, 512], BF16) for i in range(2)]
                rsa = [P.sb(st, "ars%d" % i, [128, 512], F32) for i in range(2)]
                ostage = [P.sb(st, "oast%d" % i, [128, NTOK], BF16) for i in range(2)]
                rproj = Rot([4, 5, 6, 7])
                rsc = Rot([4, 5, 6])
                cnt = [0]
                for h in range(4):
                    wq = wqk[h % 2]
                    P.dma("pool", wq[:, :, 0:128], wview(W["w_in"], (l,), 0, 1024, OFF["aq"] + h * 128, 128))
                    P.dma("pool", wq[:, :, 128:256], wview(W["w_in"], (l,), 0, 1024, OFF["ak"] + h * 128, 128))
                    for which, dst in ((0, qT), (1, kT)):
                        for (tok0, n) in TG512:
                            if which == 0 and tok0 >= SEQ and not need_ctx:
                                continue
                            bk = rproj()
                            o = P.pf(bk, 0, n)
                            proj_fm(o, wq, which * 128, 128, hT, tok0, n)
                            if tok0 >= SEQ or os.environ.get("NOROPE"):
                                P.copy("act", dst[:, tok0:tok0 + n], o)
                                continue
                            i2 = cnt[0] % 2
                            cnt[0] += 1
                            P.copy("act", xsb[i2][:, 0:n], o)
                            b2 = rproj()
                            o2 = P.pf(b2, 0, n)
                            P.mm(o2, rotb, xsb[i2][:, 0:n])
                            P.tt("dve", t1[i2][:, 0:n], o, cosb[:, tok0:tok0 + n], ALU.mult)
                            P.tt("dve", t2[i2][:, 0:n], o2, sinb[:, tok0:tok0 + n], ALU.mult)
                            P.tt("pool", dst[:, tok0:tok0 + n], t1[i2][:, 0:n], t2[i2][:, 0:n], ALU.add)
                    if b == 0 and l == 0 and h == 0:
                        tap("qT0", qT.all(), BF16)
                    if stop == "brA_qk":
                        tap("kT0", kT.all(), BF16)
                        raise _Stop()
                    nq_ = NTOK if need_ctx else SEQ
                    P.copy("pool", qz[0][0:64, 0:nq_], qT[0:64, 0:nq_])
                    P.copy("pool", qz[1][64:128, 0:nq_], qT[64:128, 0:nq_])
                    osg = ostage[h % 2]
                    qgroups = [(tok0, n, list(range(NT))) for (tok0, n) in TG512[:4]]
                    if need_ctx:
                        qgroups.append((SEQ, CTX, [16, 17]))
                    pend_norm = []
                    pbanks = [4, 6]
                    gi_ = [0]
                    for (q0, nq, ktiles) in qgroups:
                        nk = len(ktiles)
                        assert nk % 2 == 0
                        seq = [(c, kp) for c in (0, 1) for kp in range(nk // 2)]

                        def S(i, q0=q0, nq=nq, ktiles=ktiles, seq=seq):
                            c, kp = seq[i]
                            b0 = pbanks[i % 2]
                            for u in range(2):
                                kt = ktiles[2 * kp + u]
                                P.mm(P.pf(b0 + u, 0, nq), kT[:, kt * 128:(kt + 1) * 128], qz[c][:, q0:q0 + nq])
                            src = V(P.psall[:, b0 * 512:(b0 + 2) * 512].rearrange("p (u n) -> p u n", u=2)[:, :, 0:nq],
                                    P.psb[b0], ((0, 128), (0, 512)))
                            et = Et[i % 3]
                            dst = V(et.t[:, :].rearrange("p (u n) -> p u n", u=2)[:, :, 0:nq], et, ((0, 128), (0, 1024)))
                            P.act(dst, src, AF.Exp, scale=0.125, xins=[P.pf(b0 + 1, 0, 512)])

                        S(0)
                        S(1)
                        while pend_norm:
                            pend_norm.pop(0)()
                        for i, (c, kp) in enumerate(seq):
                            et = Et[i % 3]
                            for u in range(2):
                                ki = 2 * kp + u
                                kt = ktiles[ki]
                                ev = et[:, u * 512:u * 512 + nq]
                                P.mm(P.pf(c, 0, nq), vaug[:, kt, h * 128:(h + 1) * 128], ev,
                                     start=(ki == 0), stop=(ki == nk - 1))
                                P.mm(P.pf(2 + c, 0, nq), onesb, ev, start=(ki == 0), stop=(ki == nk - 1))
                            if i + 2 < len(seq):
                                S(i + 2)

                        def norm(q0=q0, nq=nq):
                            g2 = gi_[0] % 2
                            gi_[0] += 1
                            P.recip(rd[0][:, 0:nq], P.pf(2, 0, nq))
                            P.tt("dve", o1b[g2][:, 0:nq], P.pf(0, 0, nq), rd[0][:, 0:nq], ALU.mult)
                            P.recip(rd[1][:, 0:nq], P.pf(3, 0, nq))
                            P.tt("dve", o2b[g2][:, 0:nq], P.pf(1, 0, nq), rd[1][:, 0:nq], ALU.mult)
                            P.stt(o1b[g2][:, 0:nq], o2b[g2][:, 0:nq], neglam, o1b[g2][:, 0:nq], ALU.mult, ALU.add)
                            P.tt("pool", sqa[g2][:, 0:nq], o1b[g2][:, 0:nq], o1b[g2][:, 0:nq], ALU.mult)
                            P.mm(P.pf(7, 0, nq), od128, sqa[g2][:, 0:nq])
                            rsqrt(rsa[g2][:, 0:nq], P.pf(7, 0, nq), 1.0)
                            P.stt(osg[:, q0:q0 + nq], o1b[g2][:, 0:nq], gcol[:, 0:1], rsa[g2][:, 0:nq], ALU.mult, ALU.mult)
                        pend_norm.append(norm)
                    while pend_norm:
                        pend_norm.pop(0)()
                        if stop == "brA_g1":
                            tap("osg", osg[:, 0:512], BF16)
                            raise _Stop()
                    P.dma("sp", d_br[0][h * 128:(h + 1) * 128, 0:ntok_o], osg[:, 0:ntok_o])
            if stop == "brA":
                dump_br(0)
                raise _Stop()

            with P.phase("brB_%d" % l) as st:
                pv = P.sb(st, "pv", [128, PV_N], F32)
                P.dma("sp", pv.all(), d_pv[l])
                PADW = 2364
                cb = P.sb(st, "cb", [128, 4, PADW], BF16)
                P.memset("dve", cb.all(), 0.0)
                wB = P.sb(st, "wB", [128, 8, 1024], BF16)
                P.dma("pool", wB.all(), wview(W["w_in"], (l,), 0, 1024, OFF["cin"], 1024))
                Dm = P.sb(st, "Dm", [128, 4, 31, 128], BF16)
                for j4 in range(4):
                    for k in range(31):
                        c_ = PV_DW + j4 * 31 + k
                        if k % 2:
                            P.act(Dm[:, j4, k, :], identf, AF.Copy, scale=pv[:, c_:c_ + 1])
                        else:
                            P.ts("dve", Dm[:, j4, k, :], identf, pv[:, c_:c_ + 1], ALU.mult)
                sig = [P.sb(st, "sig%d" % i, [128, 512], F32) for i in range(2)]
                pairs = Rot([(0, 1), (2, 3), (4, 5), (6, 7)])
                k_ = 0
                for j4 in range(4):
                    for (tok0, n) in TGo:
                        ba, bg = pairs()
                        proj_fm(P.pf(ba, 0, n), wB, j4 * 128, 128, hT, tok0, n)
                        proj_fm(P.pf(bg, 0, n), wB, 512 + j4 * 128, 128, hT, tok0, n)
                        sg_ = sig[k_ % 2]
                        k_ += 1
                        P.act(sg_[:, 0:n], P.pf(bg, 0, n), AF.Sigmoid)
                        base = 15 + tok0 if tok0 < SEQ else 2078 + 15
                        P.tt("dve", cb[:, j4, base:base + n], P.pf(ba, 0, n), sg_[:, 0:n], ALU.mult)
                cvf = P.sb(st, "cvf", [128, 4, 512], F32)
                cvb = P.sb(st, "cvb", [128, 4, 512], BF16)
                sq = P.sb(st, "cvsq", [128, 4, 512], BF16)
                mean_s = P.sb(st, "cvmean", [128, 512], F32)
                m2 = P.sb(st, "cvm2", [128, 512], F32)
                var = P.sb(st, "cvvar", [128, 512], F32)
                rstd = P.sb(st, "cvrstd", [128, 512], F32)
                xc_ = [P.sb(st, "cvxc%d" % i, [128, 512], F32) for i in range(2)]
                obst = [P.sb(st, "obst%d" % i, [128, 4, 512], BF16) for i in range(2)]
                rconv = Rot([0, 1, 2, 3])
                rstat = Rot([(4, 5), (6, 7)])
                for gi, (tok0, n) in enumerate(TGo):
                    base = tok0 if tok0 < SEQ else 2078
                    for j4 in range(4):
                        bk = rconv()
                        o = P.pf(bk, 0, n)
                        for k in range(31):
                            P.mm(o, Dm[:, j4, k, :], cb[:, j4, base + k:base + k + n], start=(k == 0), stop=(k == 30))
                        P.act(cvf[:, j4, 0:n], o, AF.Identity, bias=pv[:, PV_DWB + j4:PV_DWB + j4 + 1])
                        P.copy("dve", cvb[:, j4, 0:n], cvf[:, j4, 0:n])
                        P.tt("pool", sq[:, j4, 0:n], cvf[:, j4, 0:n], cvf[:, j4, 0:n], ALU.mult)
                    bm, bq = rstat()
                    for j4 in range(4):
                        P.mm(P.pf(bm, 0, n), od512, cvb[:, j4, 0:n], start=(j4 == 0), stop=(j4 == 3))
                    for j4 in range(4):
                        P.mm(P.pf(bq, 0, n), od512, sq[:, j4, 0:n], start=(j4 == 0), stop=(j4 == 3))
                    P.copy("act", mean_s[:, 0:n], P.pf(bm, 0, n))
                    P.tt("pool", m2[:, 0:n], mean_s[:, 0:n], mean_s[:, 0:n], ALU.mult)
                    P.tt("dve", var[:, 0:n], P.pf(bq, 0, n), m2[:, 0:n], ALU.subtract)
                    rsqrt(rstd[:, 0:n], var[:, 0:n], 1.0)
                    ost = obst[gi % 2]
                    for j4 in range(4):
                        x_ = xc_[j4 % 2]
                        P.tt("dve", x_[:, 0:n], cvf[:, j4, 0:n], mean_s[:, 0:n], ALU.subtract)
                        P.tt("pool", x_[:, 0:n], x_[:, 0:n], rstd[:, 0:n], ALU.mult)
                        P.act(ost[:, j4, 0:n], x_[:, 0:n], AF.Silu, scale=pv[:, PV_LNG + j4:PV_LNG + j4 + 1],
                              bias=pv[:, PV_LNB + j4:PV_LNB + j4 + 1])
                    idx = (slice(None), slice(tok0, tok0 + n))
                    P.dma("sp", d_br[1].view(d_br[1].t[:, tok0:tok0 + n].rearrange("(j p) n -> p j n", p=128), idx),
                          ost[:, :, 0:n])
            if stop == "brB":
                dump_br(1)
                raise _Stop()

            with P.phase("brC_%d" % l) as st:
                pv = P.sb(st, "pv", [128, PV_N], F32)
                P.dma("sp", pv.all(), d_pv[l])
                nab = P.sb(st, "nab", [128, 4], F32)
                P.ts("dve", nab.all(), pv[:, PV_AB:PV_AB + 4], -1.0, ALU.mult)
                wgg = P.sb(st, "wgg", [128, 8, 512], BF16)
                P.dma("pool", wgg.all(), wview(W["w_in"], (l,), 0, 1024, OFF["gg"], 512))
                qdz = [P.sb(st, "qdz%d" % d, [128, 2, NT, 256], BF16) for d in range(2)]
                for d in range(2):
                    P.memset("dve" if d else "pool", qdz[d].all(), 0.0)
                kiT = [P.sb(st, "kiT%d" % d, [128, 2, NTOK], BF16) for d in range(2)]
                dec = P.sb(st, "dec", [128, 2, 2, NT], F32)
                full2 = (slice(None), slice(None))
                ki_tm = [P.sb(st, "kitm%d" % d, [128, NT, 256], BF16) for d in range(2)]
                v_tm = P.sb(st, "vtm", [128, NT, 512], BF16)
                with P.phase() as stv:
                    wv = P.sb(stv, "wgv", [128, 8, 512], BF16)
                    P.dma("pool", wv.all(), wview(W["w_in"], (l,), 0, 1024, OFF["gv"], 512))
                    rbv = Rot(range(8))
                    for t in range(NT):
                        bk = rbv()
                        o = P.pf(bk, 0, 512)
                        for kc in range(8):
                            P.mm(o, hT[:, kc, t * 128:(t + 1) * 128], wv[:, kc, :], start=(kc == 0), stop=(kc == 7))
                        P.copy("act" if t % 2 else "dve", v_tm[:, t, :], o)
                with P.phase() as st2:
                    smask = P.sb(st2, "smask", [128, NTOK], F32)
                    P.dma("sp", smask.all(), d_k["k_smask"].all())
                    aup = P.sb(st2, "aup", [16, 2, 256], BF16)
                    aidx = (l, slice(None), slice(None), slice(None))
                    P.dma("pool", aup.all(), W["gla_a_up"].view(W["gla_a_up"].t[l].rearrange("d r n -> r d n"), aidx))
                    wga = P.sb(st2, "wga", [128, 8, 32], BF16)
                    P.dma("pool", wga.all(), wview(W["w_in"], (l,), 0, 1024, OFF["ga"], 32))
                    wqk = P.sb(st2, "wgqk", [128, 8, 512], BF16)
                    P.dma("pool", wqk.all(), wview(W["w_in"], (l,), 0, 1024, OFF["gq"], 512))
                    gaT = [P.sb(st2, "gaT%d" % d, [16, NTOK], BF16) for d in range(2)]
                    rb = Rot(range(8))
                    for d in range(2):
                        for (tok0, n) in TG512:
                            bk = rb()
                            o = P.pf(bk, 0, n, 0, 16)
                            proj_fm(o, wga, d * 16, 16, hT, tok0, n)
                            P.copy("act", gaT[d][:, tok0:tok0 + n], o)
                    spt = P.sb(st2, "spt", [128, NTOK], F32)
                    cs = P.sb(st2, "cs", [128, NTOK], F32)
                    Eq = P.sb(st2, "Eq", [128, NTOK], F32)
                    Ek = P.sb(st2, "Ek", [128, NTOK], F32)
                    tmpe = [P.sb(st2, "tmpe%d" % i, [128, 512], F32) for i in range(2)]

                    def v3(tt_):
                        return V(tt_.t[:, :].rearrange("p (c i) -> p c i", i=128), tt_, tt_.reg_of(full2))
                    k_ = 0
                    for d in range(2):
                        for fc in range(2):
                            for (tok0, n) in TG512:
                                bk = rb()
                                o = P.pf(bk, 0, n)
                                P.mm(o, aup[0:16, d, fc * 128:(fc + 1) * 128], gaT[d][0:16, tok0:tok0 + n])
                                te = tmpe[k_ % 2]
                                k_ += 1
                                ci = d * 2 + fc
                                P.act(te[:, 0:n], o, AF.Exp, scale=-1.0, bias=nab[:, ci:ci + 1])
                                P.act(spt[:, tok0:tok0 + n], te[:, 0:n], AF.Ln, bias=1.0)
                            P.scan(cs.all(), smask.all(), spt.all(), 0.0, ALU.mult, ALU.add)
                            cs3 = v3(cs)
                            tot = V(cs3.ap[:, :, 127:128].broadcast_to([128, NT, 128]), cs, cs.reg_of(full2))
                            if d == 1:
                                P.tt("dve", v3(Eq), tot, cs3, ALU.subtract)
                                P.tt("pool", v3(Eq), v3(Eq), v3(spt), ALU.add)
                                Ssrc = Eq
                            else:
                                Ssrc = cs
                            P.act(dec[:, d, fc, :], V(cs3.ap[:, :, 127], cs, cs.reg_of(full2)), AF.Exp, scale=-1.0 / 16)
                            P.act(Ek.all(), Ssrc.all(), AF.Exp, scale=1.0 / 16)
                            P.act(Eq.all(), Ssrc.all(), AF.Exp, scale=-1.0 / 16, bias=math.log(0.125))
                            for (tok0, n) in TG512:
                                bq_ = rb()
                                proj_fm(P.pf(bq_, 0, n), wqk, fc * 128, 128, hT, tok0, n)
                                t0_, nt_ = tok0 // 128, n // 128
                                for hh in range(2):
                                    r0, r1 = hh * 64, hh * 64 + 64
                                    ps3 = V(P.psb[bq_].t[r0:r1, 0:n].rearrange("p (t i) -> p t i", i=128), P.psb[bq_],
                                            ((r0, r1), (0, n)))
                                    eq3 = V(Eq.t[r0:r1, tok0:tok0 + n].rearrange("p (t i) -> p t i", i=128), Eq,
                                            ((r0, r1), (tok0, tok0 + n)))
                                    P.tt("dve", qdz[d][r0:r1, fc, t0_:t0_ + nt_, hh * 128:(hh + 1) * 128], ps3, eq3,
                                         ALU.mult)
                                bk_ = rb()
                                proj_fm(P.pf(bk_, 0, n), wqk, 256 + fc * 128, 128, hT, tok0, n)
                                P.tt("dve", kiT[d][:, fc, tok0:tok0 + n], P.pf(bk_, 0, n), Ek[:, tok0:tok0 + n], ALU.mult)
                for d in range(2):
                    for t in range(NT):
                        bk = rb()
                        for fc in range(2):
                            P.tr(P.pb(bk, fc * 128, (fc + 1) * 128), kiT[d][:, fc, t * 128:(t + 1) * 128], identb,
                                 signal=(fc == 1))
                        P.copy("act" if t % 2 else "dve", ki_tm[d][:, t, :], P.pb(bk, 0, 256))
                Sst = [[P.sb(st, "Sst%d%d" % (d, fc), [128, NT, 256], BF16) for fc in range(2)] for d in range(2)]
                sfl = [[P.sb(st, "sfl%d%d" % (d, fc), [128, 256], F32) for fc in range(2)] for d in range(2)]
                tmpS = [[P.sb(st, "tmpS%d%d" % (d, fc), [128, 256], F32) for fc in range(2)] for d in range(2)]
                order = {0: [16, 17] + list(range(16)), 1: [17, 16] + list(range(15, -1, -1))}
                for d in range(2):
                    for fc in range(2):
                        P.memset("dve", sfl[d][fc].all(), 0.0)
                for i in range(NT):
                    for d in range(2):
                        for fc in range(2):
                            n_ = order[d][i]
                            s_ = sfl[d][fc]
                            P.copy("pool", Sst[d][fc][:, n_, :], s_.all())
                            if i == NT - 1:
                                continue
                            bk = rb()
                            o = P.pf(bk, 0, 256)
                            P.mm(o, ki_tm[d][:, n_, fc * 128:(fc + 1) * 128], v_tm[:, n_, fc * 256:(fc + 1) * 256])
                            P.tt("dve", tmpS[d][fc].all(), o, s_.all(), ALU.add)
                            P.act(s_.all(), tmpS[d][fc].all(), AF.Copy, scale=dec[:, d, fc, n_:n_ + 1])
                gnv = pv[:, PV_GN:PV_GN + 1]
                PT = [[P.sb(st, "PT%d%d" % (i, d), [128, 256], BF16) for d in range(2)] for i in range(2)]
                og = [P.sb(st, "og%d" % i, [128, 512], F32) for i in range(2)]
                sqb = [P.sb(st, "sqb%d" % i, [128, 512], BF16) for i in range(2)]
                rstg = [P.sb(st, "rstg%d" % i, [128, 512], F32) for i in range(2)]
                sgg = [P.sb(st, "sgg%d" % i, [128, 512], F32) for i in range(2)]
                ocst = [P.sb(st, "ocst%d" % i, [128, 4, 512], BF16) for i in range(2)]
                mask3 = [V(kc_f.t[:, 256 + 128 * d:384 + 128 * d].rearrange("p (o i) -> p o i", o=1).broadcast_to(
                    [128, 2, 128]), kc_f, ((0, 128), (256 + 128 * d, 384 + 128 * d))) for d in range(2)]
                rot_o = Rot([(0, 1), (2, 3)])
                rot_s = Rot([4, 5, 6])
                k_ = 0
                hcount = 0
                for gi, (tok0, n) in enumerate(TGo):
                    tl = list(range(tok0 // 128, (tok0 + n) // 128))
                    ost = ocst[gi % 2]
                    for fc in range(2):
                        bo = rot_o()
                        for ti, t in enumerate(tl):
                            cols = slice(t * 128, (t + 1) * 128)
                            pts = []
                            for d in range(2):
                                bs = rot_s()
                                P.mm(P.pf(bs, 0, 256), kiT[d][:, fc, cols], qdz[d][:, fc, t, :])
                                pt = PT[k_ % 2][d]
                                ps3 = V(P.psb[bs].t[:, 0:256].rearrange("p (o i) -> p o i", o=2), P.psb[bs],
                                        ((0, 128), (0, 256)))
                                pt3 = V(pt.t[:, :].rearrange("p (o i) -> p o i", o=2), pt, ((0, 128), (0, 256)))
                                P.tt("dve", pt3, ps3, mask3[d], ALU.mult)
                                pts.append(pt)
                            k_ += 1
                            for hh in range(2):
                                h = 2 * fc + hh
                                hs = slice(hh * 128, (hh + 1) * 128)
                                o = P.pf(bo[hh], ti * 128, (ti + 1) * 128)
                                P.mm(o, v_tm[:, t, h * 128:(h + 1) * 128], pts[0][:, hs], start=True, stop=False)
                                P.mm(o, Sst[0][fc][:, t, hs], qdz[0][:, fc, t, hs], start=False, stop=False)
                                P.mm(o, v_tm[:, t, h * 128:(h + 1) * 128], pts[1][:, hs], start=False, stop=False)
                                P.mm(o, Sst[1][fc][:, t, hs], qdz[1][:, fc, t, hs], start=False, stop=True)
                        for hh in range(2):
                            h = 2 * fc + hh
                            i2 = hcount % 2
                            hcount += 1
                            P.copy("act", og[i2][:, 0:n], P.pf(bo[hh], 0, n))
                            P.tt("pool", sqb[i2][:, 0:n], og[i2][:, 0:n], og[i2][:, 0:n], ALU.mult)
                            P.mm(P.pf(7, 0, n), od128, sqb[i2][:, 0:n])
                            rsqrt(rstg[i2][:, 0:n], P.pf(7, 0, n), 1.0)
                            bg = rot_s()
                            proj_fm(P.pf(bg, 0, n), wgg, h * 128, 128, hT, tok0, n)
                            P.act(sgg[i2][:, 0:n], P.pf(bg, 0, n), AF.Silu)
                            P.tt("dve", og[i2][:, 0:n], og[i2][:, 0:n], rstg[i2][:, 0:n], ALU.mult)
                            P.stt(ost[:, h, 0:n], og[i2][:, 0:n], gnv, sgg[i2][:, 0:n], ALU.mult, ALU.mult)
                    idx = (slice(None), slice(tok0, tok0 + n))
                    P.dma("sp", d_br[2].view(d_br[2].t[:, tok0:tok0 + n].rearrange("(j p) n -> p j n", p=128), idx),
                          ost[:, :, 0:n])
            if stop == "brC":
                dump_br(2)
                raise _Stop()

            with P.phase("merge_%d" % l) as st:
                mT = P.sb(st, "mT", [128, 8, NTOK], BF16)
                with P.phase() as st2:
                    oX = [P.sb(st2, "oX%d" % i, [128, 4, NTOK], BF16) for i in range(3)]
                    for i in range(3):
                        idx = (slice(None), slice(0, ntok_o))
                        P.dma("sp", oX[i][:, :, 0:ntok_o],
                              d_br[i].view(d_br[i].t[:, 0:ntok_o].rearrange("(j p) n -> p j n", p=128), idx))
                    wg = [P.sb(st2, "wg%d" % i, [128, 8, 384], BF16) for i in range(2)]
                    wp = [P.sb(st2, "wp%d" % i, [128, 4, 384], BF16) for i in range(2)]
                    sgm = [P.sb(st2, "sgm%d" % i, [128, 512], F32) for i in range(2)]
                    m_ = [P.sb(st2, "mm%d" % i, [128, 512], F32) for i in range(2)]
                    t_ = [P.sb(st2, "mt%d" % i, [128, 512], F32) for i in range(2)]
                    pairs = Rot([(0, 1), (2, 3), (4, 5), (6, 7)])
                    k_ = 0
                    g_i = 0
                    pnames = ("da_proj", "cv_proj", "gla_proj")
                    for fo in range(8):
                        g_, p_ = wg[fo % 2], wp[fo % 2]
                        for br in range(3):
                            P.dma("pool", g_[:, :, br * 128:(br + 1) * 128],
                                  wview(W["w_in"], (l,), 0, 1024, OFF["gates"] + br * 1024 + fo * 128, 128))
                            P.dma("pool", p_[:, :, br * 128:(br + 1) * 128],
                                  wview(W[pnames[br]], (l,), 0, 512, fo * 128, 128))
                        for (tok0, n) in TGo:
                            i2 = g_i % 2
                            g_i += 1
                            for br in range(3):
                                by, bg = pairs()
                                proj_fm(P.pf(by, 0, n), p_, br * 128, 128, oX[br], tok0, n, KC=4)
                                proj_fm(P.pf(bg, 0, n), g_, br * 128, 128, hT, tok0, n)
                                sg_ = sgm[k_ % 2]
                                tt_ = t_[k_ % 2]
                                k_ += 1
                                P.act(sg_[:, 0:n], P.pf(bg, 0, n), AF.Sigmoid)
                                if br == 0:
                                    P.tt("dve", m_[i2][:, 0:n], P.pf(by, 0, n), sg_[:, 0:n], ALU.mult)
                                elif br == 1:
                                    P.tt("dve", tt_[:, 0:n], P.pf(by, 0, n), sg_[:, 0:n], ALU.mult)
                                    P.tt("dve", m_[i2][:, 0:n], m_[i2][:, 0:n], tt_[:, 0:n], ALU.add)
                                else:
                                    P.tt("dve", tt_[:, 0:n], P.pf(by, 0, n), sg_[:, 0:n], ALU.mult)
                                    P.tt("pool", mT[:, fo, tok0:tok0 + n], m_[i2][:, 0:n], tt_[:, 0:n], ALU.add)
                if b == 0 and l == 0:
                    tap("mT", mT.all(), BF16)
                wout = P.sb(st, "wout", [128, 8, 1024], BF16)
                P.dma("pool", wout.all(), wview(W["w_out"], (l,), 0, 1024, 0, 1024))
                G1 = gate_vec(st, l, b, "g_mix_post", 2, "l")
                G1c = gate_vec(st, l, 4, "g_mix_post", 2, "c") if need_ctx else None
                xin = [P.sb(st, "rxin%d" % i, [128, 1024], F32) for i in range(3)]
                tm = [P.sb(st, "rtm%d" % i, [128, 1024], F32) for i in range(2)]
                xo = [P.sb(st, "rxo%d" % i, [128, 1024], F32) for i in range(2)]
                rst = P.sb(st, "rstat", [128, 4 * NT], F32)
                pairs = Rot([(0, 1), (2, 3), (4, 5), (6, 7)])
                for i, t in enumerate(tiles_out):
                    bks = pairs()
                    for hf in range(2):
                        o = P.pf(bks[hf], 0, 512)
                        for kc in range(8):
                            P.mm(o, mT[:, kc, t * 128:(t + 1) * 128], wout[:, kc, hf * 512:(hf + 1) * 512],
                                 start=(kc == 0), stop=(kc == 7))
                    xt, tmi, xoi = xin[i % 3], tm[i % 2], xo[i % 2]
                    P.dma("sp", xt.all(), src_tile(l, b, t))
                    ss0, ss1, ss, rs = (rst[:, 4 * i + q:4 * i + q + 1] for q in range(4))
                    P.act(tmi[:, 0:512], P.pf(bks[0], 0, 512), AF.Square, accum=ss0)
                    P.act(tmi[:, 512:1024], P.pf(bks[1], 0, 512), AF.Square, accum=ss1)
                    P.tt("dve", ss, ss0, ss1, ALU.add)
                    rsqrt(rs, ss, 1.0 / D)
                    G = G1 if t < 16 else G1c
                    for hf in range(2):
                        P.stt(tmi[:, hf * 512:(hf + 1) * 512], P.pf(bks[hf], 0, 512), rs, G[:, hf * 512:(hf + 1) * 512],
                              ALU.mult, ALU.mult)
                    P.tt("pool", xoi[:, 0:384], tmi[:, 0:384], xt[:, 0:384], ALU.add)
                    P.tt("dve", xoi[:, 384:1024], tmi[:, 384:1024], xt[:, 384:1024], ALU.add)
                    P.dma("sp", d_xs[t * 128:(t + 1) * 128, :], xoi.all())
            if stop == "mixer" and l == layers[-1]:
                dump_xs()
                raise _Stop()

            moe = (l == 1)
            nexp = NEXP if moe else 1
            if moe:
                stl = [list(range(0, 8)), list(range(8, 16))]
            else:
                stl = [list(range(0, 9)), list(range(9, 18))]
            for tl in stl:
                with P.phase("ffn_%d" % l) as st:
                    T = len(tl) * 128
                    h2T = hT
                    with P.phase() as st2:
                        A2, B2 = affine_vecs(st2, l, b, "g_ffn_pre", 3, 4, "l")
                        if need_ctx:
                            A2c, B2c = affine_vecs(st2, l, 4, "g_ffn_pre", 3, 4, "c")
                        norm_tiles(st2, tl, lambda t: d_xs[t * 128:(t + 1) * 128, :],
                                   lambda t: (A2, B2) if t < 16 else (A2c, B2c), h2T, lambda t: (t - tl[0]) * 128)
                    acc = P.sb(st, "acc", [128, len(tl), 1024], F32)
                    if moe:
                        gates = P.sb(st, "gates", [128, len(tl), 8], F32)
                    st3 = ExitStack()
                    st3.__enter__()
                    aT = P.sb(st3, "aT", [128, NFC, T], BF16)
                    w13 = [P.sb(st3, "w13_%d" % i, [128, 8, 2, 256], BF16) for i in range(3)]
                    w2h = [P.sb(st3, "w2h%d" % i, [128, NFC, 512], BF16) for i in range(2)]
                    sl = [P.sb(st3, "sl%d" % i, [128, 512], F32) for i in range(2)]
                    rup = Rot([(0, 1), (2, 3)])
                    rdn = Rot([4, 5, 6, 7])
                    if moe:
                        rw = P.sb(st3, "rw", [128, 8, 8], BF16)
                        ridx = (0, slice(None), slice(None))
                        P.dma("pool", rw.all(), W["moe_router"].view(
                            W["moe_router"].t[0].rearrange("(k p) e -> p k e", p=128), ridx))
                        lg = P.sb(st3, "lg", [128, len(tl), 8], F32)
                        l2 = P.sb(st3, "l2", [128, len(tl), 8], F32)
                        eq1 = P.sb(st3, "eq1", [128, len(tl), 8], F32)
                        eq2 = P.sb(st3, "eq2", [128, len(tl), 8], F32)
                        gs = P.sb(st3, "gs", [128, len(tl), 8], F32)
                        for j in range(len(tl)):
                            bk = rdn()
                            o = P.pf(bk, 0, 8)
                            for kc in range(8):
                                P.mm(o, h2T[:, kc, j * 128:(j + 1) * 128], rw[:, kc, :], start=(kc == 0), stop=(kc == 7))
                            P.copy("dve", lg[:, j, :], o)
                            m1, m2_, dd, ee, den, w1, w2 = (gs[:, j, q:q + 1] for q in range(7))
                            P.rmax(m1, lg[:, j, :])
                            P.ts("dve", eq1[:, j, :], lg[:, j, :], m1, ALU.is_equal)
                            P.stt(l2[:, j, :], eq1[:, j, :], -1e30, lg[:, j, :], ALU.mult, ALU.add)
                            P.rmax(m2_, l2[:, j, :])
                            P.ts("dve", eq2[:, j, :], l2[:, j, :], m2_, ALU.is_equal)
                            P.tt("dve", dd, m2_, m1, ALU.subtract)
                            P.act(ee, dd, AF.Exp)
                            P.ts("dve", den, ee, 1.0, ALU.add)
                            P.recip(w1, den)
                            P.tt("dve", w2, ee, w1, ALU.mult)
                            P.ts("dve", gates[:, j, :], eq1[:, j, :], w1, ALU.mult)
                            P.stt(gates[:, j, :], eq2[:, j, :], w2, gates[:, j, :], ALU.mult, ALU.add)
                    subs = [(s0, 384) for s0 in range(0, T, 384)] if T % 512 else [(s0, 512) for s0 in range(0, T, 512)]
                    k_ = 0
                    for e in range(nexp):
                        if moe:
                            w1d, w3d, w2d, pre = W["moe_w1"], W["moe_w3"], W["moe_w2"], (0, e)
                        else:
                            w1d, w3d, w2d, pre = W["ffn_w1"], W["ffn_w3"], W["ffn_w2"], (0,)

                        def load_w2(hf):
                            wh = w2h[hf]
                            idx = pre + (slice(None), slice(hf * 512, (hf + 1) * 512))
                            P.dma("pool", wh.all(), w2d.view(w2d.t[idx].rearrange("(f p) n -> p f n", p=128), idx))
                        for fp in range(NFC // 2):
                            ws = w13[(e * (NFC // 2) + fp) % 3]
                            P.dma("pool", ws[:, :, 0, :], wview(w1d, pre, 0, 1024, fp * 256, 256))
                            P.dma("pool", ws[:, :, 1, :], wview(w3d, pre, 0, 1024, fp * 256, 256))
                            if fp == 3:
                                load_w2(0)
                            if fp == 7:
                                load_w2(1)
                            for fi in range(2):
                                fc = fp * 2 + fi
                                for (s0, n) in subs:
                                    b1_, b3_ = rup()
                                    for kc in range(8):
                                        P.mm(P.pf(b1_, 0, n), ws[:, kc, 0, fi * 128:(fi + 1) * 128], h2T[:, kc, s0:s0 + n],
                                             start=(kc == 0), stop=(kc == 7))
                                    for kc in range(8):
                                        P.mm(P.pf(b3_, 0, n), ws[:, kc, 1, fi * 128:(fi + 1) * 128], h2T[:, kc, s0:s0 + n],
                                             start=(kc == 0), stop=(kc == 7))
                                    sl_ = sl[k_ % 2]
                                    k_ += 1
                                    P.act(sl_[:, 0:n], P.pf(b1_, 0, n), AF.Silu)
                                    P.tt("dve", aT[:, fc, s0:s0 + n], P.pf(b3_, 0, n), sl_[:, 0:n], ALU.mult)
                        for hf in range(2):
                            wh = w2h[hf]
                            for j in range(len(tl)):
                                bk = rdn()
                                o = P.pf(bk, 0, 512)
                                for fc in range(NFC):
                                    P.mm(o, aT[:, fc, j * 128:(j + 1) * 128], wh[:, fc, :], start=(fc == 0),
                                         stop=(fc == NFC - 1))
                                a_ = acc[:, j, hf * 512:(hf + 1) * 512]
                                if not moe:
                                    P.copy("act", a_, o)
                                elif e == 0:
                                    P.ts("dve", a_, o, gates[:, j, 0:1], ALU.mult)
                                else:
                                    P.stt(a_, o, gates[:, j, e:e + 1], a_, ALU.mult, ALU.add)
                    P.barrier()
                    st3.__exit__(None, None, None)
                    G2 = gate_vec(st, l, b, "g_ffn_post", 5, "l")
                    if need_ctx:
                        G2c = gate_vec(st, l, 4, "g_ffn_post", 5, "c")
                    xin = [P.sb(st, "fxin%d" % i, [128, 1024], F32) for i in range(2)]
                    jk = [P.sb(st, "fjk%d" % i, [128, 1024], F32) for i in range(2)]
                    xo = [P.sb(st, "fxo%d" % i, [128, 1024], F32) for i in range(2)]
                    fst = P.sb(st, "fstat", [128, 2 * len(tl)], F32)
                    for j, t in enumerate(tl):
                        xt, jki, xoi = xin[j % 2], jk[j % 2], xo[j % 2]
                        P.dma("sp", xt.all(), d_xs[t * 128:(t + 1) * 128, :])
                        ss, rs = fst[:, 2 * j:2 * j + 1], fst[:, 2 * j + 1:2 * j + 2]
                        P.stt(jki.all(), acc[:, j, :], 1.0, acc[:, j, :], ALU.mult, ALU.mult, accum=ss)
                        rsqrt(rs, ss, 1.0 / D)
                        G = G2 if t < 16 else G2c
                        P.stt(jki.all(), acc[:, j, :], rs, G.all(), ALU.mult, ALU.mult)
                        P.tt("pool", xoi[:, 0:384], jki[:, 0:384], xt[:, 0:384], ALU.add)
                        P.tt("dve", xoi[:, 384:1024], jki[:, 384:1024], xt[:, 384:1024], ALU.add)
                        if l == DEPTH - 1:
                            P.dma("sp", d_out[b, t * 128:(t + 1) * 128, :], xoi.all())
                        else:
                            P.dma("sp", d_xs[t * 128:(t + 1) * 128, :], xoi.all())
            if stop == "ffn" and l == layers[-1]:
                dump_xs()
                raise _Stop()


def make_in_maps(inp, NB, ncores, layers=(0, 1)):
    consts = _host_consts()
    pv = np.zeros((2, 128, PV_N), np.float32)
    for l in range(2):
        pv[l, :, PV_DWB:PV_DWB + 4] = inp["cv_dw_b"][l].reshape(4, 128).T
        pv[l, :, PV_LNG:PV_LNG + 4] = inp["cv_ln_g"][l].reshape(4, 128).T
        pv[l, :, PV_LNB:PV_LNB + 4] = inp["cv_ln_b"][l].reshape(4, 128).T
        pv[l, :, PV_AB:PV_AB + 4] = inp["gla_a_b"][l].reshape(4, 128).T
        pv[l, :, PV_GN] = inp["gla_norm"][l]
        pv[l, :, PV_DW:PV_DW + 124] = inp["cv_dw"][l].T.reshape(4, 128, 31).transpose(1, 0, 2).reshape(128, 124)
    maps = []
    for c in range(ncores):
        b0 = c * NB
        m = {"x": np.ascontiguousarray(inp["x"][b0:b0 + NB]), "ctx": np.ascontiguousarray(inp["ctx"][b0:b0 + NB])}
        cc = np.concatenate([inp["c"][b0:b0 + NB], np.zeros((4 - NB, D), np.float32), inp["c_ctx"][None]], 0)
        m["cT"] = np.ascontiguousarray(cc.reshape(5, 8, 128).transpose(2, 1, 0))
        m["pvec"] = pv
        m.update(consts)
        for k in WEIGHT_NAMES:
            if k not in ("cv_dw", "cv_dw_b", "cv_ln_g", "cv_ln_b", "gla_a_b", "gla_norm") \
                    and not (k.startswith("moe_") and 1 not in layers):
                m[k] = inp[k]
        maps.append(m)
    return maps


def build(NB=4, taps=(), stop=None, layers=(0, 1)):
    ctxd = {}
    try:
        _build(ctxd, NB, taps, stop, layers)
    except _Stop:
        pass
    return _finish(ctxd["nc"], ctxd["P"], ctxd["E_"], ctxd["tap_out"])


_CACHE = {}


def kernel(**inputs):
    inp = {k: np.ascontiguousarray(np.asarray(v)) for k, v in inputs.items()}
    NB = inp["x"].shape[0] // NCORES
    if "nc" not in _CACHE:
        _CACHE["nc"] = build(NB=NB)[0]
    maps = make_in_maps(inp, NB, NCORES)
    res = run_bass_kernel_spmd(_CACHE["nc"], maps, core_ids=list(range(NCORES)))
    out = np.concatenate([np.asarray(r["out"]) for r in res.results], axis=0)
    return out.astype(np.float32, copy=False)
```
